# Optimizing a Trainium2 kernel written in Bass

```python
import math
import jax
import jax.numpy as jnp
from jax import lax
import numpy as np

D_MODEL = 1024
BATCH = 8
SEQ = 4096
DEPTH = 2

GRID_W = 64
CTX_LEN = 256
N_MOD = 6
EPS = 1e-6

N_BRANCH = 4
BRANCH_W = D_MODEL // N_BRANCH
FN_GROUPS = 4
FN_GW = BRANCH_W // FN_GROUPS
RW_HD = 64
RW_HEADS = BRANCH_W // RW_HD
RW_DECAY_RANK = 32
RW_ICL_RANK = 32
RW_GATE_RANK = 64
RW_COLS = 3 * BRANCH_W + RW_DECAY_RANK + RW_ICL_RANK + RW_GATE_RANK
RW_DECAY_SCALE = math.exp(-0.5)
RW_GN_EPS = 64e-5
DN_HD = 64
DN_HEADS = BRANCH_W // DN_HD
DN_CONV = 5
DN_CHUNK = 64
S5_GW = 16
S5_GROUPS = BRANCH_W // S5_GW
S5_STATE = 64
N_EXPERTS = 16
N_EXPERT_GROUPS = 4
EXPERTS_PER_GROUP = N_EXPERTS // N_EXPERT_GROUPS
TOP_K = 2
D_EXPERT = 256

IN_SIZES = (BRANCH_W, RW_COLS, 3 * BRANCH_W, BRANCH_W, 2 * DN_HEADS, 2 * DN_HEADS, BRANCH_W, N_BRANCH * D_MODEL)
N_IN = sum(IN_SIZES)
RW_SIZES = (BRANCH_W, BRANCH_W, BRANCH_W, RW_DECAY_RANK, RW_ICL_RANK, RW_GATE_RANK)

kernel_name = "hybrid_parallel_mixer_moe_trunk"


def _split_points(sizes):
    return [int(v) for v in np.cumsum(sizes)[:-1]]


def _oriented(t, d):
    return jnp.flip(t, axis=1) if d == 1 else t


def rms_norm(x, g):
    xf = x.astype(jnp.float32)
    y = xf * lax.rsqrt(jnp.mean(xf * xf, axis=-1, keepdims=True) + EPS)
    return (y * g.astype(jnp.float32)).astype(x.dtype)


def l2_normalize(t):
    return t * lax.rsqrt(jnp.sum(t * t, axis=-1, keepdims=True) + EPS)


def adaln(cond, w_mod, b_mod):
    m = jax.nn.silu(cond) @ w_mod + b_mod
    return jnp.split(m[..., None, :], N_MOD, axis=-1)


def raster_to_columns(t, rows):
    b, l, ch = t.shape
    return t.reshape(b, rows, GRID_W, ch).swapaxes(1, 2).reshape(b, l, ch)


def columns_to_raster(t, rows):
    b, l, ch = t.shape
    return t.reshape(b, GRID_W, rows, ch).swapaxes(1, 2).reshape(b, l, ch)


def centred_token_shift(p, mu_prev, mu_next):
    prev = jnp.pad(p, ((0, 0), (1, 0), (0, 0)))[:, :-1]
    nxt = jnp.pad(p, ((0, 0), (0, 1), (0, 0)))[:, 1:]
    return p + mu_prev * (prev - p) + mu_next * (nxt - p)


def centred_depthwise_conv(u, w):
    pad = w.shape[0] // 2
    return lax.conv_general_dilated(u, w[:, None, :].astype(u.dtype), (1,), [(pad, pad)],
                                    dimension_numbers=("NWC", "WIO", "NWC"),
                                    feature_group_count=u.shape[-1])


def fourier_mix(u):
    b, l, _ = u.shape
    ug = u.astype(jnp.float32).reshape(b, l, FN_GROUPS, FN_GW)
    f = jnp.fft.fftn(ug, axes=(1, 3), norm="ortho")
    return jnp.real(f).reshape(b, l, BRANCH_W).astype(u.dtype)


def rwkv_scan(r, w, k, v, kk, a, s0):
    def step(s, inp):
        r_t, w_t, k_t, v_t, kk_t, a_t = inp
        s_kk = jnp.einsum("bhvk,bhk->bhv", s, -kk_t)
        s = (s * w_t[:, :, None, :]
             + jnp.einsum("bhv,bhk->bhvk", s_kk, kk_t * a_t)
             + jnp.einsum("bhv,bhk->bhvk", v_t, k_t))
        return s, jnp.einsum("bhvk,bhk->bhv", s, r_t)
    xs = tuple(jnp.moveaxis(t, 1, 0) for t in (r, w, k, v, kk, a))
    s_fin, ys = lax.scan(step, s0, xs)
    return jnp.moveaxis(ys, 0, 1), s_fin


def rwkv_mixer(p_c, p_l, mu, w0, w_up, a0, a_up, k_k, k_a, r_k, g_up, ln_g, ln_b, want_ctx):
    f32 = jnp.float32

    def heads(t):
        return t.reshape(t.shape[0], t.shape[1], RW_HEADS, RW_HD)

    def streams(p):
        p = centred_token_shift(p, mu[0], mu[1]).astype(f32)
        return jnp.split(p, _split_points(RW_SIZES), axis=-1)

    def scan_inputs(s, d):
        r, k, v, wl, al, _ = s
        w = jnp.exp(-RW_DECAY_SCALE * jax.nn.sigmoid(w0[d] + jnp.tanh(wl) @ w_up[d]))
        a = jax.nn.sigmoid(a0[d] + al @ a_up[d])
        kk = l2_normalize(heads(k * k_k))
        k_mod = k * (1.0 + (a - 1.0) * k_a)
        out = (heads(r), heads(w), heads(k_mod), heads(v), kk, heads(a))
        return tuple(_oriented(t, d) for t in out)

    s_c, s_l = streams(p_c), streams(p_l)
    b = p_l.shape[0]
    y_c, y_l = 0.0, 0.0
    for d in range(2):
        s0 = jnp.zeros((b, RW_HEADS, RW_HD, RW_HD), f32)
        yc_d, s_ctx = rwkv_scan(*scan_inputs(s_c, d), s0)
        yl_d, _ = rwkv_scan(*scan_inputs(s_l, d), s_ctx)
        y_c = y_c + _oriented(yc_d, d)
        y_l = y_l + _oriented(yl_d, d)

    def post(y, s, dtype):
        r, k, v, _, _, gl = s
        mean = jnp.mean(y, axis=-1, keepdims=True)
        var = jnp.mean(jnp.square(y - mean), axis=-1, keepdims=True)
        yn = ((y - mean) * lax.rsqrt(var + RW_GN_EPS)).reshape(r.shape) * ln_g + ln_b
        bonus = (jnp.sum(heads(r) * heads(k) * r_k, axis=-1, keepdims=True) * heads(v)).reshape(r.shape)
        g = jax.nn.sigmoid(gl) @ g_up
        return ((yn + bonus) * g).astype(dtype)

    out_l = post(y_l, s_l, p_l.dtype)
    out_c = post(y_c, s_c, p_c.dtype) if want_ctx else None
    return out_c, out_l


def gated_delta_chunked(q, k, v, log_a, beta, s0):
    b, l, h, n = q.shape
    nc, cs = l // DN_CHUNK, DN_CHUNK

    def chunk(t):
        return jnp.moveaxis(t.reshape((b, nc, cs) + t.shape[2:]), 2, 3)

    q = chunk(q) * (n ** -0.5)
    k = chunk(k)
    v = chunk(v)
    g = jnp.cumsum(chunk(log_a), axis=-1)
    bt = chunk(beta)
    pos = jnp.arange(cs)
    incl = pos[:, None] >= pos[None, :]
    strict = pos[:, None] > pos[None, :]
    decay = jnp.exp(jnp.where(incl, g[..., :, None] - g[..., None, :], -jnp.inf))
    kb = k * bt[..., None]
    a_low = jnp.where(strict, jnp.einsum("bnhik,bnhjk->bnhij", kb, k) * decay, 0.0)
    m = a_low + jnp.eye(cs, dtype=q.dtype)
    rhs = jnp.concatenate([v * bt[..., None], kb * jnp.exp(g)[..., None]], axis=-1)
    sol = lax.linalg.triangular_solve(m, rhs, left_side=True, lower=True, unit_diagonal=True)
    u_val, w_dec = sol[..., :n], sol[..., n:]
    attn = jnp.einsum("bnhik,bnhjk->bnhij", q, k) * decay
    q_dec = q * jnp.exp(g)[..., None]
    k_tail = k * jnp.exp(g[..., -1:] - g)[..., None]
    g_tot = jnp.exp(g[..., -1])

    def step(s, inp):
        u_c, w_c, attn_c, qd_c, kt_c, gt_c = inp
        v_new = u_c - jnp.einsum("bhck,bhkv->bhcv", w_c, s)
        o = jnp.einsum("bhck,bhkv->bhcv", qd_c, s) + jnp.einsum("bhij,bhjv->bhiv", attn_c, v_new)
        s = s * gt_c[..., None, None] + jnp.einsum("bhck,bhcv->bhkv", kt_c, v_new)
        return s, o

    xs = tuple(jnp.moveaxis(t, 1, 0) for t in (u_val, w_dec, attn, q_dec, k_tail, g_tot))
    s_fin, o = lax.scan(step, s0, xs)
    o = jnp.moveaxis(jnp.moveaxis(o, 0, 1), 3, 2).reshape(b, l, h, n)
    return o, s_fin


def deltanet_mixer(qkv_c, qkv_l, gate_c, gate_l, al_c, al_l, be_c, be_l, conv_w, a_log, dt_bias, norm_g, want_ctx):
    f32 = jnp.float32

    def prep(qkv, al, be):
        b, l, _ = qkv.shape
        u = jax.nn.silu(centred_depthwise_conv(qkv, conv_w)).astype(f32).reshape(b, l, 3, DN_HEADS, DN_HD)
        q, k, v = l2_normalize(u[:, :, 0]), l2_normalize(u[:, :, 1]), u[:, :, 2]
        al = al.astype(f32).reshape(b, l, 2, DN_HEADS)
        be = be.astype(f32).reshape(b, l, 2, DN_HEADS)
        log_a = -jnp.exp(a_log) * jax.nn.softplus(al + dt_bias)
        return q, k, v, log_a, jax.nn.sigmoid(be)

    def dir_inputs(p, d):
        q, k, v, la, bt = p
        return tuple(_oriented(t, d) for t in (q, k, v, la[:, :, d], bt[:, :, d]))

    p_c, p_l = prep(qkv_c, al_c, be_c), prep(qkv_l, al_l, be_l)
    b = qkv_l.shape[0]
    o_c, o_l = 0.0, 0.0
    for d in range(2):
        s0 = jnp.zeros((b, DN_HEADS, DN_HD, DN_HD), f32)
        oc_d, s_ctx = gated_delta_chunked(*dir_inputs(p_c, d), s0)
        ol_d, _ = gated_delta_chunked(*dir_inputs(p_l, d), s_ctx)
        o_c = o_c + _oriented(oc_d, d)
        o_l = o_l + _oriented(ol_d, d)

    def post(o, gate):
        b_, l_ = o.shape[:2]
        on = o * lax.rsqrt(jnp.mean(o * o, axis=-1, keepdims=True) + EPS) * norm_g
        return (on.reshape(b_, l_, BRANCH_W) * jax.nn.silu(gate.astype(f32))).astype(gate.dtype)

    out_l = post(o_l, gate_l)
    out_c = post(o_c, gate_c) if want_ctx else None
    return out_c, out_l


def s5_scan(u, lam_bar, b_bar, c_mat, s0):
    bu = jnp.einsum("gph,blgh->blgp", b_bar, u.astype(jnp.complex64))
    bu = bu.at[:, 0].add(lam_bar * s0)
    a = jnp.broadcast_to(lam_bar, bu.shape)

    def combine(e1, e2):
        a1, b1 = e1
        a2, b2 = e2
        return a1 * a2, a2 * b1 + b2

    _, states = lax.associative_scan(combine, (a, bu), axis=1)
    y = jnp.real(jnp.einsum("ghp,blgp->blgh", c_mat, states))
    return y, states[:, -1]


def s5_mixer(u_c, u_l, rows, lam_re, lam_im, log_step, b_re, b_im, c_re, c_im, d_skip, w_glu, b_glu, want_ctx):
    f32 = jnp.float32

    def groups(u):
        return u.astype(f32).reshape(u.shape[0], u.shape[1], S5_GROUPS, S5_GW)

    uc = groups(u_c)
    ul = groups(raster_to_columns(u_l, rows))
    dg = d_skip.astype(f32).reshape(S5_GROUPS, S5_GW)
    y_c, y_l = uc * dg, ul * dg
    b = u_l.shape[0]
    for d in range(2):
        lam = lax.complex(lam_re[d].astype(f32), lam_im[d].astype(f32))
        lam_bar = jnp.exp(lam * jnp.exp(log_step[d].astype(f32))[:, None])
        b_bar = ((lam_bar - 1.0) / lam)[..., None] * lax.complex(b_re[d].astype(f32), b_im[d].astype(f32))
        c_mat = lax.complex(c_re[d].astype(f32), c_im[d].astype(f32))
        s0 = jnp.zeros((b, S5_GROUPS, S5_STATE), jnp.complex64)
        yc_d, s_ctx = s5_scan(_oriented(uc, d), lam_bar, b_bar, c_mat, s0)
        yl_d, _ = s5_scan(_oriented(ul, d), lam_bar, b_bar, c_mat, s_ctx)
        y_c = y_c + _oriented(yc_d, d)
        y_l = y_l + _oriented(yl_d, d)

    def glu(y):
        z = jax.nn.gelu(y.reshape(y.shape[0], y.shape[1], BRANCH_W))
        return z * jax.nn.sigmoid(z @ w_glu.astype(f32) + b_glu.astype(f32))

    out_l = columns_to_raster(glu(y_l), rows).astype(u_l.dtype)
    out_c = glu(y_c).astype(u_c.dtype) if want_ctx else None
    return out_c, out_l


def merge_branches(ys, gates, w_branch, w_out):
    g = gates.reshape(gates.shape[:-1] + (N_BRANCH, D_MODEL))
    m = 0.0
    for i, y in enumerate(ys):
        m = m + jax.nn.sigmoid(g[..., i, :]) * (y @ w_branch[i])
    return m @ w_out


def moe_ffn(h, router_w, router_b, w1, w3, w2):
    f32 = jnp.float32
    scores = jax.nn.sigmoid(h.astype(f32) @ router_w.astype(f32))
    sel = (scores + router_b.astype(f32)).reshape(scores.shape[:-1] + (N_EXPERT_GROUPS, EXPERTS_PER_GROUP))
    group_score = jnp.sum(lax.top_k(sel, TOP_K)[0], axis=-1)
    best = jnp.argmax(group_score, axis=-1)
    in_group = (best[..., None] == jnp.arange(N_EXPERT_GROUPS))[..., None]
    masked = jnp.where(in_group, sel, -jnp.inf).reshape(scores.shape)
    _, idx = lax.top_k(masked, TOP_K)
    w_sel = jnp.take_along_axis(scores, idx, axis=-1)
    w_sel = w_sel / jnp.sum(w_sel, axis=-1, keepdims=True)
    combine = jnp.sum(jax.nn.one_hot(idx, N_EXPERTS, dtype=f32) * w_sel[..., None], axis=-2)
    y = jnp.zeros(h.shape, f32)
    for e in range(N_EXPERTS):
        act = jax.nn.silu(h @ w1[e]) * (h @ w3[e])
        y = y + combine[..., e:e + 1] * (act @ w2[e])
    return y.astype(h.dtype)


def trunk_layer(x, xc, c, c_ctx, rows, want_ctx, router_w, router_b, lp):
    mod = adaln(c, lp["w_mod"], lp["b_mod"])
    mod_c = adaln(c_ctx, lp["w_mod"], lp["b_mod"])
    splits = _split_points(IN_SIZES)
    h = rms_norm(x, lp["norm1_g"]) * (1.0 + mod[1]) + mod[0]
    hc = rms_norm(xc, lp["norm1_g"]) * (1.0 + mod_c[1]) + mod_c[0]
    fn_l, rw_l, dqkv_l, dg_l, dal_l, dbe_l, s5_l, gate_l = jnp.split(h @ lp["w_in"], splits, axis=-1)
    fn_c, rw_c, dqkv_c, dg_c, dal_c, dbe_c, s5_c, gate_c = jnp.split(hc @ lp["w_in"], splits, axis=-1)

    ya_l = fourier_mix(fn_l)
    ya_c = fourier_mix(fn_c) if want_ctx else None
    yb_c, yb_l = rwkv_mixer(rw_c, rw_l, lp["rw_mu"], lp["rw_w0"], lp["rw_w_up"], lp["rw_a0"], lp["rw_a_up"],
                            lp["rw_k_k"], lp["rw_k_a"], lp["rw_r_k"], lp["rw_g_up"], lp["rw_ln_g"], lp["rw_ln_b"],
                            want_ctx)
    yc_c, yc_l = deltanet_mixer(dqkv_c, dqkv_l, dg_c, dg_l, dal_c, dal_l, dbe_c, dbe_l, lp["dn_conv"],
                                lp["dn_a_log"], lp["dn_dt_bias"], lp["dn_norm_g"], want_ctx)
    yd_c, yd_l = s5_mixer(s5_c, s5_l, rows, lp["s5_lam_re"], lp["s5_lam_im"], lp["s5_log_step"],
                          lp["s5_b_re"], lp["s5_b_im"], lp["s5_c_re"], lp["s5_c_im"], lp["s5_d"],
                          lp["s5_w_glu"], lp["s5_b_glu"], want_ctx)

    x = x + mod[2] * merge_branches((ya_l, yb_l, yc_l, yd_l), gate_l, lp["w_branch"], lp["w_out"])
    h2 = rms_norm(x, lp["norm2_g"]) * (1.0 + mod[4]) + mod[3]
    x = x + mod[5] * moe_ffn(h2, router_w, router_b, lp["moe_w1"], lp["moe_w3"], lp["moe_w2"])
    if want_ctx:
        xc = xc + mod_c[2] * merge_branches((ya_c, yb_c, yc_c, yd_c), gate_c, lp["w_branch"], lp["w_out"])
        h2c = rms_norm(xc, lp["norm2_g"]) * (1.0 + mod_c[4]) + mod_c[3]
        xc = xc + mod_c[5] * moe_ffn(h2c, router_w, router_b, lp["moe_w1"], lp["moe_w3"], lp["moe_w2"])
    return x, xc


def setup_inputs(seed: int = 0) -> dict:
    key = jax.random.key(seed)
    keys = iter(jax.random.split(key, 64))
    f32 = jnp.float32

    def nrm(shape, scale):
        return jax.random.normal(next(keys), shape, f32) * scale

    def uni(shape, lo, hi):
        return jax.random.uniform(next(keys), shape, f32, lo, hi)

    nl, d = DEPTH, D_MODEL
    dt = jnp.exp(uni((nl, 2, DN_HEADS), math.log(1e-3), math.log(1e-1)))
    n_idx = jnp.arange(S5_STATE, dtype=f32)
    return {
        "x": nrm((BATCH, SEQ, d), 1.0),
        "c": nrm((BATCH, d), 1.0),
        "ctx": nrm((BATCH, CTX_LEN, d), 1.0),
        "c_ctx": nrm((d,), 1.0),
        "w_mod": nrm((nl, d, N_MOD * d), 0.5 * d ** -0.5),
        "b_mod": nrm((nl, N_MOD * d), 0.02),
        "norm1_g": 1.0 + nrm((nl, d), 0.02),
        "norm2_g": 1.0 + nrm((nl, d), 0.02),
        "w_in": nrm((nl, d, N_IN), d ** -0.5),
        "rw_mu": uni((nl, 2, RW_COLS), 0.0, 0.5),
        "rw_w0": uni((nl, 2, BRANCH_W), -5.0, -0.5),
        "rw_w_up": nrm((nl, 2, RW_DECAY_RANK, BRANCH_W), 0.1),
        "rw_a0": nrm((nl, 2, BRANCH_W), 0.1),
        "rw_a_up": nrm((nl, 2, RW_ICL_RANK, BRANCH_W), 0.1),
        "rw_k_k": 0.85 + nrm((nl, BRANCH_W), 0.02),
        "rw_k_a": 1.0 + nrm((nl, BRANCH_W), 0.02),
        "rw_r_k": nrm((nl, RW_HEADS, RW_HD), 0.1),
        "rw_g_up": nrm((nl, RW_GATE_RANK, BRANCH_W), RW_GATE_RANK ** -0.5),
        "rw_ln_g": 1.0 + nrm((nl, BRANCH_W), 0.02),
        "rw_ln_b": nrm((nl, BRANCH_W), 0.02),
        "dn_conv": nrm((nl, DN_CONV, 3 * BRANCH_W), DN_CONV ** -0.5),
        "dn_a_log": jnp.log(uni((nl, 2, DN_HEADS), 1.0, 16.0)),
        "dn_dt_bias": dt + jnp.log(-jnp.expm1(-dt)),
        "dn_norm_g": 1.0 + nrm((nl, DN_HD), 0.02),
        "s5_lam_re": -0.5 + nrm((nl, 2, S5_GROUPS, S5_STATE), 0.01),
        "s5_lam_im": math.pi * n_idx + nrm((nl, 2, S5_GROUPS, S5_STATE), 0.01),
        "s5_log_step": uni((nl, 2, S5_GROUPS), math.log(1e-3), math.log(1e-1)),
        "s5_b_re": nrm((nl, 2, S5_GROUPS, S5_STATE, S5_GW), (2 * S5_GW) ** -0.5),
        "s5_b_im": nrm((nl, 2, S5_GROUPS, S5_STATE, S5_GW), (2 * S5_GW) ** -0.5),
        "s5_c_re": nrm((nl, 2, S5_GROUPS, S5_GW, S5_STATE), (2 * S5_STATE) ** -0.5),
        "s5_c_im": nrm((nl, 2, S5_GROUPS, S5_GW, S5_STATE), (2 * S5_STATE) ** -0.5),
        "s5_d": nrm((nl, BRANCH_W), 1.0),
        "s5_w_glu": nrm((nl, BRANCH_W, BRANCH_W), BRANCH_W ** -0.5),
        "s5_b_glu": nrm((nl, BRANCH_W), 0.02),
        "w_branch": nrm((nl, N_BRANCH, BRANCH_W, d), BRANCH_W ** -0.5),
        "w_out": nrm((nl, d, d), d ** -0.5),
        "router_w": nrm((d, N_EXPERTS), d ** -0.5),
        "router_b": nrm((N_EXPERTS,), 0.01),
        "moe_w1": nrm((nl, N_EXPERTS, d, D_EXPERT), d ** -0.5),
        "moe_w3": nrm((nl, N_EXPERTS, d, D_EXPERT), d ** -0.5),
        "moe_w2": nrm((nl, N_EXPERTS, D_EXPERT, d), D_EXPERT ** -0.5),
        "final_g": 1.0 + nrm((d,), 0.02),
    }


def reference(x, c, ctx, c_ctx, w_mod, b_mod, norm1_g, norm2_g, w_in, rw_mu, rw_w0, rw_w_up, rw_a0, rw_a_up,
              rw_k_k, rw_k_a, rw_r_k, rw_g_up, rw_ln_g, rw_ln_b, dn_conv, dn_a_log, dn_dt_bias, dn_norm_g,
              s5_lam_re, s5_lam_im, s5_log_step, s5_b_re, s5_b_im, s5_c_re, s5_c_im, s5_d, s5_w_glu, s5_b_glu,
              w_branch, w_out, router_w, router_b, moe_w1, moe_w3, moe_w2, final_g):
    rows = x.shape[1] // GRID_W
    xc = ctx
    for i in range(DEPTH):
        lp = {
            "w_mod": w_mod[i], "b_mod": b_mod[i], "norm1_g": norm1_g[i], "norm2_g": norm2_g[i],
            "w_in": w_in[i], "rw_mu": rw_mu[i], "rw_w0": rw_w0[i], "rw_w_up": rw_w_up[i],
            "rw_a0": rw_a0[i], "rw_a_up": rw_a_up[i], "rw_k_k": rw_k_k[i], "rw_k_a": rw_k_a[i],
            "rw_r_k": rw_r_k[i], "rw_g_up": rw_g_up[i], "rw_ln_g": rw_ln_g[i], "rw_ln_b": rw_ln_b[i],
            "dn_conv": dn_conv[i], "dn_a_log": dn_a_log[i], "dn_dt_bias": dn_dt_bias[i],
            "dn_norm_g": dn_norm_g[i], "s5_lam_re": s5_lam_re[i], "s5_lam_im": s5_lam_im[i],
            "s5_log_step": s5_log_step[i], "s5_b_re": s5_b_re[i], "s5_b_im": s5_b_im[i],
            "s5_c_re": s5_c_re[i], "s5_c_im": s5_c_im[i], "s5_d": s5_d[i], "s5_w_glu": s5_w_glu[i],
            "s5_b_glu": s5_b_glu[i], "w_branch": w_branch[i], "w_out": w_out[i],
            "moe_w1": moe_w1[i], "moe_w3": moe_w3[i], "moe_w2": moe_w2[i],
        }
        x, xc = trunk_layer(x, xc, c, c_ctx, rows, i < DEPTH - 1, router_w, router_b, lp)
    return rms_norm(x, final_g)
```

```python
import numpy as np
import concourse.bass as bass
import concourse.mybir as mybir

F32 = mybir.dt.float32
BF16 = mybir.dt.bfloat16
I32 = mybir.dt.int32
ALU = mybir.AluOpType
AF = mybir.ActivationFunctionType
AX = mybir.AxisListType

DMA_RING = 8
SB_BASE = 16640
SB_LIMIT = 228000


class Buf:
    def __init__(self, name, handle, ap):
        self.name = name
        self.h = handle
        self.ap = ap

    def __getitem__(self, idx):
        return self.ap[idx]


class Op:
    __slots__ = ("eng", "fn", "dma", "deps", "idx", "needs_inc", "count", "slot", "slot_use")


class Prog:
    ENGS = ("pe", "dve", "act", "pool", "sp")

    def __init__(self, nc):
        self.nc = nc
        self.ops = []
        self.trk = {}
        self.eng_ops = {e: [] for e in self.ENGS}
        self.dma_cnt = {e: 0 for e in self.ENGS}
        self.slot_last = {}
        self.slot_uses = {}
        self.last_compute = {}
        self.barrier_deps = []
        self.sb_off = SB_BASE
        self.sb_names = 0
        self.sb_hi = 0

    def sb_reset(self, off=None):
        self.sb_off = SB_BASE if off is None else off

    def sbuf(self, name, shape, dtype=F32):
        esz = mybir.dt.size(dtype) if hasattr(mybir.dt, "size") else {F32: 4, BF16: 2, I32: 4, mybir.dt.float32r: 4}[dtype]
        free = 1
        for s in shape[1:]:
            free *= s
        nbytes = free * esz
        nbytes = (nbytes + 63) // 64 * 64
        self.sb_names += 1
        nm = f"{name}_{self.sb_names}"
        h = self.nc.alloc_sbuf_tensor_at(nm, list(shape), dtype, offset=self.sb_off)
        self.sb_off += nbytes
        self.sb_hi = max(self.sb_hi, self.sb_off)
        assert self.sb_off <= SB_LIMIT, f"SBUF overflow {self.sb_off}"
        return Buf(nm, h, h[:] if len(shape) == 1 else h[tuple(slice(None) for _ in shape)])

    def dram(self, name, shape, dtype=F32, kind="Internal"):
        h = self.nc.dram_tensor(name, list(shape), dtype, kind=kind)
        return Buf(name, h, h.ap())

    def _deps(self, reads, writes):
        deps = {}

        def add(i, kind):
            if i is None:
                return
            if i in deps:
                if kind == "RAW":
                    deps[i] = "RAW"
            else:
                deps[i] = kind

        def norm(x):
            if isinstance(x, tuple):
                return x[0].name, x[1]
            return x.name, None

        for r in reads:
            nm, key = norm(r)
            t = self.trk.setdefault(nm, {"w": {}, "r": {}})
            if key is None:
                for i in t["w"].values():
                    add(i, "RAW")
            else:
                add(t["w"].get(key), "RAW")
                add(t["w"].get(None), "RAW")
        for w in writes:
            nm, key = norm(w)
            t = self.trk.setdefault(nm, {"w": {}, "r": {}})
            if key is None:
                for i in t["w"].values():
                    add(i, "WAW")
                for l in t["r"].values():
                    for i in l:
                        add(i, "WAR")
            else:
                add(t["w"].get(key), "WAW")
                add(t["w"].get(None), "WAW")
                for i in t["r"].get(key, ()):
                    add(i, "WAR")
                for i in t["r"].get(None, ()):
                    add(i, "WAR")
        return deps, norm

    def add(self, eng, fn, reads=(), writes=(), dma=False):
        op = Op()
        op.eng = eng
        op.fn = fn
        op.dma = dma
        op.idx = len(self.ops)
        op.needs_inc = False
        op.count = None
        deps, norm = self._deps(reads, writes)
        for r in reads:
            nm, key = norm(r)
            if nm.startswith("ps"):
                for lst in self.trk[nm]["r"].values():
                    for i in lst[-4:]:
                        if self.ops[i].eng != eng and i not in deps:
                            deps[i] = "RAW"
        for d in self.barrier_deps:
            if d not in deps:
                deps[d] = "RAW"
        op.deps = deps
        for r in reads:
            nm, key = norm(r)
            self.trk[nm]["r"].setdefault(key, []).append(op.idx)
        for w in writes:
            nm, key = norm(w)
            t = self.trk[nm]
            if key is None:
                t["w"] = {None: op.idx}
                t["r"] = {}
            else:
                t["w"][key] = op.idx
                t["r"][key] = []
        if dma:
            n = self.dma_cnt[eng]
            self.dma_cnt[eng] = n + 1
            slot = n % DMA_RING
            op.slot = slot
            prev = self.slot_last.get((eng, slot))
            if prev is not None and prev not in op.deps:
                op.deps[prev] = "RAW"
            self.slot_last[(eng, slot)] = op.idx
            u = self.slot_uses.get((eng, slot), 0) + 1
            self.slot_uses[(eng, slot)] = u
            op.slot_use = u
        else:
            self.last_compute[eng] = op.idx
        self.ops.append(op)
        self.eng_ops[eng].append(op)
        return op

    def barrier(self):
        deps = list(self.last_compute.values()) + list(self.slot_last.values())
        self.barrier_deps = deps

    def emit(self, stack):
        nc = self.nc
        ops = self.ops
        real = {}
        for op in ops:
            lst = []
            for d, kind in op.deps.items():
                src = ops[d]
                if not src.dma and not op.dma and src.eng == op.eng:
                    if op.eng == "pe":
                        continue
                    if kind != "RAW":
                        continue
                lst.append(d)
                if not src.dma:
                    src.needs_inc = True
            real[op.idx] = lst
        for e in self.ENGS:
            c = 0
            for op in self.eng_ops[e]:
                if not op.dma and op.needs_inc:
                    c += 1
                    op.count = c
        sem = {e: stack.enter_context(nc.semaphore(f"s_{e}")) for e in self.ENGS}
        dsem = {}
        for (e, slot) in self.slot_last:
            dsem[(e, slot)] = stack.enter_context(nc.semaphore(f"d_{e}_{slot}"))
        block = stack.enter_context(nc.Block())
        engobj = {"pe": block.tensor, "dve": block.vector, "act": block.scalar,
                  "pool": block.gpsimd, "sp": block.sync}
        nwaits = [0]

        def run_engine(e):
            def body(eng):
                known = {}
                for op in self.eng_ops[e]:
                    need = {}
                    for d in real[op.idx]:
                        src = ops[d]
                        if src.dma:
                            s = dsem[(src.eng, src.slot)]
                            v = 16 * src.slot_use
                        else:
                            s = sem[src.eng]
                            v = src.count
                        k = id(s)
                        if known.get(k, 0) >= v:
                            continue
                        if k not in need or need[k][1] < v:
                            need[k] = (s, v)
                    for k, (s, v) in need.items():
                        eng.wait_ge(s, v)
                        known[k] = v
                        nwaits[0] += 1
                    ins = op.fn(eng)
                    if op.dma:
                        ins.then_inc(dsem[(op.eng, op.slot)], 16)
                    elif op.needs_inc:
                        ins.then_inc(sem[e], 1)
                for (ee, slot), s in dsem.items():
                    if ee == e:
                        v = 16 * self.slot_uses[(ee, slot)]
                        if known.get(id(s), 0) < v:
                            eng.wait_ge(s, v)
            engobj[e](body)

        for e in self.ENGS:
            if self.eng_ops[e]:
                run_engine(e)
        self.nwaits = nwaits[0]

    def dma(self, out, in_, reads, writes, eng="sp", **kw):
        return self.add(eng, lambda e: e.dma_start(out=out, in_=in_, **kw), reads, writes, dma=True)

    def mm(self, out, lhsT, rhs, reads, writes, start=True, stop=True, **kw):
        return self.add("pe", lambda e: e.matmul(out, lhsT, rhs, start=start, stop=stop, **kw), reads, writes)

    def tr(self, out, in_, ident, reads, writes):
        return self.add("pe", lambda e: e.transpose(out, in_, ident), reads, writes)

    def act(self, out, in_, func, reads, writes, **kw):
        return self.add("act", lambda e: e.activation(out, in_, func, **kw), reads, writes)

    def ew(self, eng, name, reads, writes, *a, **kw):
        return self.add(eng, lambda e: getattr(e, name)(*a, **kw), reads, writes)


import numpy as np, math
from contextlib import ExitStack

T = 4352; NT = 34; D = 1024; KC = 8; CTX = 256; L = 4096
EPS = 1e-6
NFEAT = 2192
NL = 2

PER_LAYER = {
 "w_mod": [D, 6144], "b_mod": [6144], "norm1_g": [D], "norm2_g": [D], "w_in": [D, 6544],
 "rw_mu": [2, 896], "rw_w0": [2, 256], "rw_w_up": [2, 32, 256], "rw_a0": [2, 256], "rw_a_up": [2, 32, 256],
 "rw_k_k": [256], "rw_k_a": [256], "rw_r_k": [4, 64], "rw_g_up": [64, 256], "rw_ln_g": [256], "rw_ln_b": [256],
 "dn_conv": [5, 768], "dn_a_log": [2, 4], "dn_dt_bias": [2, 4], "dn_norm_g": [64],
 "s5_lam_re": [2, 16, 64], "s5_lam_im": [2, 16, 64], "s5_log_step": [2, 16],
 "s5_b_re": [2, 16, 64, 16], "s5_b_im": [2, 16, 64, 16], "s5_c_re": [2, 16, 16, 64], "s5_c_im": [2, 16, 16, 64],
 "s5_d": [256], "s5_w_glu": [256, 256], "s5_b_glu": [256], "w_branch": [4, 256, D], "w_out": [D, D],
 "moe_w1": [16, D, 256], "moe_w3": [16, D, 256], "moe_w2": [16, 256, D],
}
SHARED = {"c_ctx": [D], "router_w": [D, 16], "router_b": [16], "final_g": [D]}


class K:
    pass


def build(dbg=(), stop_after=None, nlayers=NL, yT_input=False, mixers=("fn", "s5", "dn", "rw")):
    nc = bass.Bass("TRN2", target_bir_lowering=False)
    P = Prog(nc)
    k = K(); k.P = P; k.nc = nc; k.dbg = set(dbg)
    I = {}
    I["x"] = P.dram("x", [L, D], F32, kind="ExternalInput")
    I["ctx"] = P.dram("ctx", [CTX, D], F32, kind="ExternalInput")
    I["c"] = P.dram("c", [D], F32, kind="ExternalInput")
    for n, s in SHARED.items():
        I[n] = P.dram(n, s, F32, kind="ExternalInput")
    for n, s in PER_LAYER.items():
        I[n] = P.dram(n, [NL] + s, F32, kind="ExternalInput")
    k.I = I
    k.out = P.dram("out", [L, D], F32, kind="ExternalOutput")

    def scratch(name, shape, dtype=F32):
        kind = "ExternalOutput" if name in k.dbg else "Internal"
        return P.dram(name, shape, dtype, kind=kind)
    k.scratch = scratch
    k.xres = scratch("xres", [T, D])
    k.mods = [scratch(f"mods{l}", [2, 6144]) for l in range(NL)]
    k.featT = scratch("featT", [NFEAT, T])
    k.projfn = scratch("projfn", [T, 256])
    k.yT = P.dram("yT", [1024, T], BF16, kind="ExternalInput") if yT_input else scratch("yT", [1024, T], BF16)
    k.yT_input = yT_input
    k.mixers = set(mixers)
    alloc_dplr_scratch(k)
    k.mTd = scratch("mTd", [1024, T], BF16)
    k.hTd = scratch("hTd", [1024, T], BF16)
    k.ps = []
    for i in range(8):
        h = nc.alloc_psum_tensor(f"ps{i}", [128, 512], F32)
        k.ps.append(Buf(f"ps{i}", h, h[:, :]))
    k.ones_b = P.sbuf("ones_b", [128, 128], BF16)
    k.ident_b = P.sbuf("ident_b", [128, 128], BF16)
    k.ones_r = P.sbuf("ones_r", [128, 128], mybir.dt.float32r)
    k.ones_f = P.sbuf("ones_f", [128, 128], F32)
    k.ident_f = P.sbuf("ident_f", [128, 128], F32)
    P.ew("pool", "memset", [], [k.ones_b], k.ones_b[:, :], 1.0)
    P.ew("pool", "memset", [], [k.ones_f], k.ones_f[:, :], 1.0)
    P.add("act", lambda e: e.activation(k.ones_r[:, :], k.ones_f[:, :], AF.Copy), [k.ones_f], [k.ones_r])
    P.add("pool", lambda e: e.affine_select(k.ident_b[:, :], k.ones_b[:, :], [[-1, 128]], ALU.is_equal, 0.0, base=0, channel_multiplier=1), [k.ones_b], [k.ident_b])
    P.add("pool", lambda e: e.affine_select(k.ident_f[:, :], k.ones_f[:, :], [[-1, 128]], ALU.is_equal, 0.0, base=0, channel_multiplier=1), [k.ones_f], [k.ident_f])
    gen_consts(k)
    k.const_mark = P.sb_off

    P.dma(k.xres[0:CTX, :], I["ctx"][:, :], [I["ctx"]], [k.xres])
    P.dma(k.xres[CTX:T, :], I["x"][:, :], [I["x"]], [k.xres], eng="pool")

    phase_mods(k)
    if stop_after == "mods":
        return finish(k)
    for l in range(nlayers):
        phase_norm(k, l, which=1)
        for kc in range(KC):
            P.dma(k.hTd[kc * 128:(kc + 1) * 128, :], k.hT[:, kc, :], [k.hT], [(k.hTd, kc)], eng=("sp" if kc % 2 else "pool"))
        phase_inproj(k, l)
        if stop_after == f"inproj{l}":
            return finish(k)
        if not k.yT_input:
            if "fn" in k.mixers:
                phase_fnet(k, l)
            if stop_after == f"fnet{l}":
                return finish(k)
            phase_mixers(k, l)
            if stop_after == f"mix{l}":
                return finish(k)
        phase_load_hT(k, l)
        phase_merge(k, l, final=(l == nlayers - 1))
        if stop_after == f"merge{l}":
            return finish(k)
        phase_norm(k, l, which=2)
        phase_moe(k, l, final=(l == nlayers - 1))
        if stop_after == f"moe{l}":
            return finish(k)
    return finish(k)


def finish(k):
    k.stack = ExitStack()
    k.P.emit(k.stack)
    return k


def phase_mods(k):
    P = k.P; I = k.I
    P.barrier(); P.sb_reset(k.const_mark)
    cT = P.sbuf("cT", [128, 8, 2], F32)
    P.dma(cT[:, :, 0], I["c"].ap.rearrange("(kc p) -> p kc", p=128), [I["c"]], [cT], allow_slow_non_contiguous=True)
    P.dma(cT[:, :, 1], I["c_ctx"].ap.rearrange("(kc p) -> p kc", p=128), [I["c_ctx"]], [cT], allow_slow_non_contiguous=True)
    cS = P.sbuf("cS", [128, 8, 2], F32)
    P.act(cS[:, :, :], cT[:, :, :], AF.Silu, [cT], [cS])
    wts = [P.sbuf(f"wm{i}", [128, 3072], F32) for i in range(2)]
    bm = P.sbuf("bm", [2, 6144], F32)
    msb = P.sbuf("msb", [2, 6144], F32)
    n = 0
    for l in range(NL):
        P.dma(bm[:, :], I["b_mod"].ap[l:l + 1, :].broadcast_to([2, 6144]) if False else I["b_mod"].ap[l, :].partition_broadcast(2), [I["b_mod"]], [bm])
        for half in range(2):
            for kc in range(8):
                wt = wts[n % 2]; n += 1
                P.dma(wt[:, :], I["w_mod"].ap[l, kc * 128:(kc + 1) * 128, half * 3072:(half + 1) * 3072], [I["w_mod"]], [wt], eng=("sp" if n % 2 else "pool"))
                for j in range(6):
                    P.mm(k.ps[j][0:2, :], cS[:, kc, :], wt[:, j * 512:(j + 1) * 512], [cS, wt], [k.ps[j]], start=(kc == 0), stop=(kc == 7))
            for j in range(6):
                o = half * 3072 + j * 512
                P.ew("dve", "tensor_tensor", [k.ps[j], bm], [(msb, o)], msb[:, o:o + 512], k.ps[j][0:2, :], bm[:, o:o + 512], ALU.add)
        P.dma(k.mods[l][:, :], msb[:, :], [msb], [k.mods[l]])


def load_mod_tiles(k, l, which):
    P = k.P; I = k.I
    gname = "norm1_g" if which == 1 else "norm2_g"
    so = 0 if which == 1 else 3072
    gb = P.sbuf("gb", [128, D], F32)
    P.dma(gb[:, :], I[gname].ap[l, :].partition_broadcast(128), [I[gname]], [gb])
    res = []
    for row in range(2):
        sc = P.sbuf(f"sc{row}", [128, D], F32)
        sh = P.sbuf(f"sh{row}", [128, D], F32)
        P.dma(sc[:, :], k.mods[l].ap[row, so + 1024:so + 2048].partition_broadcast(128), [k.mods[l]], [sc], eng="pool")
        P.dma(sh[:, :], k.mods[l].ap[row, so:so + 1024].partition_broadcast(128), [k.mods[l]], [sh])
        P.ew("dve", "scalar_tensor_tensor", [sc, gb], [sc], sc[:, :], sc[:, :], 1.0, gb[:, :], ALU.add, ALU.mult)
        res.append((sc, sh))
    return res


def phase_load_hT(k, l):
    P = k.P
    P.barrier(); P.sb_reset(k.const_mark)
    k.hT = P.sbuf("hT", [128, KC, T], BF16)
    k.norm_mark = P.sb_off
    for kc in range(KC):
        P.dma(k.hT[:, kc, :], k.hTd[kc * 128:(kc + 1) * 128, :], [k.hTd], [(k.hT, ("ld", kc))], eng=("sp" if kc % 2 else "pool"))


def phase_norm(k, l, which):
    P = k.P
    P.barrier(); P.sb_reset(k.const_mark)
    k.hT = P.sbuf("hT", [128, KC, T], BF16)
    if which == 2:
        k.comb = P.sbuf("comb", [128, NT, 16], F32)
    k.norm_mark = P.sb_off
    (A_l, sh_l), (A_c, sh_c) = load_mod_tiles(k, l, which)
    xts = [P.sbuf(f"xt{i}", [128, D], F32) for i in range(2)]
    hfs = [P.sbuf(f"hf{i}", [128, D], F32) for i in range(2)]
    hbs = [P.sbuf(f"hb{i}", [128, D], BF16) for i in range(2)]
    junk = P.sbuf("junk", [128, D], F32)
    ss = [P.sbuf(f"ss{i}", [128, 1], F32) for i in range(2)]
    rs = [P.sbuf(f"rs{i}", [128, 1], F32) for i in range(2)]
    epsb = P.sbuf("epsb", [128, 1], F32)
    P.ew("pool", "memset", [], [epsb], epsb[:, :], EPS)
    if which == 2:
        router_setup(k)
    for i in range(NT):
        xt = xts[i % 2]; hf = hfs[i % 2]; hb = hbs[i % 2]; s = ss[i % 2]; r = rs[i % 2]
        A, sh = (A_c, sh_c) if i < 2 else (A_l, sh_l)
        P.dma(xt[:, :], k.xres[i * 128:(i + 1) * 128, :], [(k.xres, i)], [xt], eng=("sp" if i % 2 else "pool"))
        P.add("act", lambda e, xt=xt, s=s: e.activation(junk[:, :], xt[:, :], AF.Square, accum_out=s[:, :]), [xt], [junk, s])
        P.add("act", lambda e, s=s, r=r: e.activation(r[:, :], s[:, :], AF.Sqrt, scale=1.0 / D, bias=epsb[:, :]), [s, epsb], [r])
        P.ew("dve", "reciprocal", [r], [r], r[:, :], r[:, :])
        P.ew("dve", "scalar_tensor_tensor", [xt, r, A], [hf], hf[:, :], xt[:, :], r[:, 0:1], A[:, :], ALU.mult, ALU.mult)
        P.ew("dve", "tensor_tensor", [hf, sh], [hf], hf[:, :], hf[:, :], sh[:, :], ALU.add)
        ACT(P, hb[:, :], hf[:, :], AF.Copy, [hf], [hb])
        if which == 2:
            router_tile(k, i, hf)
        pst = k.ps[i % 2]
        pv = pst.ap.bitcast(BF16)
        for kc in range(KC):
            P.tr(pv[:, kc * 128:(kc + 1) * 128], hb[:, kc * 128:(kc + 1) * 128], k.ident_b[:, :], [hb, k.ident_b], [pst])
        P.add("act", lambda e, pv=pv, i=i: e.activation(k.hT[:, :, i * 128:(i + 1) * 128], pv.rearrange("p (a b) -> p a b", a=KC), AF.Copy), [pst], [(k.hT, i)])
    if which == 2:
        router_batch(k)


FCH = [(256 + 128 * j, 128) for j in range(7)] + [(1152 + 128 * j, 128) for j in range(6)] + \
      [(1920, 128), (2048, 128), (2176, 16), (2192, 128), (2320, 128)]


def phase_inproj(k, l):
    P = k.P; I = k.I
    P.barrier(); P.sb_reset(k.norm_mark)
    wb = P.sbuf("wb", [128, KC, 2448], BF16)
    stg = [P.sbuf(f"wstg{i}", [128, 2448], F32) for i in range(2)]
    for kc in range(KC):
        st = stg[kc % 2]
        P.dma(st[:, :], I["w_in"].ap[l, kc * 128:(kc + 1) * 128, 0:2448], [I["w_in"]], [st], eng=("sp" if kc % 2 else "pool"))
        if kc % 2:
            P.ew("dve", "tensor_copy", [st], [(wb, kc)], wb[:, kc, :], st[:, :])
        else:
            P.add("act", lambda e, st=st, kc=kc: e.activation(wb[:, kc, :], st[:, :], AF.Copy), [st], [(wb, kc)])
    P.sb_reset(stg[0].sb_off if hasattr(stg[0], "sb_off") else P.sb_off)
    fo = [P.sbuf(f"fo{i}", [128, 256], F32) for i in range(2)]
    for i in range(NT):
        pst = k.ps[i % 2]
        for kc in range(KC):
            P.mm(pst[:, 0:256], k.hT[:, kc, i * 128:(i + 1) * 128], wb[:, kc, 0:256], [(k.hT, i), wb], [pst], start=(kc == 0), stop=(kc == KC - 1))
        f = fo[i % 2]
        P.add("act", lambda e, f=f, pst=pst: e.activation(f[:, :], pst[:, 0:256], AF.Copy), [pst], [f])
        P.dma(k.projfn[i * 128:(i + 1) * 128, :], f[:, :], [f], [(k.projfn, i)])
    so = [P.sbuf(f"so{i}", [128, T], F32) for i in range(2)]
    n = 0
    for ci, (c0, ncol) in enumerate(FCH):
        s = so[ci % 2]
        for b in range(9):
            t0 = b * 512; tn = min(512, T - t0)
            pst = k.ps[2 + (n % 4)]; n += 1
            for kc in range(KC):
                P.mm(pst[0:ncol, 0:tn], wb[:, kc, c0:c0 + ncol], k.hT[:, kc, t0:t0 + tn], [wb, k.hT], [pst], start=(kc == 0), stop=(kc == KC - 1))
            if n % 2:
                P.ew("dve", "tensor_copy", [pst], [(s, b)], s[0:ncol, t0:t0 + tn], pst[0:ncol, 0:tn])
            else:
                P.add("act", lambda e, s=s, pst=pst, ncol=ncol, t0=t0, tn=tn: e.activation(s[0:ncol, t0:t0 + tn], pst[0:ncol, 0:tn], AF.Copy), [pst], [(s, b)])
        P.dma(k.featT[c0 - 256:c0 - 256 + ncol, :], s[0:ncol, :], [s], [(k.featT, ci)], eng=("sp" if ci % 2 else "pool"))


def TT(P, eng, out, a, b, op, reads, writes):
    return P.add(eng, lambda e: e.tensor_tensor(out, a, b, op), reads, writes)


def TS(P, eng, out, a, s1, s2, op0, op1, reads, writes):
    if s2 is None:
        return P.add(eng, lambda e: e.tensor_scalar(out, a, s1, None, op0), reads, writes)
    return P.add(eng, lambda e: e.tensor_scalar(out, a, s1, s2, op0, op1), reads, writes)


def STT(P, eng, out, a, s, b, op0, op1, reads, writes):
    return P.add(eng, lambda e: e.scalar_tensor_tensor(out, a, s, b, op0, op1), reads, writes)


def ACT(P, out, in_, func, reads, writes, **kw):
    return P.add("act", lambda e: e.activation(out, in_, func, **kw), reads, writes)


def gen_consts(k):
    P = k.P
    k.jrow_i = P.sbuf("jrow_i", [128, 128], I32)
    P.add("pool", lambda e: e.iota(k.jrow_i[:, :], [[1, 128]], base=0, channel_multiplier=0), [], [k.jrow_i])
    ti = P.sbuf("ti", [128, 32], I32)
    k.lcols = P.sbuf("lcols", [128, 32], F32)
    P.add("pool", lambda e: e.iota(ti[:, 0:32], [[128, 32]], base=0, channel_multiplier=1), [], [ti])
    P.ew("dve", "tensor_copy", [ti], [k.lcols], k.lcols[:, :], ti[:, 0:32])
    k.negpi = P.sbuf("negpi", [128, 1], F32)
    P.ew("pool", "memset", [], [k.negpi], k.negpi[:, :], -math.pi)
    k.halfpi = P.sbuf("halfpi", [128, 1], F32)
    P.ew("pool", "memset", [], [k.halfpi], k.halfpi[:, :], math.pi / 2)
    pmi = P.sbuf("pmi", [128, 1], I32)
    P.add("pool", lambda e: e.iota(pmi[:, :], [[1, 1]], base=0, channel_multiplier=1), [], [pmi])
    TS(P, "dve", pmi[:, :], pmi[:, :], 63, None, ALU.bitwise_and, None, [pmi], [pmi])
    pm = P.sbuf("pm", [128, 1], F32)
    P.ew("dve", "tensor_copy", [pmi], [pm], pm[:, :], pmi[:, :])
    v = P.sbuf("v64", [128, 128], I32)
    TS(P, "dve", v[:, :], k.jrow_i[:, 0:128], 63, None, ALU.bitwise_and, None, [k.jrow_i], [v])
    TS(P, "dve", v[:, :], v[:, :], pm[:, 0:1], None, ALU.mult, None, [v, pm], [v])
    TS(P, "dve", v[:, :], v[:, :], 63, None, ALU.bitwise_and, None, [v], [v])
    w = P.sbuf("w64", [128, 128], I32)
    TS(P, "dve", w[:, :], v[:, :], 16, None, ALU.add, None, [v], [w])
    TS(P, "dve", w[:, :], w[:, :], 63, None, ALU.bitwise_and, None, [w], [w])
    bmk = P.sbuf("bmk", [128, 128], F32)
    P.ew("pool", "memset", [], [bmk], bmk[:, :], 0.0)
    P.ew("pool", "memset", [bmk], [bmk], bmk[0:64, 0:64], 1.0)
    P.ew("pool", "memset", [bmk], [bmk], bmk[64:128, 64:128], 1.0)
    k.blk64 = bmk
    sn = P.sbuf("sn64", [128, 128], F32)
    cn = P.sbuf("cn64", [128, 128], F32)
    ACT(P, sn[:, :], v[:, :], AF.Sin, [v, k.negpi], [sn], scale=2 * math.pi / 64, bias=k.negpi[:, :])
    ACT(P, cn[:, :], w[:, :], AF.Sin, [w, k.negpi], [cn], scale=2 * math.pi / 64, bias=k.negpi[:, :])
    k.Cn64 = P.sbuf("Cn64", [128, 128], BF16)
    k.S64 = P.sbuf("S64", [128, 128], BF16)
    TT(P, "dve", k.Cn64[:, :], cn[:, :], bmk[:, :], ALU.mult, [cn, bmk], [k.Cn64])
    STT(P, "dve", k.S64[:, :], sn[:, :], -1.0, bmk[:, :], ALU.mult, ALU.mult, [sn, bmk], [k.S64])
    k.blk64r = P.sbuf("blk64r", [128, 128], mybir.dt.float32r)
    P.add("act", lambda e: e.activation(k.blk64r[:, :], bmk[:, :], AF.Copy), [bmk], [k.blk64r])
    k.blk64b = P.sbuf("blk64b", [128, 128], BF16)
    P.ew("dve", "tensor_copy", [bmk], [k.blk64b], k.blk64b[:, :], bmk[:, :])


def phase_fnet(k, l):
    P = k.P
    P.barrier(); P.sb_reset(k.const_mark)
    jr = P.sbuf("jrow_big", [128, 4096], I32)
    P.add("pool", lambda e: e.iota(jr[:, :], [[1, 4096]], base=0, channel_multiplier=0), [], [jr])
    fmark = P.sb_off
    for (t0, Ls) in ((0, CTX), (CTX, L)):
        P.sb_reset(fmark)
        nt = Ls // 128
        KQ = min(1024, Ls)
        nb = KQ // 512 if KQ >= 512 else 1
        bw = min(512, KQ)
        uf = P.sbuf("uf", [128, nt, 256], F32)
        ub = P.sbuf("ub", [128, nt, 256], BF16)
        P.dma(uf[:, :, :], k.projfn.ap[t0:t0 + Ls, :].rearrange("(n p) c -> p n c", p=128), [k.projfn], [uf])
        P.ew("dve", "tensor_copy", [uf], [ub], ub[:, :, :], uf[:, :, :])
        Z = P.sbuf("Z", [128, 2, 2, Ls], BF16)
        vs = [P.sbuf(f"fv{i}", [128, KQ], I32) for i in range(2)]
        ws = [P.sbuf(f"fw{i}", [128, KQ], I32) for i in range(2)]
        sns = [P.sbuf(f"fsn{i}", [128, KQ], BF16) for i in range(2)]
        cns = [P.sbuf(f"fcn{i}", [128, KQ], BF16) for i in range(2)]
        n = 0
        for kq in range(Ls // KQ):
            for lc in range(nt):
                v = vs[n % 2]; w = ws[n % 2]; sn = sns[n % 2]; cn = cns[n % 2]; n += 1
                TS(P, "dve", v[:, :], jr[:, kq * KQ:(kq + 1) * KQ], k.lcols[:, lc:lc + 1], None, ALU.mult, None, [jr, k.lcols], [v])
                TS(P, "dve", v[:, :], v[:, :], Ls - 1, None, ALU.bitwise_and, None, [v], [v])
                TS(P, "dve", w[:, :], v[:, :], Ls // 4, None, ALU.add, None, [v], [w])
                TS(P, "dve", w[:, :], w[:, :], Ls - 1, None, ALU.bitwise_and, None, [w], [w])
                ACT(P, sn[:, :], v[:, :], AF.Sin, [v, k.negpi], [sn], scale=2 * math.pi / Ls, bias=k.negpi[:, :])
                ACT(P, cn[:, :], w[:, :], AF.Sin, [w, k.negpi], [cn], scale=2 * math.pi / Ls, bias=k.negpi[:, :])
                for h in range(2):
                    for ti_, trig in enumerate((cn, sn)):
                        for b in range(nb):
                            pst = k.ps[(h * 2 + ti_) * nb + b]
                            P.mm(pst[:, 0:bw], ub[:, lc, h * 128:(h + 1) * 128], trig[:, b * bw:(b + 1) * bw], [ub, trig], [pst], start=(lc == 0), stop=(lc == nt - 1))
            for h in range(2):
                for ti_ in range(2):
                    for b in range(nb):
                        pst = k.ps[(h * 2 + ti_) * nb + b]
                        o = kq * KQ + b * bw
                        if (h + ti_ + b) % 2:
                            P.ew("dve", "tensor_copy", [pst], [(Z, (h, ti_, o))], Z[:, h, ti_, o:o + bw], pst[:, 0:bw])
                        else:
                            ACT(P, Z[:, h, ti_, o:o + bw], pst[:, 0:bw], AF.Copy, [pst], [(Z, (h, ti_, o))])
        yst = P.sbuf("yst", [128, 2, Ls], BF16)
        scale = 1.0 / math.sqrt(Ls * 64.0)
        n = 0
        for h in range(2):
            for b in range(max(1, Ls // 512)):
                o = b * 512; bw2 = min(512, Ls)
                pst = k.ps[n % 8]; n += 1
                P.mm(pst[:, 0:bw2], k.Cn64[:, :], Z[:, h, 0, o:o + bw2], [k.Cn64, Z], [pst], start=True, stop=False)
                P.mm(pst[:, 0:bw2], k.S64[:, :], Z[:, h, 1, o:o + bw2], [k.S64, Z], [pst], start=False, stop=True)
                ACT(P, yst[:, h, o:o + bw2], pst[:, 0:bw2], AF.Copy, [pst], [(yst, (h, b))], scale=scale)
        for h in range(2):
            P.dma(k.yT[h * 128:(h + 1) * 128, t0:t0 + Ls], yst[:, h, :], [yst], [(k.yT, ("fn", h, t0))])


def router_setup(k):
    P = k.P; I = k.I
    k.rw = P.sbuf("rw", [128, KC, 16], F32)
    P.dma(k.rw[:, :, :], I["router_w"].ap.rearrange("(kc p) e -> p kc e", p=128), [I["router_w"]], [k.rw])
    k.rb = P.sbuf("rb", [128, 16], F32)
    P.dma(k.rb[:, :], I["router_b"].ap.partition_broadcast(128), [I["router_b"]], [k.rb])
    k.hTf = [P.sbuf(f"hTf{i}", [128, KC, 128], F32) for i in range(2)]
    k.sc_all = P.sbuf("sc_all", [128, NT, 16], F32)


def router_tile(k, i, hf):
    P = k.P
    hTf = k.hTf[i % 2]
    for half in range(2):
        pst = k.ps[2 + half]
        for q in range(4):
            kc = half * 4 + q
            P.tr(pst[:, q * 128:(q + 1) * 128], hf[:, kc * 128:(kc + 1) * 128], k.ident_f[:, :], [hf, k.ident_f], [pst])
        ACT(P, hTf[:, half * 4:(half + 1) * 4, :], pst.ap.rearrange("p (a b) -> p a b", a=4), AF.Copy, [pst], [(hTf, half)])
    pl = k.ps[4 + (i % 2)]
    for kc in range(KC):
        P.mm(pl[:, 0:16], hTf[:, kc, :], k.rw[:, kc, :], [hTf, k.rw], [pl], start=(kc == 0), stop=(kc == KC - 1))
    ACT(P, k.sc_all[:, i, :], pl[:, 0:16], AF.Sigmoid, [pl], [(k.sc_all, i)])


def router_batch(k):
    P = k.P
    BIG = 1.0e4
    N = NT
    def tl(name, w):
        return P.sbuf("rb_" + name, [128, N * w], F32)
    sc = k.sc_all
    sel = tl("sel", 16); eq = tl("eq", 16); s2 = tl("s2", 16); msk = tl("msk", 16); m2t = tl("m2t", 16); e1 = tl("e1", 16); e2 = tl("e2", 16); ws = tl("ws", 16)
    m1 = tl("m1", 4); m2 = tl("m2", 4); gs = tl("gs", 4); ing = tl("ing", 4)
    gm = tl("gm", 1); t1 = tl("t1", 1); t2 = tl("t2", 1); nrm = tl("nrm", 1)
    scf = sc.ap.rearrange("p n e -> p (n e)")
    v_ne = lambda t: t.ap.rearrange("p (n e) -> p n e", e=16)
    v_ge = lambda t: t.ap.rearrange("p (g e) -> p g e", e=4)
    v_ng = lambda t: t.ap.rearrange("p (n g) -> p n g", g=4)
    TT(P, "dve", v_ne(sel), sc.ap, k.rb.ap.unsqueeze(1).broadcast_to([128, N, 16]), ALU.add, [sc, k.rb], [sel])
    P.add("dve", lambda e: e.tensor_reduce(m1.ap, v_ge(sel), AX.X, ALU.max), [sel], [m1])
    TT(P, "dve", v_ge(eq), v_ge(sel), m1.ap.unsqueeze(2).broadcast_to([128, N * 4, 4]), ALU.is_equal, [sel, m1], [eq])
    STT(P, "dve", s2.ap, eq.ap, -BIG, sel.ap, ALU.mult, ALU.add, [eq, sel], [s2])
    P.add("dve", lambda e: e.tensor_reduce(m2.ap, v_ge(s2), AX.X, ALU.max), [s2], [m2])
    TT(P, "dve", gs.ap, m1.ap, m2.ap, ALU.add, [m1, m2], [gs])
    P.add("dve", lambda e: e.tensor_reduce(gm.ap, v_ng(gs), AX.X, ALU.max), [gs], [gm])
    TT(P, "dve", v_ng(ing), v_ng(gs), gm.ap.unsqueeze(2).broadcast_to([128, N, 4]), ALU.is_equal, [gs, gm], [ing])
    TS(P, "dve", ing.ap, ing.ap, -1.0, BIG, ALU.add, ALU.mult, [ing], [ing])
    TT(P, "dve", v_ge(msk), v_ge(sel), ing.ap.unsqueeze(2).broadcast_to([128, N * 4, 4]), ALU.add, [sel, ing], [msk])
    P.add("dve", lambda e: e.tensor_reduce(t1.ap, v_ne(msk), AX.X, ALU.max), [msk], [t1])
    TT(P, "dve", v_ne(e1), v_ne(msk), t1.ap.unsqueeze(2).broadcast_to([128, N, 16]), ALU.is_equal, [msk, t1], [e1])
    STT(P, "dve", m2t.ap, e1.ap, -BIG, msk.ap, ALU.mult, ALU.add, [e1, msk], [m2t])
    P.add("dve", lambda e: e.tensor_reduce(t2.ap, v_ne(m2t), AX.X, ALU.max), [m2t], [t2])
    TT(P, "dve", v_ne(e2), v_ne(m2t), t2.ap.unsqueeze(2).broadcast_to([128, N, 16]), ALU.is_equal, [m2t, t2], [e2])
    TT(P, "dve", e1.ap, e1.ap, e2.ap, ALU.add, [e1, e2], [e1])
    TT(P, "dve", ws.ap, e1.ap, scf, ALU.mult, [e1, sc], [ws])
    P.add("dve", lambda e: e.tensor_reduce(nrm.ap, v_ne(ws), AX.X, ALU.add), [ws], [nrm])
    P.ew("dve", "reciprocal", [nrm], [nrm], nrm.ap, nrm.ap)
    TT(P, "dve", k.comb.ap, v_ne(ws), nrm.ap.unsqueeze(2).broadcast_to([128, N, 16]), ALU.mult, [ws, nrm], [k.comb])


def phase_merge(k, l, final=False):
    P = k.P; I = k.I
    P.barrier(); P.sb_reset(k.norm_mark)
    mTd = k.mTd
    wbr = P.sbuf("wbr", [128, 4, 2, D], BF16)
    wbfs = [P.sbuf(f"wbf{i}", [128, 2, D], F32) for i in range(2)]
    for i in range(4):
        P.dma(wbfs[i % 2][:, :, :], I["w_branch"].ap[l, i].rearrange("(c p) n -> p c n", p=128), [I["w_branch"]], [wbfs[i % 2]])
        P.ew("pool", "tensor_copy", [wbfs[i % 2]], [(wbr, i)], wbr[:, i, :, :], wbfs[i % 2][:, :, :])
    mark = k.norm_mark
    mst = [P.sbuf(f"mst{i}", [128, 512], BF16) for i in range(2)]
    wgf = [P.sbuf(f"wgf{i}", [128, KC, 4, 128], F32) for i in range(1)]
    wgb = [P.sbuf(f"wgb{i}", [128, KC, 4, 128], BF16) for i in range(2)]
    yb = [P.sbuf(f"yb{i}", [128, 8, 512], BF16) for i in range(2)]
    sg = [P.sbuf(f"sg{i}", [128, 512], F32) for i in range(2)]
    tmp = [P.sbuf(f"mtmp{i}", [128, 512], F32) for i in range(2)]
    acc = [P.sbuf(f"macc{i}", [128, 512], F32) for i in range(2)]
    n = 0; nb = 0
    for oc in range(8):
        wf = wgf[0]; wg = wgb[oc % 2]
        src = I["w_in"].ap[l, :, 2448:6544].rearrange("(kc p) (i n) -> p kc i n", p=128, i=4)[:, :, :, oc * 128:(oc + 1) * 128]
        for kc in range(KC):
            P.dma(wf[:, kc, :, :], src[:, kc, :, :], [I["w_in"]], [(wf, kc)], eng=("sp" if kc % 2 else "pool"))
        ACT(P, wg[:, :, :, :], wf[:, :, :, :], AF.Copy, [wf], [wg])
        for tb in range(9):
            t0 = tb * 512; tn = min(512, T - t0)
            y = yb[nb % 2]; a = acc[nb % 2]; nb += 1
            P.dma(y[:, :, 0:tn], k.yT.ap[:, t0:t0 + tn].rearrange("(j p) t -> p j t", p=128), [k.yT], [y])
            for i in range(4):
                pm = k.ps[(n % 2) * 2]; pg = k.ps[(n % 2) * 2 + 1]; s_ = sg[n % 2]; tm = tmp[n % 2]; n += 1
                for c2 in range(2):
                    P.mm(pm[:, 0:tn], wbr[:, i, c2, oc * 128:(oc + 1) * 128], y[:, i * 2 + c2, 0:tn], [wbr, y], [pm], start=(c2 == 0), stop=(c2 == 1))
                for kc in range(KC):
                    P.mm(pg[:, 0:tn], wg[:, kc, i, :], k.hT[:, kc, t0:t0 + tn], [wg, k.hT], [pg], start=(kc == 0), stop=(kc == KC - 1))
                ACT(P, s_[:, 0:tn], pg[:, 0:tn], AF.Sigmoid, [pg], [s_])
                if i == 0:
                    TT(P, "dve", a[:, 0:tn], s_[:, 0:tn], pm[:, 0:tn], ALU.mult, [s_, pm], [a])
                else:
                    TT(P, "dve", tm[:, 0:tn], s_[:, 0:tn], pm[:, 0:tn], ALU.mult, [s_, pm], [tm])
                    TT(P, "dve", a[:, 0:tn], a[:, 0:tn], tm[:, 0:tn], ALU.add, [a, tm], [a])
            ms = mst[nb % 2]
            ACT(P, ms[:, 0:tn], a[:, 0:tn], AF.Copy, [a], [ms])
            P.dma(mTd[oc * 128:(oc + 1) * 128, t0:t0 + tn], ms[:, 0:tn], [ms], [(mTd, (oc, tb))])
    P.barrier(); P.sb_reset(mark)
    wof = P.sbuf("wof", [128, KC, D], F32)
    wo = P.sbuf("wo", [128, KC, D], BF16)
    P.dma(wof[:, :, :], I["w_out"].ap[l].rearrange("(kc p) n -> p kc n", p=128), [I["w_out"]], [wof])
    P.ew("pool", "tensor_copy", [wof], [wo], wo[:, :, :], wof[:, :, :])
    P.sb_reset(wof_off(P, wof))
    g1 = []
    for row in range(2):
        g = P.sbuf(f"g1_{row}", [128, D], F32)
        P.dma(g[:, :], k.mods[l].ap[row, 2048:3072].partition_broadcast(128), [k.mods[l]], [g])
        g1.append(g)
    xts = [P.sbuf(f"mx{i}", [128, D], F32) for i in range(2)]
    tms = [P.sbuf(f"mt{i}", [128, D], F32) for i in range(2)]
    mTs = [P.sbuf(f"mTs{i}", [128, KC, 128], BF16) for i in range(2)]
    for i in range(2 if final else 0, NT):
        xt = xts[i % 2]; tm = tms[i % 2]; g = g1[1] if i < 2 else g1[0]
        mT = mTs[i % 2]
        P.dma(mT[:, :, :], mTd.ap[:, i * 128:(i + 1) * 128].rearrange("(kc p) t -> p kc t", p=128), [mTd], [mT])
        P.dma(xt[:, :], k.xres[i * 128:(i + 1) * 128, :], [(k.xres, i)], [xt])
        for nh in range(2):
            pst = k.ps[(i % 2) * 2 + nh]
            for kc in range(KC):
                P.mm(pst[:, :], mT[:, kc, :], wo[:, kc, nh * 512:(nh + 1) * 512], [mT, wo], [pst], start=(kc == 0), stop=(kc == KC - 1))
            TT(P, "dve", tm[:, nh * 512:(nh + 1) * 512], pst[:, :], g[:, nh * 512:(nh + 1) * 512], ALU.mult, [pst, g], [(tm, nh)])
        TT(P, "dve", xt[:, :], xt[:, :], tm[:, :], ALU.add, [xt, tm], [xt])
        P.dma(k.xres[i * 128:(i + 1) * 128, :], xt[:, :], [xt], [(k.xres, i)], eng="pool")


def wof_off(P, buf):
    return P.sb_off


def phase_moe(k, l, final):
    P = k.P; I = k.I
    P.barrier(); P.sb_reset(k.norm_mark)
    g2 = []
    for row in range(2):
        g = P.sbuf(f"g2_{row}", [128, D], F32)
        P.dma(g[:, :], k.mods[l].ap[row, 5120:6144].partition_broadcast(128), [k.mods[l]], [g])
        g2.append(g)
    if final:
        fg = P.sbuf("fg", [128, D], F32)
        P.dma(fg[:, :], I["final_g"].ap.partition_broadcast(128), [I["final_g"]], [fg])
        epsb = P.sbuf("epsb2", [128, 1], F32)
        P.ew("pool", "memset", [], [epsb], epsb[:, :], EPS)
    yacc = P.sbuf("yacc", [128, 7, D], F32)
    w13f = [P.sbuf(f"w13f{i}", [128, 2, KC, 256], F32) for i in range(1)]
    w2f = [P.sbuf(f"w2f{i}", [128, 2, D], F32) for i in range(1)]
    w13 = [P.sbuf(f"w13b{i}", [128, 2, KC, 256], BF16) for i in range(2)]
    w2 = [P.sbuf(f"w2b{i}", [128, 2, D], BF16) for i in range(2)]
    xts = [P.sbuf(f"ox{i}", [128, D], F32) for i in range(2)]
    tms = [P.sbuf(f"ot{i}", [128, D], F32) for i in range(2)]
    junk = P.sbuf("ojunk", [128, D], F32)
    ss = [P.sbuf(f"oss{i}", [128, 1], F32) for i in range(2)]
    sbs = [(2, 7), (9, 7), (16, 6), (22, 6), (28, 6)] if final else [(0, 7), (7, 7), (14, 7), (21, 7), (28, 6)]
    a1 = [P.sbuf(f"a1w_{i}", [128, 2, 512], F32) for i in range(2)]
    ab = [P.sbuf(f"abw_{i}", [128, 2, 512], BF16) for i in range(2)]
    ne = 0; nw = 0; orr = 0
    for (ts0, nts) in sbs:
        pending = None

        def emit_out(item):
            nonlocal orr
            e_, w2b_, ab_, j_, nt_ = item
            for tt_ in range(nt_):
                til = j_ + tt_
                gi = ts0 + til
                for nh in range(2):
                    po = k.ps[4 + (orr % 4)]; orr += 1
                    for dh in range(2):
                        P.mm(po[:, :], ab_[:, dh, tt_ * 128:(tt_ + 1) * 128], w2b_[:, dh, nh * 512:(nh + 1) * 512], [ab_, w2b_], [po], start=(dh == 0), stop=(dh == 1))
                    dst = yacc[:, til, nh * 512:(nh + 1) * 512]
                    if e_ == 0:
                        TS(P, "dve", dst, po[:, :], k.comb[:, gi, e_:e_ + 1], None, ALU.mult, None, [po, (k.comb, gi)], [(yacc, (til, nh))])
                    else:
                        STT(P, "dve", dst, po[:, :], k.comb[:, gi, e_:e_ + 1], dst, ALU.mult, ALU.add, [po, (k.comb, gi), (yacc, (til, nh))], [(yacc, (til, nh))])
        for e in range(16):
            wf = w13f[0]; w2f_ = w2f[0]; wb = w13[ne % 2]; w2b = w2[ne % 2]; ne += 1
            P.dma(wf[:, 0, :, :], I["moe_w1"].ap[l, e].rearrange("(kc p) n -> p kc n", p=128), [I["moe_w1"]], [(wf, 0)])
            P.dma(wf[:, 1, :, :], I["moe_w3"].ap[l, e].rearrange("(kc p) n -> p kc n", p=128), [I["moe_w3"]], [(wf, 1)], eng="pool")
            P.dma(w2f_[:, :, :], I["moe_w2"].ap[l, e].rearrange("(c p) n -> p c n", p=128), [I["moe_w2"]], [w2f_])
            ACT(P, wb[:, 0, :, :], wf[:, 0, :, :], AF.Copy, [(wf, 0)], [(wb, 0)])
            P.ew("pool", "tensor_copy", [(wf, 1)], [(wb, 1)], wb[:, 1, :, :], wf[:, 1, :, :])
            P.ew("dve", "tensor_copy", [w2f_], [w2b], w2b[:, :, :], w2f_[:, :, :])
            j = 0
            while j < nts:
                nt4 = min(4, nts - j)
                tok0 = (ts0 + j) * 128; ntok = nt4 * 128
                a1_ = a1[nw % 2]; ab_ = ab[nw % 2]; nw += 1
                pbanks = [[k.ps[0], k.ps[1]], [k.ps[2], k.ps[3]]]
                for which in range(2):
                    for dh in range(2):
                        pp = pbanks[which][dh]
                        for kc in range(KC):
                            P.mm(pp[:, 0:ntok], wb[:, which, kc, dh * 128:(dh + 1) * 128], k.hT[:, kc, tok0:tok0 + ntok], [wb, k.hT], [pp], start=(kc == 0), stop=(kc == KC - 1))
                for dh in range(2):
                    ACT(P, a1_[:, dh, 0:ntok], pbanks[0][dh][:, 0:ntok], AF.Silu, [pbanks[0][dh]], [(a1_, dh)])
                    TT(P, "dve", ab_[:, dh, 0:ntok], a1_[:, dh, 0:ntok], pbanks[1][dh][:, 0:ntok], ALU.mult, [(a1_, dh), pbanks[1][dh]], [(ab_, dh)])
                if pending is not None:
                    emit_out(pending)
                pending = (e, w2b, ab_, j, nt4)
                j += nt4
        if pending is not None:
            emit_out(pending)
            pending = None
        for til in range(nts):
            gi = ts0 + til
            xt = xts[gi % 2]; tm = tms[gi % 2]; g = g2[1] if gi < 2 else g2[0]
            if final and gi < 2:
                continue
            P.dma(xt[:, :], k.xres[gi * 128:(gi + 1) * 128, :], [(k.xres, gi)], [xt])
            TT(P, "pool", tm[:, :], yacc[:, til, :], g[:, :], ALU.mult, [yacc, g], [tm])
            TT(P, "pool", xt[:, :], xt[:, :], tm[:, :], ALU.add, [xt, tm], [xt])
            if not final:
                P.dma(k.xres[gi * 128:(gi + 1) * 128, :], xt[:, :], [xt], [(k.xres, gi)], eng="pool")
            else:
                s = ss[gi % 2]
                P.add("act", lambda e_, xt=xt, s=s: e_.activation(junk[:, :], xt[:, :], AF.Square, accum_out=s[:, :]), [xt], [junk, s])
                P.add("act", lambda e_, s=s: e_.activation(s[:, :], s[:, :], AF.Sqrt, scale=1.0 / D, bias=epsb[:, :]), [s, epsb], [s])
                P.ew("dve", "reciprocal", [s], [s], s[:, :], s[:, :])
                STT(P, "dve", tm[:, :], xt[:, :], s[:, 0:1], fg[:, :], ALU.mult, ALU.mult, [xt, s, fg], [tm])
                P.dma(k.out[(gi - 2) * 128:(gi - 1) * 128, :], tm[:, :], [tm], [(k.out, gi)], eng="pool")


def phase_mixers(k, l):
    phase_zero_missing(k, l)
    if "s5" in k.mixers:
        phase_s5(k, l)
    if "dn" in k.mixers and "rw" in k.mixers:
        phase_rw_dn(k, l)
    elif "dn" in k.mixers:
        phase_dn(k, l)
    elif "rw" in k.mixers:
        phase_rw(k, l)


def phase_zero_missing(k, l):
    P = k.P
    P.barrier(); P.sb_reset(k.const_mark)
    z = P.sbuf("zfill", [128, T], BF16)
    P.ew("pool", "memset", [], [z], z[:, :], 0.0)
    for j in range(2, 6):
        P.dma(k.yT[j * 128:(j + 1) * 128, :], z[:, :], [z], [(k.yT, ("z", j))])


def phase_s5(k, l):
    P = k.P; I = k.I
    P.barrier(); P.sb_reset(k.const_mark)
    TC = 256
    NCH = T // TC
    S5ROW = 1936
    E2 = P.sbuf("E2", [128, 16, 2, TC], F32); E3 = P.sbuf("E3", [128, 16, 2, TC], F32)
    Rt = P.sbuf("Rt", [128, 16, TC], F32)
    Bb = P.sbuf("Bb", [128, 2, 2, 8, 128], BF16)
    Cb = P.sbuf("Cb", [128, 2, 2, 8, 128], BF16)
    s5mark = P.sb_off
    def ptile(name, shape=(128, 2, 8)):
        return P.sbuf(name, list(shape), F32)
    lre = ptile("lre"); lim = ptile("lim"); dt = ptile("dt")
    for d in range(2):
        P.dma(lre[:, d, :], I["s5_lam_re"].ap[l, d].rearrange("(b g) n -> (g n) b", g=2), [I["s5_lam_re"]], [lre], allow_slow_non_contiguous=True)
        P.dma(lim[:, d, :], I["s5_lam_im"].ap[l, d].rearrange("(b g) n -> (g n) b", g=2), [I["s5_lam_im"]], [lim], allow_slow_non_contiguous=True)
        for gl in range(2):
            P.dma(dt[gl * 64:(gl + 1) * 64, d, :], I["s5_log_step"].ap[l, d].rearrange("(b g) -> g b", g=2)[gl].partition_broadcast(64), [I["s5_log_step"]], [dt], allow_slow_non_contiguous=True)
    ACT(P, dt[:, :, :], dt[:, :, :], AF.Exp, [dt], [dt])
    are = ptile("are"); th = ptile("th"); rr = ptile("rr")
    TT(P, "dve", are[:, :, :], lre[:, :, :], dt[:, :, :], ALU.mult, [lre, dt], [are])
    TT(P, "dve", th[:, :, :], lim[:, :, :], dt[:, :, :], ALU.mult, [lim, dt], [th])
    ACT(P, rr[:, :, :], are[:, :, :], AF.Exp, [are], [rr])
    q = ptile("q"); qi = P.sbuf("qi", [128, 2, 8], I32); thm = ptile("thm"); thc = ptile("thc")
    TS(P, "dve", q[:, :, :], th[:, :, :], 1.0 / (2 * math.pi), None, ALU.mult, None, [th], [q])
    P.ew("dve", "tensor_copy", [q], [qi], qi[:, :, :], q[:, :, :])
    P.ew("dve", "tensor_copy", [qi], [q], q[:, :, :], qi[:, :, :])
    STT(P, "dve", thm[:, :, :], q[:, :, :], -2 * math.pi, th[:, :, :], ALU.mult, ALU.add, [q, th], [thm])
    gt = ptile("gt")
    TS(P, "dve", gt[:, :, :], thm[:, :, :], math.pi, -2 * math.pi, ALU.is_gt, ALU.mult, [thm], [gt])
    TT(P, "dve", thm[:, :, :], thm[:, :, :], gt[:, :, :], ALU.add, [thm, gt], [thm])
    TS(P, "dve", gt[:, :, :], thm[:, :, :], -math.pi, 2 * math.pi, ALU.is_lt, ALU.mult, [thm], [gt])
    TT(P, "dve", thm[:, :, :], thm[:, :, :], gt[:, :, :], ALU.add, [thm, gt], [thm])
    TS(P, "dve", thc[:, :, :], thm[:, :, :], math.pi / 2, None, ALU.add, None, [thm], [thc])
    TS(P, "dve", gt[:, :, :], thc[:, :, :], math.pi, -2 * math.pi, ALU.is_gt, ALU.mult, [thc], [gt])
    TT(P, "dve", thc[:, :, :], thc[:, :, :], gt[:, :, :], ALU.add, [thc, gt], [thc])
    sn1 = ptile("sn1"); cs1 = ptile("cs1")
    ACT(P, sn1[:, :, :], thm[:, :, :], AF.Sin, [thm], [sn1])
    ACT(P, cs1[:, :, :], thc[:, :, :], AF.Sin, [thc], [cs1])
    nre = ptile("nre"); nim = ptile("nim"); den = ptile("den"); kre = ptile("kre"); kim = ptile("kim"); t1 = ptile("t1"); t2 = ptile("t2")
    TT(P, "dve", nre[:, :, :], rr[:, :, :], cs1[:, :, :], ALU.mult, [rr, cs1], [nre])
    TS(P, "dve", nre[:, :, :], nre[:, :, :], -1.0, None, ALU.add, None, [nre], [nre])
    TT(P, "dve", nim[:, :, :], rr[:, :, :], sn1[:, :, :], ALU.mult, [rr, sn1], [nim])
    TT(P, "dve", t1[:, :, :], lre[:, :, :], lre[:, :, :], ALU.mult, [lre], [t1])
    TT(P, "dve", t2[:, :, :], lim[:, :, :], lim[:, :, :], ALU.mult, [lim], [t2])
    TT(P, "dve", den[:, :, :], t1[:, :, :], t2[:, :, :], ALU.add, [t1, t2], [den])
    P.ew("dve", "reciprocal", [den], [den], den[:, :, :], den[:, :, :])
    TT(P, "dve", t1[:, :, :], nre[:, :, :], lre[:, :, :], ALU.mult, [nre, lre], [t1])
    TT(P, "dve", t2[:, :, :], nim[:, :, :], lim[:, :, :], ALU.mult, [nim, lim], [t2])
    TT(P, "dve", kre[:, :, :], t1[:, :, :], t2[:, :, :], ALU.add, [t1, t2], [kre])
    TT(P, "dve", kre[:, :, :], kre[:, :, :], den[:, :, :], ALU.mult, [kre, den], [kre])
    TT(P, "dve", t1[:, :, :], nim[:, :, :], lre[:, :, :], ALU.mult, [nim, lre], [t1])
    TT(P, "dve", t2[:, :, :], nre[:, :, :], lim[:, :, :], ALU.mult, [nre, lim], [t2])
    TT(P, "dve", kim[:, :, :], t1[:, :, :], t2[:, :, :], ALU.subtract, [t1, t2], [kim])
    TT(P, "dve", kim[:, :, :], kim[:, :, :], den[:, :, :], ALU.mult, [kim, den], [kim])
    Ere = P.sbuf("Ere", [128, 16, TC], F32); Eim = P.sbuf("Eim", [128, 16, TC], F32)
    cs1f = cs1.ap.rearrange("p d b -> p (d b)"); sn1f = sn1.ap.rearrange("p d b -> p (d b)")
    P.ew("dve", "tensor_copy", [cs1], [Ere], Ere[:, :, 0:1], cs1f.unsqueeze(2))
    P.ew("dve", "tensor_copy", [sn1], [Eim], Eim[:, :, 0:1], sn1f.unsqueeze(2))
    ta = P.sbuf("Eta", [128, 16, TC // 2], F32); tb_ = P.sbuf("Etb", [128, 16, TC // 2], F32)
    m = 1
    while m < TC:
        cr = Ere[:, :, m - 1:m].broadcast_to([128, 16, m]); ci = Eim[:, :, m - 1:m].broadcast_to([128, 16, m])
        TT(P, "dve", ta[:, :, 0:m], Ere[:, :, 0:m], cr, ALU.mult, [Ere], [ta])
        TT(P, "pool", tb_[:, :, 0:m], Eim[:, :, 0:m], ci, ALU.mult, [Eim], [tb_])
        TT(P, "dve", Ere[:, :, m:2 * m], ta[:, :, 0:m], tb_[:, :, 0:m], ALU.subtract, [ta, tb_, Eim], [Ere])
        TT(P, "dve", ta[:, :, 0:m], Ere[:, :, 0:m], ci, ALU.mult, [Ere, Eim], [ta])
        TT(P, "pool", tb_[:, :, 0:m], Eim[:, :, 0:m], cr, ALU.mult, [Eim, Ere], [tb_])
        TT(P, "dve", Eim[:, :, m:2 * m], ta[:, :, 0:m], tb_[:, :, 0:m], ALU.add, [ta, tb_], [Eim])
        m *= 2
    P.ew("dve", "tensor_copy", [Ere], [E2], E2[:, :, 0, :], Ere[:, :, :])
    P.ew("pool", "tensor_copy", [Ere], [E2], E2[:, :, 1, :], Ere[:, :, :])
    P.ew("dve", "tensor_copy", [Eim], [E3], E3[:, :, 0, :], Eim[:, :, :])
    TS(P, "dve", E3[:, :, 1, :], Eim[:, :, :], -1.0, None, ALU.mult, None, [Eim], [E3])
    P.ew("dve", "tensor_copy", [rr], [Rt], Rt[:, :, :], rr.ap.rearrange("p d b -> p (d b)").unsqueeze(2).broadcast_to([128, 16, TC]))
    Bf = P.sbuf("Bf", [128, 2, 2, 8, 128], F32)
    Cf = P.sbuf("Cf", [128, 2, 2, 8, 128], F32)
    P.ew("pool", "memset", [], [Bf], Bf[:, :, :, :, :], 0.0)
    P.ew("pool", "memset", [], [Cf], Cf[:, :, :, :, :], 0.0)
    nq = 0
    for d in range(2):
        for g in range(16):
            blk = g // 2; gl = g % 2; c0 = (g % 8) * 16
            for ri, (bn, cn_) in enumerate((("s5_b_re", "s5_c_re"), ("s5_b_im", "s5_c_im"))):
                eng = "sp" if nq % 2 else "pool"; nq += 1
                P.dma(Bf[c0:c0 + 16, d, ri, blk, gl * 64:(gl + 1) * 64], I[bn].ap[l, d, g].rearrange("n h -> h n"), [I[bn]], [Bf], eng=eng, allow_slow_non_contiguous=True)
                P.dma(Cf[gl * 64:(gl + 1) * 64, d, ri, blk, c0:c0 + 16], I[cn_].ap[l, d, g].rearrange("h n -> n h"), [I[cn_]], [Cf], eng=eng, allow_slow_non_contiguous=True)
    P.ew("dve", "tensor_copy", [Bf], [Bb], Bb[:, :, :, :, :], Bf[:, :, :, :, :])
    c1 = P.sbuf("c1", [128, 8, 128], F32); c2 = P.sbuf("c2", [128, 8, 128], F32)
    for d in range(2):
        kr = kre[:, d, :].unsqueeze(2).broadcast_to([128, 8, 128]); ki = kim[:, d, :].unsqueeze(2).broadcast_to([128, 8, 128])
        TT(P, "dve", c1[:, :, :], Cf[:, d, 0, :, :], kr, ALU.mult, [Cf, kre], [c1])
        TT(P, "pool", c2[:, :, :], Cf[:, d, 1, :, :], ki, ALU.mult, [Cf, kim], [c2])
        TT(P, "dve", Cb[:, d, 0, :, :], c1[:, :, :], c2[:, :, :], ALU.subtract, [c1, c2], [(Cb, (d, 0))])
        TT(P, "dve", c1[:, :, :], Cf[:, d, 0, :, :], ki, ALU.mult, [Cf, kim], [c1])
        TT(P, "pool", c2[:, :, :], Cf[:, d, 1, :, :], kr, ALU.mult, [Cf, kre], [c2])
        STT(P, "dve", Cb[:, d, 1, :, :], c1[:, :, :], -1.0, c2[:, :, :], ALU.mult, ALU.subtract, [c1, c2], [(Cb, (d, 1))])
    P.barrier()
    P.sb_reset(s5mark)
    uS0 = P.sbuf("uS0", [128, 2, T], BF16)
    uS = [uS0, uS0]
    yacc_off = P.sb_off
    yacc = P.sbuf("s5yacc", [128, 2, T], F32)
    ur_off = P.sb_off
    ur = P.sbuf("ur", [128, 2, T], F32)
    for h in range(2):
        P.dma(ur[:, h, :], k.featT[S5ROW + h * 128:S5ROW + (h + 1) * 128, :], [k.featT], [ur], eng=("sp" if h else "pool"))
    for h in range(2):
        P.ew("dve", "tensor_copy", [ur], [(uS[0], h)], uS[0][:, h, 0:CTX], ur[:, h, 0:CTX])
        P.ew("dve", "tensor_copy", [ur], [(uS[0], h)], uS[0][:, h, CTX:T].rearrange("p (c r) -> p c r", c=64), ur[:, h, CTX:T].rearrange("p (r c) -> p c r", c=64))
    P.barrier(); P.sb_reset(ur_off)
    def wt(name, dt=F32):
        return [P.sbuf(f"{name}{i}", [128, 2, TC], dt) for i in range(4)]
    tA = wt("s5A"); tB = wt("s5B"); tC = wt("s5C"); tD = wt("s5D"); sbb = wt("s5sbb", BF16)
    carry = P.sbuf("carry", [128, 8, 2], F32)
    zero1 = P.sbuf("zero1", [128, 1], F32)
    P.ew("pool", "memset", [], [zero1], zero1[:, :], 0.0)
    n = 0
    for d in range(2):
        for ci in range(NCH):
            t0 = ci * TC
            if d == 0:
                sl0, sl1 = t0, t0 + TC; rev = False
            elif t0 < CTX:
                sl0, sl1 = 0, CTX; rev = True
            else:
                sl0 = CTX + (L - (t0 - CTX) - TC); sl1 = sl0 + TC; rev = True
            for h in range(2):
                usl = uS0[:, h, sl0:sl1]
                if rev:
                    usl = usl[:, ::-1]
                blks = [h * 4 + q for q in range(4)]
                pzs_ = [k.ps[q] for q in range(4)]
                py = k.ps[4 + (n % 2)]; n += 1
                for q, blk in enumerate(blks):
                    pz = pzs_[q]
                    P.mm(pz[:, 0:TC], Bb[:, d, 0, blk, :], usl, [Bb, (uS0, h)], [pz], start=True, stop=True)
                    P.mm(pz[:, TC:2 * TC], Bb[:, d, 1, blk, :], usl, [Bb, (uS0, h)], [pz], start=True, stop=True)
                for q, blk in enumerate(blks):
                    e16 = d * 8 + blk
                    pz3 = pzs_[q].ap.rearrange("p (a b) -> p a b", a=2)
                    TT(P, "dve", tA[q][:, :, :], pz3, E2[:, e16, :, :], ALU.mult, [pzs_[q], E2], [tA[q]])
                    TT(P, "dve", tB[q][:, :, :], pz3[:, ::-1, :], E3[:, e16, :, :], ALU.mult, [pzs_[q], E3], [tB[q]])
                for q, blk in enumerate(blks):
                    TT(P, "pool", tC[q][:, :, :], tA[q][:, :, :], tB[q][:, :, :], ALU.add, [tA[q], tB[q]], [tC[q]])
                for q, blk in enumerate(blks):
                    e16 = d * 8 + blk
                    for c_ in range(2):
                        init = zero1[:, 0:1] if ci == 0 else carry[:, blk, c_:c_ + 1]
                        P.add("dve", lambda e, q=q, c_=c_, init=init, e16=e16: e.tensor_tensor_scan(tA[q][:, c_, :], Rt[:, e16, :], tC[q][:, c_, :], init, ALU.mult, ALU.add), [Rt, tC[q], (carry, blk), zero1], [(tA[q], c_)])
                for q, blk in enumerate(blks):
                    ACT(P, tB[q][:, :, :], tA[q][:, ::-1, :], AF.Copy, [tA[q]], [tB[q]])
                for q, blk in enumerate(blks):
                    e16 = d * 8 + blk
                    TT(P, "pool", tC[q][:, :, :], tA[q][:, :, :], E2[:, e16, :, :], ALU.mult, [tA[q], E2], [tC[q]])
                    TT(P, "dve" if q % 2 else "pool", tD[q][:, :, :], tB[q][:, :, :], E3[:, e16, :, :], ALU.mult, [tB[q], E3], [tD[q]])
                for q, blk in enumerate(blks):
                    TT(P, "dve", tA[q][:, :, :], tC[q][:, :, :], tD[q][:, :, :], ALU.subtract, [tC[q], tD[q]], [tA[q]])
                for q, blk in enumerate(blks):
                    ACT(P, sbb[q][:, :, :], tA[q][:, :, :], AF.Copy, [tA[q]], [sbb[q]])
                    ACT(P, carry[:, blk, :], tA[q][:, :, TC - 1], AF.Copy, [tA[q]], [(carry, blk)])
                for q, blk in enumerate(blks):
                    P.mm(py[:, 0:TC], Cb[:, d, 0, blk, :], sbb[q][:, 0, :], [Cb, sbb[q]], [py], start=(q == 0), stop=False)
                    P.mm(py[:, 0:TC], Cb[:, d, 1, blk, :], sbb[q][:, 1, :], [Cb, sbb[q]], [py], start=False, stop=(q == 3))
                if d == 0:
                    ACT(P, yacc[:, h, t0:t0 + TC], py[:, 0:TC], AF.Copy, [py], [(yacc, (h, sl0 // TC))])
                else:
                    dst = yacc[:, h, sl0:sl1][:, ::-1]
                    TT(P, "dve", dst, dst, py[:, 0:TC], ALU.add, [py, (yacc, (h, sl0 // TC))], [(yacc, (h, sl0 // TC))])
    P.barrier(); P.sb_reset(ur_off)
    ur = P.sbuf("ur2", [128, 2, T], F32)
    for h in range(2):
        P.dma(ur[:, h, :], k.featT[S5ROW + h * 128:S5ROW + (h + 1) * 128, :], [k.featT], [ur], eng=("sp" if h else "pool"))
    ur_end = P.sb_off
    dsk = P.sbuf("dsk", [128, 2], F32); bgl = P.sbuf("bgl", [128, 2], F32)
    P.dma(dsk[:, :], I["s5_d"].ap[l].rearrange("(h p) -> p h", p=128), [I["s5_d"]], [dsk], allow_slow_non_contiguous=True)
    P.dma(bgl[:, :], I["s5_b_glu"].ap[l].rearrange("(h p) -> p h", p=128), [I["s5_b_glu"]], [bgl], allow_slow_non_contiguous=True)
    wgf = P.sbuf("s5wgf", [128, 2, 256], F32); wgb = P.sbuf("s5wgb", [128, 2, 256], BF16)
    P.dma(wgf[:, :, :], I["s5_w_glu"].ap[l].rearrange("(c p) n -> p c n", p=128), [I["s5_w_glu"]], [wgf])
    P.ew("dve", "tensor_copy", [wgf], [wgb], wgb[:, :, :], wgf[:, :, :])
    zb = uS0
    P.barrier()
    for h in range(2):
        STT(P, "dve", yacc[:, h, 0:CTX], ur[:, h, 0:CTX], dsk[:, h:h + 1], yacc[:, h, 0:CTX], ALU.mult, ALU.add, [ur, dsk, yacc], [yacc])
        STT(P, "dve", yacc[:, h, CTX:T].rearrange("p (c r) -> p c r", c=64), ur[:, h, CTX:T].rearrange("p (r c) -> p c r", c=64), dsk[:, h:h + 1], yacc[:, h, CTX:T].rearrange("p (c r) -> p c r", c=64), ALU.mult, ALU.add, [ur, dsk, yacc], [yacc])
    P.barrier()
    save_off = P.sb_off
    P.sb_reset(ur_off)
    zf = P.sbuf("s5zf", [128, 2, T], F32)
    P.sb_reset(max(save_off, P.sb_off))
    for h in range(2):
        y = yacc[:, h, :]
        TT(P, "dve", zf[:, h, :], y, y, ALU.mult, [yacc], [zf])
        TS(P, "dve", zf[:, h, :], zf[:, h, :], 0.044715, 1.0, ALU.mult, ALU.add, [zf], [zf])
        TT(P, "dve", zf[:, h, :], zf[:, h, :], y, ALU.mult, [zf, yacc], [zf])
        ACT(P, zf[:, h, :], zf[:, h, :], AF.Sigmoid, [zf], [zf], scale=1.5957691216057308)
        TT(P, "dve", zf[:, h, :], zf[:, h, :], y, ALU.mult, [zf, yacc], [zf])
        ACT(P, zb[:, h, :], zf[:, h, :], AF.Copy, [zf], [zb])
    P.barrier()
    save2 = P.sb_off
    P.sb_reset(yacc_off)
    yd = P.sbuf("s5yd", [128, 2, T], BF16)
    P.sb_reset(save2)
    sgl = [P.sbuf(f"sgl{i}", [128, 512], F32) for i in range(2)]
    blocks = [(0, CTX)] + [(CTX + b * 512, 512) for b in range(8)]
    n = 0
    for (t0, tn) in blocks:
        for ho in range(2):
            pg = k.ps[n % 2]; sg_ = sgl[n % 2]; n += 1
            for hi in range(2):
                P.mm(pg[:, 0:tn], wgb[:, hi, ho * 128:(ho + 1) * 128], zb[:, hi, t0:t0 + tn], [wgb, zb], [pg], start=(hi == 0), stop=(hi == 1))
            ACT(P, sg_[:, 0:tn], pg[:, 0:tn], AF.Sigmoid, [pg, bgl], [sg_], bias=bgl[:, ho:ho + 1])
            if t0 < CTX:
                TT(P, "dve", yd[:, ho, 0:CTX], zf[:, ho, 0:CTX], sg_[:, 0:CTX], ALU.mult, [zf, sg_], [(yd, (ho, t0))])
            else:
                c0 = (t0 - CTX) // 64
                dst = yd[:, ho, CTX:T].rearrange("p (r c) -> p c r", c=64)[:, c0:c0 + 8, :]
                TT(P, "dve", dst, zf[:, ho, t0:t0 + tn].rearrange("p (c r) -> p c r", c=8), sg_[:, 0:tn].rearrange("p (c r) -> p c r", c=8), ALU.mult, [zf, sg_], [(yd, (ho, t0))])
    for ho in range(2):
        P.dma(k.yT[768 + ho * 128:768 + (ho + 1) * 128, :], yd[:, ho, :], [yd], [(k.yT, ("s5", ho))])


def Bf_mark(P, Bf, k):
    return P.sb_off


USE_F32R = False


def R32(ap):
    return ap.bitcast(mybir.dt.float32r) if USE_F32R else ap


NCHK = T // 64
INV_DT = mybir.dt.float32r
BLK = 128


def build_masks(k):
    P = k.P
    ones = P.sbuf("m_ones", [64, 8, 64], F32)
    P.ew("pool", "memset", [], [ones], ones[:, :, :], 1.0)
    k.m_ts_s = P.sbuf("m_ts_s", [64, 8, 64], F32)
    k.m_st_s = P.sbuf("m_st_s", [64, 8, 64], F32)
    k.m_st_i = P.sbuf("m_st_i", [64, 8, 64], F32)
    P.add("pool", lambda e: e.affine_select(k.m_ts_s[:, :, :], ones[:, :, :], [[0, 8], [-1, 64]], ALU.is_gt, 0.0, base=0, channel_multiplier=1), [ones], [k.m_ts_s])
    P.add("pool", lambda e: e.affine_select(k.m_st_s[:, :, :], ones[:, :, :], [[0, 8], [1, 64]], ALU.is_gt, 0.0, base=0, channel_multiplier=-1), [ones], [k.m_st_s])
    P.add("pool", lambda e: e.affine_select(k.m_st_i[:, :, :], ones[:, :, :], [[0, 8], [1, 64]], ALU.is_ge, 0.0, base=0, channel_multiplier=-1), [ones], [k.m_st_i])
    k.identf8 = P.sbuf("identf8", [64, 8, 64], F32)
    P.add("pool", lambda e: e.affine_select(k.identf8[:, :, :], ones[:, :, :], [[0, 8], [1, 64]], ALU.is_equal, 0.0, base=0, channel_multiplier=-1), [ones], [k.identf8])


class DplrChain:
    def __init__(self, k, tag, opsD, pcD, youtD, opmap, dn_rows, banks):
        P = k.P
        self.k = k; self.opsD = opsD; self.youtD = youtD; self.opmap = opmap; self.dn_rows = dn_rows
        self.b = [k.ps[i] for i in banks]
        self.NO = max(opmap.values()) + 1
        self.pc = P.sbuf("pc", [64, 8, NCHK], F32)
        P.dma(self.pc[:, :, :], pcD.ap.rearrange("i c n -> c i n"), [pcD], [self.pc])
        self.ST = P.sbuf("ST", [64, 8, 64], F32); self.STb = P.sbuf("STb", [64, 8, 64], BF16)
        P.ew("pool", "memset", [], [self.ST], self.ST[:, :, :], 0.0)
        P.ew("pool", "memset", [], [self.STb], self.STb[:, :, :], 0.0)
        self.opt = [[P.sbuf(f"opt{b}_{o}", [64, 8, BLK], BF16) for o in range(self.NO)] for b in range(2)]
        self.yblk = [P.sbuf(f"yblk{b}", [64, 8, BLK], F32) for b in range(2)]

        def t3(name, dt=F32, n=2):
            return [P.sbuf(f"{name}{i}", [64, 8, 64], dt) for i in range(n)]
        self.S_ab = t3("Sab", INV_DT); self.S_abT = t3("SabT", INV_DT)
        self.Xs = t3("Xs", INV_DT, 1)[0]; self.Ub = t3("Ub", BF16, 1)[0]
        self.S_akT2 = t3("SakT2", BF16); self.S_rbT2 = t3("SrbT2", BF16); self.S_rkT2 = t3("SrkT2", BF16)
        self.BhT2 = t3("BhT2", BF16); self.KhT2 = t3("KhT2", BF16); self.vT2 = t3("vT2", BF16)
        self.Pm2 = [t3("PmA", INV_DT), t3("PmB", INV_DT)]
        if dn_rows is not None:
            self.Lg = [P.sbuf(f"Lg{b}", [2, 8, BLK], F32) for b in range(2)]
            self.Rg = [P.sbuf(f"Rg{b}", [2, 8, BLK], F32) for b in range(2)]
            for b in range(2):
                P.ew("pool", "memset", [], [self.Lg[b]], self.Lg[b][:, :, :], 1.0)
                P.ew("pool", "memset", [], [self.Rg[b]], self.Rg[b][:, :, :], 1.0)
            self.bias = []
            for nm, mk in (("b_ts_s", k.m_ts_s), ("b_st_s", k.m_st_s), ("b_st_i", k.m_st_i)):
                bt = P.sbuf(nm, [64, 8, 64], F32)
                TS(P, "dve", bt[:, :, :], mk[:, :, :], -1.0, 30000.0, ALU.add, ALU.mult, [mk], [bt])
                self.bias.append(bt)
            self.D = [P.sbuf(nm, [64, 8, 64], F32) for nm in ("D_ts", "D_st_s", "D_st_i")]

    def load(self, blk):
        P = self.k.P
        t0 = blk * BLK
        ob = self.opt[blk % 2]
        for o in range(self.NO):
            P.dma(ob[o][:, :, :], self.opsD.ap[o, :, :, t0:t0 + BLK].rearrange("i c t -> c i t"), [self.opsD], [ob[o]], eng=("sp" if o % 2 else "pool"))
        if self.dn_rows is not None:
            GrowD, nGrowD = self.dn_rows
            lg = self.Lg[blk % 2]; rg = self.Rg[blk % 2]
            P.dma(lg[0:1, :, :], GrowD.ap[:, t0:t0 + BLK].unsqueeze(0), [GrowD], [(lg, 0)])
            P.dma(rg[1:2, :, :], nGrowD.ap[:, t0:t0 + BLK].unsqueeze(0), [nGrowD], [(rg, 0)])

    def store(self, blk):
        P = self.k.P
        t0 = blk * BLK
        yb = self.yblk[blk % 2]
        P.dma(self.youtD.ap[:, :, t0:t0 + BLK].rearrange("i c t -> c i t"), yb[:, :, :], [yb], [(self.youtD, blk)], eng="pool")

    def par(self, g):
        k = self.k; P = k.P
        blk, jl = divmod(g, BLK // 64)
        slot = g % 2
        b0, b1, b2 = self.b[0], self.b[1], self.b[2]
        ob = self.opt[blk % 2]
        ib = k.ident_b[0:64, 0:64]
        S_ab, S_abT = self.S_ab, self.S_abT
        S_akT, S_rbT, S_rkT = self.S_akT2[slot], self.S_rbT2[slot], self.S_rkT2[slot]
        Pm = self.Pm2[slot]; BhT, KhT, vT = self.BhT2[slot], self.KhT2[slot], self.vT2[slot]

        def sub(pst, i):
            return pst[0:64, i * 64:(i + 1) * 64]

        def v3(pst):
            return pst.ap[0:64, :].rearrange("p (i c) -> p i c", i=8)
        sl = slice(jl * 64, (jl + 1) * 64)
        O = {n: ob[ix] for n, ix in self.opmap.items()}
        if self.dn_rows is None:
            mult = {"ab": (k.m_ts_s, 1.0), "abT": (k.m_st_s, 1.0), "akT": (k.m_st_s, 1.0), "rbT": (k.m_st_i, 1.0), "rkT": (k.m_st_i, 1.0)}
        else:
            lg = self.Lg[blk % 2]; rg = self.Rg[blk % 2]
            D_ts, D_st_s, D_st_i = self.D
            bias_ts_s, bias_st_s, bias_st_i = self.bias
            pd = b1; pdt = b2
            for i in range(8):
                P.mm(sub(pd, i), lg[:, i, sl], rg[:, i, sl], [lg, rg], [pd])
                P.mm(sub(pdt, i), rg[:, i, sl], lg[:, i, sl], [lg, rg], [pdt])
            TT(P, "dve", D_ts[:, :, :], v3(pd), bias_ts_s[:, :, :], ALU.add, [pd, bias_ts_s], [D_ts])
            ACT(P, D_ts[:, :, :], D_ts[:, :, :], AF.Exp, [D_ts], [D_ts])
            yield
            TT(P, "dve", D_st_s[:, :, :], v3(pdt), bias_st_s[:, :, :], ALU.add, [pdt, bias_st_s], [D_st_s])
            ACT(P, D_st_s[:, :, :], D_st_s[:, :, :], AF.Exp, [D_st_s], [D_st_s])
            TT(P, "dve", D_st_i[:, :, :], v3(pdt), bias_st_i[:, :, :], ALU.add, [pdt, bias_st_i], [D_st_i])
            ACT(P, D_st_i[:, :, :], D_st_i[:, :, :], AF.Exp, [D_st_i], [D_st_i])
            yield
            mult = {"ab": (D_ts, -1.0), "abT": (D_st_s, -1.0), "akT": (D_st_s, -1.0), "rbT": (D_st_i, 1.0), "rkT": (D_st_i, 1.0)}
        A0 = S_ab[0]; B0 = S_abT[0]
        kinds = [("ab", "sc_a", "sc_b", A0, b0), ("abT", "sc_b", "sc_a", B0, b1), ("akT", "sc_k", "sc_a", S_akT, b2),
                 ("rbT", "sc_b", "sc_r", S_rbT, b0), ("rkT", "sc_k", "sc_r", S_rkT, b1)]
        for kind, ln, rn, dst, pst in kinds:
            for i in range(8):
                P.mm(sub(pst, i), O[ln][:, i, sl], O[rn][:, i, sl], [O[ln], O[rn]], [pst])
            mt, sgn = mult[kind]
            STT(P, "dve", dst[:, :, :], v3(pst), sgn, mt[:, :, :], ALU.mult, ALU.mult, [pst, mt], [dst])
            yield
        for name, dst, pst, half in (("Bh", BhT, b2, 0), ("Kh", KhT, b2, 1), ("v", vT, b0, 0)):
            pv = pst.ap.bitcast(BF16)
            for i in range(8):
                P.tr(pv[0:64, half * 512 + i * 64: half * 512 + (i + 1) * 64], O[name][:, i, sl], ib, [O[name], k.ident_b], [pst])
            ACT(P, dst[:, :, :], pv[0:64, half * 512:(half + 1) * 512].rearrange("p (i c) -> p i c", i=8), AF.Copy, [pst], [dst])
            yield
        Pc_ = Pm[0]
        TT(P, "pool", Pc_[:, :, :], B0[:, :, :], k.identf8[:, :, :], ALU.add, [B0, k.identf8], [Pc_])
        Ac, Bc = A0, B0
        for jj in range(5):
            An = S_ab[(jj + 1) % 2]; Bn = S_abT[(jj + 1) % 2]; Pn = Pm[(jj + 1) % 2]
            pa = b0; pb = b1; pp = b2
            for i in range(8):
                P.mm(sub(pa, i), R32(Bc[:, i, :]), R32(Ac[:, i, :]), [Ac, Bc], [pa])
            ACT(P, An[:, :, :], v3(pa), AF.Copy, [pa], [An])
            if jj < 4:
                for i in range(8):
                    P.mm(sub(pb, i), R32(Ac[:, i, :]), R32(Bc[:, i, :]), [Ac, Bc], [pb])
                P.ew("dve", "tensor_copy", [pb], [Bn], Bn[:, :, :], v3(pb))
            yield
            for i in range(8):
                P.mm(sub(pp, i), R32(An[:, i, :]), R32(Pc_[:, i, :]), [An, Pc_], [pp])
            TT(P, "dve", Pn[:, :, :], v3(pp), Pc_[:, :, :], ALU.add, [pp, Pc_], [Pn])
            yield
            Ac, Bc, Pc_ = An, Bn, Pn

    def seq(self, g):
        k = self.k; P = k.P
        blk, jl = divmod(g, BLK // 64)
        slot = g % 2
        bq = self.b[3]
        ob = self.opt[blk % 2]; yb = self.yblk[blk % 2]
        S_akT, S_rbT, S_rkT = self.S_akT2[slot], self.S_rbT2[slot], self.S_rkT2[slot]
        NT = self.Pm2[slot][1]; BhT, KhT, vT = self.BhT2[slot], self.KhT2[slot], self.vT2[slot]
        Xs, Ub, ST, STb, pc = self.Xs, self.Ub, self.ST, self.STb, self.pc

        def sub(pst, i):
            return pst[0:64, i * 64:(i + 1) * 64]

        def v3(pst):
            return pst.ap[0:64, :].rearrange("p (i c) -> p i c", i=8)
        cj = g
        sl = slice(jl * 64, (jl + 1) * 64)
        O = {n: ob[ix] for n, ix in self.opmap.items()}
        for i in range(8):
            P.mm(sub(bq, i), O["st_a"][:, i, sl], STb[:, i, :], [O["st_a"], STb], [bq], start=True, stop=False)
            P.mm(sub(bq, i), S_akT[:, i, :], vT[:, i, :], [S_akT, vT], [bq], start=False, stop=True)
        ACT(P, Xs[:, :, :], v3(bq), AF.Copy, [bq], [Xs])
        yield
        for i in range(8):
            P.mm(sub(bq, i), R32(NT[:, i, :]), R32(Xs[:, i, :]), [NT, Xs], [bq])
        P.ew("dve", "tensor_copy", [bq], [Ub], Ub[:, :, :], v3(bq))
        yield
        for i in range(8):
            P.mm(sub(bq, i), BhT[:, i, :], Ub[:, i, :], [BhT, Ub], [bq], start=True, stop=False)
            P.mm(sub(bq, i), KhT[:, i, :], vT[:, i, :], [KhT, vT], [bq], start=False, stop=True)
        TT(P, "pool", ST[:, :, :], ST[:, :, :], pc[:, :, cj:cj + 1].broadcast_to([64, 8, 64]), ALU.mult, [ST, pc], [ST])
        TT(P, "dve", ST[:, :, :], ST[:, :, :], v3(bq), ALU.add, [ST, bq], [ST])
        yield
        for i in range(8):
            P.mm(sub(bq, i), STb[:, i, :], O["st_r"][:, i, sl], [STb, O["st_r"]], [bq], start=True, stop=False)
            P.mm(sub(bq, i), Ub[:, i, :], S_rbT[:, i, :], [Ub, S_rbT], [bq], start=False, stop=False)
            P.mm(sub(bq, i), vT[:, i, :], S_rkT[:, i, :], [vT, S_rkT], [bq], start=False, stop=True)
        ACT(P, yb[:, :, sl], v3(bq), AF.Copy, [bq], [(yb, jl)])
        ACT(P, STb[:, :, :], ST[:, :, :], AF.Copy, [ST], [STb])
        yield


def dplr_run(k, chains_spec):
    P = k.P
    P.barrier(); P.sb_reset(k.const_mark)
    build_masks(k)
    chains = []
    for ci, (tag, opsD, pcD, youtD, opmap, dn_rows) in enumerate(chains_spec):
        banks = (0, 1, 2, 3) if ci == 0 else (4, 5, 6, 7)
        chains.append(DplrChain(k, tag, opsD, pcD, youtD, opmap, dn_rows, banks))
    spb = BLK // 64
    nstep = NCHK

    def drive(gens):
        live = list(gens)
        while live:
            nxt = []
            for g_ in live:
                try:
                    next(g_)
                    nxt.append(g_)
                except StopIteration:
                    pass
            live = nxt
    for c in chains:
        c.load(0)
        c.load(1)
    drive([c.par(0) for c in chains])
    for g in range(nstep):
        gens = []
        for c in chains:
            if g + 1 < nstep:
                gens.append(c.par(g + 1))
            gens.append(c.seq(g))
        drive(gens)
        if (g + 1) % spb == 0:
            blk = g // spb
            for c in chains:
                c.store(blk)
                if blk + 2 < T // BLK:
                    c.load(blk + 2)


def seg_pairs(d):
    if d == 0:
        return [((0, T), (0, T), False)]
    return [((0, CTX), (0, CTX), True), ((CTX, T), (CTX, T), True)]


def nat2scan(src, d, lo, hi):
    a = src[:, lo:hi]
    return a[:, ::-1] if d == 1 else a


DN_MAP = {"sc_a": 0, "sc_b": 1, "sc_k": 0, "sc_r": 2, "st_a": 3, "st_r": 4, "Bh": 5, "Kh": 6, "v": 7}
RW_MAP = {"sc_a": 0, "sc_b": 1, "sc_k": 2, "sc_r": 3, "st_a": 0, "st_r": 3, "Bh": 4, "Kh": 5, "v": 6}


def alloc_dplr_scratch(k):
    s = k.scratch
    k.dn_ops = s("dn_ops", [8, 8, 64, T], BF16); k.dn_pc = s("dn_pc", [8, 64, NCHK]); k.dn_y = s("dn_y", [8, 64, T])
    k.dn_G = s("dn_G", [8, T]); k.dn_nG = s("dn_nG", [8, T]); k.dn_rows = s("dn_rows", [3, 8, T])
    k.rw_ops = s("rw_ops", [7, 8, 64, T], BF16); k.rw_pc = s("rw_pc", [8, 64, NCHK]); k.rw_y = s("rw_y", [8, 64, T])


def phase_dn_prep(k, l):
    P = k.P; I = k.I
    P.barrier(); P.sb_reset(k.const_mark)
    mark0 = P.sb_off
    def rt(name):
        return P.sbuf(name, [8, T], F32)
    t_al = rt("t_al"); t_be = rt("t_be"); t_lar = rt("t_lar"); t_ber = rt("t_ber"); rm = rt("rm"); Gn = rt("Gn"); Gr = rt("Gr"); tmp1 = rt("tmp1"); tmp2 = rt("tmp2")
    P.dma(t_al[:, :], k.featT[1920:1928, :], [k.featT], [t_al])
    P.dma(t_be[:, :], k.featT[1928:1936, :], [k.featT], [t_be], eng="pool")
    alog = P.sbuf("alog", [8, 1], F32); dtb = P.sbuf("dtb", [8, 1], F32); one8 = P.sbuf("one8", [8, 1], F32)
    P.dma(alog[:, :], I["dn_a_log"].ap[l].rearrange("d (h o) -> (d h) o", o=1), [I["dn_a_log"]], [alog])
    P.dma(dtb[:, :], I["dn_dt_bias"].ap[l].rearrange("d (h o) -> (d h) o", o=1), [I["dn_dt_bias"]], [dtb])
    P.ew("pool", "memset", [], [one8], one8[:, :], 1.0)
    ACT(P, alog[:, :], alog[:, :], AF.Exp, [alog], [alog])
    TS(P, "dve", alog[:, :], alog[:, :], -1.0, None, ALU.mult, None, [alog], [alog])
    ACT(P, t_al[:, :], t_al[:, :], AF.Exp, [t_al, dtb], [t_al], bias=dtb[:, :])
    ACT(P, t_al[:, :], t_al[:, :], AF.Ln, [t_al, one8], [t_al], bias=one8[:, :])
    TS(P, "dve", t_al[:, :], t_al[:, :], alog[:, 0:1], None, ALU.mult, None, [t_al, alog], [t_al])
    ACT(P, t_be[:, :], t_be[:, :], AF.Sigmoid, [t_be], [t_be])
    for (dlo, dhi), (slo, shi), _ in seg_pairs(1):
        P.ew("dve", "tensor_copy", [t_al], [t_lar], t_lar[:, dlo:dhi], t_al[:, slo:shi][:, ::-1])
        P.ew("pool", "tensor_copy", [t_be], [t_ber], t_ber[:, dlo:dhi], t_be[:, slo:shi][:, ::-1])
    P.ew("pool", "memset", [], [rm], rm[:, :], 1.0)
    P.ew("pool", "memset", [rm], [rm], rm.ap.rearrange("p (n c) -> p n c", c=64)[:, :, 0:1], 0.0)
    P.add("dve", lambda e: e.tensor_tensor_scan(Gn[:, :], rm[:, :], t_al[:, :], 0.0, ALU.mult, ALU.add), [rm, t_al], [Gn])
    P.add("dve", lambda e: e.tensor_tensor_scan(Gr[:, :], rm[:, :], t_lar[:, :], 0.0, ALU.mult, ALU.add), [rm, t_lar], [Gr])

    def put_rows(dst_fn, nat, rev):
        P.dma(dst_fn(0, 4), nat[0:4, :], [nat], [k.dn_rows, k.dn_G, k.dn_nG])
        P.dma(dst_fn(4, 8), rev[4:8, :], [rev], [k.dn_rows, k.dn_G, k.dn_nG], eng="pool")
    put_rows(lambda a, b: k.dn_rows.ap[0, a:b, :], t_be, t_ber)
    put_rows(lambda a, b: k.dn_G.ap[a:b, :], Gn, Gr)
    ACT(P, tmp1[:, :], Gn[:, :], AF.Exp, [Gn], [tmp1]); ACT(P, tmp2[:, :], Gr[:, :], AF.Exp, [Gr], [tmp2])
    put_rows(lambda a, b: k.dn_rows.ap[1, a:b, :], tmp1, tmp2)
    pcr = P.sbuf("pcr", [8, 2, NCHK], F32)
    for j, (G_, tp) in enumerate(((Gn, tmp1), (Gr, tmp2))):
        G3 = G_.ap.rearrange("p (n c) -> p n c", c=64)
        ACT(P, pcr[:, j, :], G3[:, :, 63], AF.Exp, [G_], [(pcr, j)])
    TS(P, "dve", t_al[:, :], Gn[:, :], -1.0, None, ALU.mult, None, [Gn], [t_al])
    TS(P, "dve", t_lar[:, :], Gr[:, :], -1.0, None, ALU.mult, None, [Gr], [t_lar])
    put_rows(lambda a, b: k.dn_nG.ap[a:b, :], t_al, t_lar)
    for G_, tp in ((Gn, tmp1), (Gr, tmp2)):
        G3 = G_.ap.rearrange("p (n c) -> p n c", c=64)
        TT(P, "dve", tp.ap.rearrange("p (n c) -> p n c", c=64), G3[:, :, 63:64].broadcast_to([8, NCHK, 64]), G3, ALU.subtract, [G_], [tp])
        ACT(P, tp[:, :], tp[:, :], AF.Exp, [tp], [tp])
    put_rows(lambda a, b: k.dn_rows.ap[2, a:b, :], tmp1, tmp2)
    pcs = k.scratch(f"dn_pcrow{l}", [8, NCHK])
    P.dma(pcs.ap[0:4, :], pcr[0:4, 0, :], [pcr], [pcs])
    P.dma(pcs.ap[4:8, :], pcr[4:8, 1, :], [pcr], [pcs])
    pcb = P.sbuf("pcb", [64, 8, NCHK], F32)
    for i in range(8):
        P.dma(pcb[:, i, :], pcs.ap[i, :].partition_broadcast(64), [pcs], [(pcb, i)])
    P.dma(k.dn_pc.ap.rearrange("i c n -> c i n"), pcb[:, :, :], [pcb], [k.dn_pc])
    P.barrier(); P.sb_reset(mark0)
    def ft(name, dt=F32):
        return P.sbuf(name, [128, T], dt)
    xin = ft("dxin"); u = [ft(f"du{j}") for j in range(3)]
    bbc = [ft(f"dbc{j}") for j in range(3)]; kb = ft("dkb")
    outs = [ft(f"dout{j}", BF16) for j in range(3)]
    sq = [P.sbuf(f"dsq{j}", [128, 512], mybir.dt.float32r) for j in range(2)]
    rs = [P.sbuf(f"drs{j}", [128, 512], F32) for j in range(2)]
    cw = P.sbuf("dcw", [128, 3, 5], F32)
    epsb = P.sbuf("depsb", [128, 1], F32)
    P.ew("pool", "memset", [], [epsb], epsb[:, :], EPS)
    no = 0
    for h in range(2):
        for part in range(3):
            P.dma(cw[:, part, :], I["dn_conv"].ap[l, :, part * 256 + h * 128: part * 256 + (h + 1) * 128].rearrange("j c -> c j"), [I["dn_conv"]], [(cw, part)], allow_slow_non_contiguous=True)
        for part in range(3):
            r0 = 896 + part * 256 + h * 128
            P.dma(xin[:, :], k.featT[r0:r0 + 128, :], [k.featT], [xin])
            acc = u[part]
            TS(P, "dve", acc[:, :], xin[:, :], cw[:, part, 2:3], None, ALU.mult, None, [xin, cw], [acc])
            n_ = 0
            for (lo, hi) in ((0, CTX), (CTX, T)):
                for j in (0, 1, 3, 4):
                    sh = j - 2
                    a = max(lo, lo - sh); b = min(hi, hi - sh)
                    STT(P, "dve", acc[:, a:b], xin[:, a + sh:b + sh], cw[:, part, j:j + 1], acc[:, a:b], ALU.mult, ALU.add, [xin, cw, acc], [acc]); n_ += 1
            ACT(P, acc[:, :], acc[:, :], AF.Silu, [acc], [acc])
        n_ = 0
        for part in range(2):
            for b in range(9):
                t0 = b * 512; tn = min(512, T - t0)
                s_ = sq[n_ % 2]; r_ = rs[n_ % 2]; pst = k.ps[n_ % 2]; n_ += 1
                ACT(P, s_[:, 0:tn], u[part][:, t0:t0 + tn], AF.Square, [u[part]], [s_])
                P.mm(pst[:, 0:tn], k.blk64r[:, :], s_[:, 0:tn], [k.blk64r, s_], [pst])
                ACT(P, r_[:, 0:tn], pst[:, 0:tn], AF.Ln, [pst, epsb], [r_], bias=epsb[:, :])
                ACT(P, r_[:, 0:tn], r_[:, 0:tn], AF.Exp, [r_], [r_], scale=-0.5)
                if part == 0:
                    STT(P, "dve", u[0][:, t0:t0 + tn], u[0][:, t0:t0 + tn], 0.125, r_[:, 0:tn], ALU.mult, ALU.mult, [u[0], r_], [u[0]])
                else:
                    TT(P, "dve", u[1][:, t0:t0 + tn], u[1][:, t0:t0 + tn], r_[:, 0:tn], ALU.mult, [u[1], r_], [u[1]])
        for d in range(2):
            i = d * 4 + 2 * h
            for j in range(3):
                for hh in range(2):
                    P.dma(bbc[j][hh * 64:(hh + 1) * 64, :], k.dn_rows.ap[j, i + hh, :].partition_broadcast(64), [k.dn_rows], [(bbc[j], hh)], eng=("sp" if (j + hh) % 2 else "pool"))
            beta_bc, EG_bc, E2_bc = bbc

            def emit(o_idx, fn):
                nonlocal no
                ot = outs[no % 3]; no += 1
                for (dlo, dhi), (slo, shi), _ in seg_pairs(d):
                    fn(ot, dlo, dhi)
                P.dma(k.dn_ops.ap[o_idx, i:i + 2, :, :].rearrange("i c t -> (i c) t"), ot[:, :], [ot], [(k.dn_ops, (o_idx, i))], eng=("sp" if no % 2 else "pool"))
            qs = lambda lo, hi: nat2scan(u[0], d, lo, hi)
            ks = lambda lo, hi: nat2scan(u[1], d, lo, hi)
            vs = lambda lo, hi: nat2scan(u[2], d, lo, hi)
            for (dlo, dhi), _, _ in seg_pairs(d):
                TT(P, "dve", kb[:, dlo:dhi], ks(dlo, dhi), beta_bc[:, dlo:dhi], ALU.mult, [u[1], beta_bc], [kb])
            emit(0, lambda ot, lo, hi: ACT(P, ot[:, lo:hi], kb[:, lo:hi], AF.Copy, [kb], [ot]))
            emit(1, lambda ot, lo, hi: ACT(P, ot[:, lo:hi], ks(lo, hi), AF.Copy, [u[1]], [ot]))
            emit(2, lambda ot, lo, hi: ACT(P, ot[:, lo:hi], qs(lo, hi), AF.Copy, [u[0]], [ot]))
            emit(3, lambda ot, lo, hi: STT(P, "dve", ot[:, lo:hi], kb[:, lo:hi], -1.0, EG_bc[:, lo:hi], ALU.mult, ALU.mult, [kb, EG_bc], [ot]))
            emit(4, lambda ot, lo, hi: TT(P, "dve", ot[:, lo:hi], qs(lo, hi), EG_bc[:, lo:hi], ALU.mult, [u[0], EG_bc], [ot]))
            emit(5, lambda ot, lo, hi: TT(P, "dve", ot[:, lo:hi], ks(lo, hi), E2_bc[:, lo:hi], ALU.mult, [u[1], E2_bc], [ot]))
            emit(6, lambda ot, lo, hi: TT(P, "dve", ot[:, lo:hi], kb[:, lo:hi], E2_bc[:, lo:hi], ALU.mult, [kb, E2_bc], [ot]))
            emit(7, lambda ot, lo, hi: ACT(P, ot[:, lo:hi], vs(lo, hi), AF.Copy, [u[2]], [ot]))


def phase_dn_post(k, l):
    P = k.P; I = k.I
    P.barrier(); P.sb_reset(k.const_mark)
    yf = P.sbuf("pyf", [128, T], F32); yr = P.sbuf("pyr", [128, T], F32); gt = P.sbuf("pgt", [128, T], F32)
    ob = P.sbuf("pob", [128, T], BF16)
    ng = P.sbuf("png", [128, 1], F32); epsb = P.sbuf("pepsb", [128, 1], F32)
    P.ew("pool", "memset", [], [epsb], epsb[:, :], EPS)
    P.dma(ng[0:64, :], I["dn_norm_g"].ap[l].rearrange("(c o) -> c o", o=1), [I["dn_norm_g"]], [ng])
    P.dma(ng[64:128, :], I["dn_norm_g"].ap[l].rearrange("(c o) -> c o", o=1), [I["dn_norm_g"]], [ng])
    sq = [P.sbuf(f"psq{j}", [128, 512], mybir.dt.float32r) for j in range(2)]
    rs = [P.sbuf(f"prs{j}", [128, 512], F32) for j in range(2)]
    n_ = 0
    for h in range(2):
        P.dma(yf[:, :], k.dn_y.ap[2 * h:2 * h + 2].rearrange("i c t -> (i c) t"), [k.dn_y], [yf])
        P.dma(yr[:, :], k.dn_y.ap[4 + 2 * h:4 + 2 * h + 2].rearrange("i c t -> (i c) t"), [k.dn_y], [yr], eng="pool")
        P.dma(gt[:, :], k.featT[1664 + h * 128:1664 + (h + 1) * 128, :], [k.featT], [gt])
        for (dlo, dhi), (slo, shi), _ in seg_pairs(1):
            TT(P, "dve", yf[:, slo:shi], yf[:, slo:shi], yr[:, dlo:dhi][:, ::-1], ALU.add, [yf, yr], [yf])
        ACT(P, gt[:, :], gt[:, :], AF.Silu, [gt], [gt])
        for b in range(9):
            t0 = b * 512; tn = min(512, T - t0)
            s_ = sq[n_ % 2]; r_ = rs[n_ % 2]; pst = k.ps[n_ % 2]; n_ += 1
            ACT(P, s_[:, 0:tn], yf[:, t0:t0 + tn], AF.Square, [yf], [s_])
            P.mm(pst[:, 0:tn], k.blk64r[:, :], s_[:, 0:tn], [k.blk64r, s_], [pst])
            ACT(P, r_[:, 0:tn], pst[:, 0:tn], AF.Ln, [pst, epsb], [r_], scale=1.0 / 64, bias=epsb[:, :])
            ACT(P, r_[:, 0:tn], r_[:, 0:tn], AF.Exp, [r_], [r_], scale=-0.5)
            STT(P, "dve", r_[:, 0:tn], yf[:, t0:t0 + tn], ng[:, 0:1], r_[:, 0:tn], ALU.mult, ALU.mult, [yf, ng, r_], [r_])
            TT(P, "dve", ob[:, t0:t0 + tn], r_[:, 0:tn], gt[:, t0:t0 + tn], ALU.mult, [r_, gt], [ob])
        P.dma(k.yT[512 + h * 128:512 + (h + 1) * 128, :], ob[:, :], [ob], [(k.yT, ("dn", h))])


def phase_dn(k, l):
    phase_dn_prep(k, l)
    dplr_run(k, [("dn", k.dn_ops, k.dn_pc, k.dn_y, DN_MAP, (k.dn_G, k.dn_nG))])
    phase_dn_post(k, l)


def rw_shift(k, l, dst, src, r0, n, mu, c0):
    P = k.P; I = k.I
    P.dma(mu[0:n, :], I["rw_mu"].ap[l, :, r0:r0 + n].rearrange("m c -> c m"), [I["rw_mu"]], [mu], allow_slow_non_contiguous=True)
    TT(P, "dve", c0[0:n, :], mu[0:n, 0:1], mu[0:n, 1:2], ALU.add, [mu], [c0])
    TS(P, "dve", c0[0:n, :], c0[0:n, :], -1.0, 1.0, ALU.mult, ALU.add, [c0], [c0])
    if dst is src:
        raise ValueError
    TS(P, "dve", dst[0:n, :], src[0:n, :], c0[0:n, 0:1], None, ALU.mult, None, [src, c0], [dst])
    for (lo, hi) in ((0, CTX), (CTX, T)):
        STT(P, "dve", dst[0:n, lo + 1:hi], src[0:n, lo:hi - 1], mu[0:n, 0:1], dst[0:n, lo + 1:hi], ALU.mult, ALU.add, [src, mu, dst], [dst])
        STT(P, "dve", dst[0:n, lo:hi - 1], src[0:n, lo + 1:hi], mu[0:n, 1:2], dst[0:n, lo:hi - 1], ALU.mult, ALU.add, [src, mu, dst], [dst])


def phase_rw_prep(k, l):
    P = k.P; I = k.I
    P.barrier(); P.sb_reset(k.const_mark)
    def ft(name, dt=F32, n=128):
        return P.sbuf(name, [n, T], dt)
    kS = ft("rkS"); kk = ft("rkk"); lwS = ft("rlwS"); aK = ft("raK"); bb = ft("rbb"); G = ft("rG"); xin = G; E = ft("rE"); rm = ft("rrm", BF16)
    rS = ft("rrS", BF16); vS = ft("rvS", BF16); wlS = ft("rwlS", BF16); alS = ft("ralS", BF16)
    outs = [ft(f"rout{j}", BF16) for j in range(2)]
    mu = P.sbuf("rmu", [128, 2], F32); c0 = P.sbuf("rc0", [128, 1], F32)
    sq = [P.sbuf(f"rsq{j}", [128, 512], mybir.dt.float32r) for j in range(2)]
    rs = [P.sbuf(f"rrs{j}", [128, 512], F32) for j in range(2)]
    epsb = P.sbuf("repsb", [128, 1], F32)
    P.ew("pool", "memset", [], [epsb], epsb[:, :], EPS)
    P.ew("pool", "memset", [], [rm], rm[:, :], 1.0)
    P.ew("pool", "memset", [rm], [rm], rm.ap.rearrange("p (n c) -> p n c", c=64)[:, :, 0:1], 0.0)
    P.dma(xin[0:32, :], k.featT[768:800, :], [k.featT], [xin])
    rw_shift(k, l, E, xin, 768, 32, mu, c0)
    ACT(P, wlS[0:32, :], E[0:32, :], AF.Tanh, [E], [wlS])
    P.dma(xin[0:32, :], k.featT[800:832, :], [k.featT], [xin])
    rw_shift(k, l, E, xin, 800, 32, mu, c0)
    ACT(P, alS[0:32, :], E[0:32, :], AF.Copy, [E], [alS])
    wuf = P.sbuf("rwuf", [32, 2, 2, 256], F32); wub = P.sbuf("rwub", [32, 2, 2, 256], BF16)
    for d in range(2):
        P.dma(wuf[:, d, 0, :], I["rw_w_up"].ap[l, d], [I["rw_w_up"]], [wuf])
        P.dma(wuf[:, d, 1, :], I["rw_a_up"].ap[l, d], [I["rw_a_up"]], [wuf])
    P.ew("dve", "tensor_copy", [wuf], [wub], wub[:, :, :, :], wuf[:, :, :, :])
    cols = P.sbuf("rcols", [128, 8], F32)
    pcb = P.sbuf("rpcb", [128, NCHK], F32)
    no = 0; n_ = 0
    for h in range(2):
        hs = slice(h * 128, (h + 1) * 128)
        for d in range(2):
            P.dma(cols[:, d:d + 1], I["rw_w0"].ap[l, d, hs].rearrange("(c o) -> c o", o=1), [I["rw_w0"]], [cols])
            P.dma(cols[:, 2 + d:3 + d], I["rw_a0"].ap[l, d, hs].rearrange("(c o) -> c o", o=1), [I["rw_a0"]], [cols])
        P.dma(cols[:, 4:5], I["rw_k_k"].ap[l, hs].rearrange("(c o) -> c o", o=1), [I["rw_k_k"]], [cols])
        P.dma(cols[:, 5:6], I["rw_k_a"].ap[l, hs].rearrange("(c o) -> c o", o=1), [I["rw_k_a"]], [cols])
        P.dma(xin[:, :], k.featT[h * 128:(h + 1) * 128, :], [k.featT], [xin])
        rw_shift(k, l, rS, xin, h * 128, 128, mu, c0)
        P.dma(xin[:, :], k.featT[512 + h * 128:512 + (h + 1) * 128, :], [k.featT], [xin])
        rw_shift(k, l, vS, xin, 512 + h * 128, 128, mu, c0)
        P.dma(xin[:, :], k.featT[256 + h * 128:256 + (h + 1) * 128, :], [k.featT], [xin])
        rw_shift(k, l, kS, xin, 256 + h * 128, 128, mu, c0)
        TS(P, "dve", kk[:, :], kS[:, :], cols[:, 4:5], None, ALU.mult, None, [kS, cols], [kk])
        for b in range(9):
            t0 = b * 512; tn = min(512, T - t0)
            s_ = sq[n_ % 2]; r_ = rs[n_ % 2]; pst = k.ps[n_ % 2]; n_ += 1
            ACT(P, s_[:, 0:tn], kk[:, t0:t0 + tn], AF.Square, [kk], [s_])
            P.mm(pst[:, 0:tn], k.blk64r[:, :], s_[:, 0:tn], [k.blk64r, s_], [pst])
            ACT(P, r_[:, 0:tn], pst[:, 0:tn], AF.Ln, [pst, epsb], [r_], bias=epsb[:, :])
            ACT(P, r_[:, 0:tn], r_[:, 0:tn], AF.Exp, [r_], [r_], scale=-0.5)
            TT(P, "dve", kk[:, t0:t0 + tn], kk[:, t0:t0 + tn], r_[:, 0:tn], ALU.mult, [kk, r_], [kk])
        for d in range(2):
            i = d * 4 + 2 * h
            for b in range(9):
                t0 = b * 512; tn = min(512, T - t0)
                pw = k.ps[2 + (n_ % 2)]; pa = k.ps[4 + (n_ % 2)]; n_ += 1
                P.mm(pw[:, 0:tn], wub[:, d, 0, hs], wlS[0:32, t0:t0 + tn], [wub, wlS], [pw])
                P.mm(pa[:, 0:tn], wub[:, d, 1, hs], alS[0:32, t0:t0 + tn], [wub, alS], [pa])
                ACT(P, E[:, t0:t0 + tn], pw[:, 0:tn], AF.Sigmoid, [pw, cols], [E], bias=cols[:, d:d + 1])
                ACT(P, aK[:, t0:t0 + tn], pa[:, 0:tn], AF.Sigmoid, [pa, cols], [aK], bias=cols[:, 2 + d:3 + d])
            for (dlo, dhi), _, _ in seg_pairs(d):
                TS(P, "dve", lwS[:, dlo:dhi], nat2scan(E, d, dlo, dhi), -R_DECAY, None, ALU.mult, None, [E], [lwS])
            TT(P, "dve", bb[:, :], kk[:, :], aK[:, :], ALU.mult, [kk, aK], [bb])
            TS(P, "dve", aK[:, :], aK[:, :], -1.0, cols[:, 5:6], ALU.add, ALU.mult, [aK, cols], [aK])
            STT(P, "dve", aK[:, :], aK[:, :], 1.0, kS[:, :], ALU.add, ALU.mult, [aK, kS], [aK])
            P.add("dve", lambda e: e.tensor_tensor_scan(G[:, :], rm[:, :], lwS[:, :], 0.0, ALU.mult, ALU.add), [rm, lwS], [G])
            G3 = G.ap.rearrange("p (n c) -> p n c", c=64)
            ACT(P, pcb[:, :], G3[:, :, 63], AF.Exp, [G], [pcb])
            P.dma(k.rw_pc.ap[i:i + 2].rearrange("i c n -> (i c) n"), pcb[:, :], [pcb], [(k.rw_pc, i)])

            def emit(o_idx, fn):
                nonlocal no
                ot = outs[no % 2]; no += 1
                for (dlo, dhi), _, _ in seg_pairs(d):
                    fn(ot, dlo, dhi)
                P.dma(k.rw_ops.ap[o_idx, i:i + 2, :, :].rearrange("i c t -> (i c) t"), ot[:, :], [ot], [(k.rw_ops, (o_idx, i))], eng=("sp" if no % 2 else "pool"))
            sc = lambda tl: (lambda lo, hi: nat2scan(tl, d, lo, hi))
            kks, bs, kms, rs_, vs_ = sc(kk), sc(bb), sc(aK), sc(rS), sc(vS)
            TT(P, "dve", E[:, :], G[:, :], lwS[:, :], ALU.subtract, [G, lwS], [E])
            ACT(P, E[:, :], E[:, :], AF.Exp, [E], [E])
            emit(0, lambda ot, lo, hi: STT(P, "dve", ot[:, lo:hi], kks(lo, hi), -1.0, E[:, lo:hi], ALU.mult, ALU.mult, [kk, E], [ot]))
            ACT(P, E[:, :], G[:, :], AF.Exp, [G], [E], scale=-1.0)
            emit(1, lambda ot, lo, hi: TT(P, "dve", ot[:, lo:hi], bs(lo, hi), E[:, lo:hi], ALU.mult, [bb, E], [ot]))
            emit(2, lambda ot, lo, hi: TT(P, "dve", ot[:, lo:hi], kms(lo, hi), E[:, lo:hi], ALU.mult, [aK, E], [ot]))
            ACT(P, E[:, :], G[:, :], AF.Exp, [G], [E])
            emit(3, lambda ot, lo, hi: TT(P, "dve", ot[:, lo:hi], rs_(lo, hi), E[:, lo:hi], ALU.mult, [rS, E], [ot]))
            TT(P, "dve", E.ap.rearrange("p (n c) -> p n c", c=64), G3[:, :, 63:64].broadcast_to([128, NCHK, 64]), G3, ALU.subtract, [G], [E])
            ACT(P, E[:, :], E[:, :], AF.Exp, [E], [E])
            emit(4, lambda ot, lo, hi: TT(P, "dve", ot[:, lo:hi], bs(lo, hi), E[:, lo:hi], ALU.mult, [bb, E], [ot]))
            emit(5, lambda ot, lo, hi: TT(P, "dve", ot[:, lo:hi], kms(lo, hi), E[:, lo:hi], ALU.mult, [aK, E], [ot]))
            emit(6, lambda ot, lo, hi: ACT(P, ot[:, lo:hi], vs_(lo, hi), AF.Copy, [vS], [ot]))


R_DECAY = math.exp(-0.5)
RW_GN_EPS = 64e-5


def phase_rw_post(k, l):
    P = k.P; I = k.I
    P.barrier(); P.sb_reset(k.const_mark)
    def ft(name, dt=F32, n=128):
        return P.sbuf(name, [n, T], dt)
    xin = ft("qxin"); yf = ft("qyf"); yr = ft("qyr"); rS = ft("qrS"); kS = ft("qkS"); vS = ft("qvS"); tmp = ft("qtmp")
    glS = ft("qglS", BF16); ob = ft("qob", BF16)
    mu = P.sbuf("qmu", [128, 2], F32); c0 = P.sbuf("qc0", [128, 1], F32)
    cols = P.sbuf("qcols", [128, 4], F32)
    gne = P.sbuf("qgne", [128, 1], F32)
    P.ew("pool", "memset", [], [gne], gne[:, :], RW_GN_EPS)
    guf = P.sbuf("qguf", [64, 256], F32); gub = P.sbuf("qgub", [64, 256], BF16)
    P.dma(guf[:, :], I["rw_g_up"].ap[l], [I["rw_g_up"]], [guf])
    P.ew("dve", "tensor_copy", [guf], [gub], gub[:, :], guf[:, :])
    P.dma(xin[0:64, :], k.featT[832:896, :], [k.featT], [xin])
    rw_shift(k, l, tmp, xin, 832, 64, mu, c0)
    ACT(P, glS[0:64, :], tmp[0:64, :], AF.Sigmoid, [tmp], [glS])
    w = [[P.sbuf(f"qw{j}_{b}", [128, 512], F32) for b in range(2)] for j in range(3)]
    n_ = 0
    for h in range(2):
        hs = slice(h * 128, (h + 1) * 128)
        P.dma(cols[:, 0:1], I["rw_r_k"].ap[l, 2 * h:2 * h + 2].rearrange("h (c o) -> (h c) o", o=1), [I["rw_r_k"]], [cols])
        P.dma(cols[:, 1:2], I["rw_ln_g"].ap[l, hs].rearrange("(c o) -> c o", o=1), [I["rw_ln_g"]], [cols])
        P.dma(cols[:, 2:3], I["rw_ln_b"].ap[l, hs].rearrange("(c o) -> c o", o=1), [I["rw_ln_b"]], [cols])
        for dst, r0 in ((rS, h * 128), (kS, 256 + h * 128), (vS, 512 + h * 128)):
            P.dma(xin[:, :], k.featT[r0:r0 + 128, :], [k.featT], [xin])
            rw_shift(k, l, dst, xin, r0, 128, mu, c0)
        P.dma(yf[:, :], k.rw_y.ap[2 * h:2 * h + 2].rearrange("i c t -> (i c) t"), [k.rw_y], [yf])
        P.dma(yr[:, :], k.rw_y.ap[4 + 2 * h:4 + 2 * h + 2].rearrange("i c t -> (i c) t"), [k.rw_y], [yr], eng="pool")
        for (dlo, dhi), (slo, shi), _ in seg_pairs(1):
            TT(P, "dve", yf[:, slo:shi], yf[:, slo:shi], yr[:, dlo:dhi][:, ::-1], ALU.add, [yf, yr], [yf])
        STT(P, "dve", tmp[:, :], rS[:, :], cols[:, 0:1], kS[:, :], ALU.mult, ALU.mult, [rS, cols, kS], [tmp])
        for b in range(9):
            t0 = b * 512; tn = min(512, T - t0)
            ym, s_, o_ = w[0][n_ % 2], w[1][n_ % 2], w[2][n_ % 2]
            pm = k.ps[(n_ % 2) * 4]; pv = k.ps[(n_ % 2) * 4 + 1]; pb = k.ps[(n_ % 2) * 4 + 2]; pg = k.ps[(n_ % 2) * 4 + 3]; n_ += 1
            P.mm(pm[:, 0:tn], k.blk64[:, :], yf[:, t0:t0 + tn], [k.blk64, yf], [pm])
            STT(P, "dve", ym[:, 0:tn], pm[:, 0:tn], -1.0 / 64, yf[:, t0:t0 + tn], ALU.mult, ALU.add, [pm, yf], [ym])
            ACT(P, s_[:, 0:tn], ym[:, 0:tn], AF.Square, [ym], [s_])
            P.mm(pv[:, 0:tn], k.blk64[:, :], s_[:, 0:tn], [k.blk64, s_], [pv])
            ACT(P, s_[:, 0:tn], pv[:, 0:tn], AF.Ln, [pv, gne], [s_], scale=1.0 / 64, bias=gne[:, :])
            ACT(P, s_[:, 0:tn], s_[:, 0:tn], AF.Exp, [s_], [s_], scale=-0.5)
            TT(P, "dve", ym[:, 0:tn], ym[:, 0:tn], s_[:, 0:tn], ALU.mult, [ym, s_], [ym])
            TS(P, "dve", ym[:, 0:tn], ym[:, 0:tn], cols[:, 1:2], cols[:, 2:3], ALU.mult, ALU.add, [ym, cols], [ym])
            P.mm(pb[:, 0:tn], k.blk64[:, :], tmp[:, t0:t0 + tn], [k.blk64, tmp], [pb])
            TT(P, "dve", o_[:, 0:tn], pb[:, 0:tn], vS[:, t0:t0 + tn], ALU.mult, [pb, vS], [o_])
            TT(P, "dve", o_[:, 0:tn], o_[:, 0:tn], ym[:, 0:tn], ALU.add, [o_, ym], [o_])
            P.mm(pg[:, 0:tn], gub[:, hs], glS[0:64, t0:t0 + tn], [gub, glS], [pg])
            TT(P, "dve", ob[:, t0:t0 + tn], o_[:, 0:tn], pg[:, 0:tn], ALU.mult, [o_, pg], [ob])
        P.dma(k.yT[256 + h * 128:256 + (h + 1) * 128, :], ob[:, :], [ob], [(k.yT, ("rw", h))])


def phase_rw(k, l):
    phase_rw_prep(k, l)
    dplr_run(k, [("rw", k.rw_ops, k.rw_pc, k.rw_y, RW_MAP, None)])
    phase_rw_post(k, l)


def phase_rw_dn(k, l):
    phase_rw_prep(k, l)
    phase_dn_prep(k, l)
    dplr_run(k, [("rw", k.rw_ops, k.rw_pc, k.rw_y, RW_MAP, None), ("dn", k.dn_ops, k.dn_pc, k.dn_y, DN_MAP, (k.dn_G, k.dn_nG))])
    phase_rw_post(k, l)
    phase_dn_post(k, l)


from concourse.bass_utils import run_bass_kernel_spmd

_CACHE = {}


def kernel(**inputs):
    if "k" not in _CACHE:
        _CACHE["k"] = build()
    k = _CACHE["k"]
    f32 = np.float32
    shared = {n: np.ascontiguousarray(inputs[n], dtype=f32) for n in list(SHARED) + list(PER_LAYER)}
    in_maps = []
    for b in range(8):
        m = dict(shared)
        m["x"] = np.ascontiguousarray(inputs["x"][b], dtype=f32)
        m["ctx"] = np.ascontiguousarray(inputs["ctx"][b], dtype=f32)
        m["c"] = np.ascontiguousarray(inputs["c"][b], dtype=f32)
        in_maps.append(m)
    res = run_bass_kernel_spmd(k.nc, in_maps, core_ids=list(range(8)))
    return np.stack([np.asarray(r["out"], dtype=f32) for r in res.results], axis=0)
```

```python
import numpy as np
import concourse.bass as bass
import concourse.mybir as mybir

F32 = mybir.dt.float32
BF16 = mybir.dt.bfloat16
I32 = mybir.dt.int32
ALU = mybir.AluOpType
AF = mybir.ActivationFunctionType
AX = mybir.AxisListType

DMA_RING = 8
SB_BASE = 16640
SB_LIMIT = 228000


class Buf:
    def __init__(self, name, handle, ap):
        self.name = name
        self.h = handle
        self.ap = ap

    def __getitem__(self, idx):
        return self.ap[idx]


class Op:
    __slots__ = ("eng", "fn", "dma", "deps", "idx", "needs_inc", "count", "slot", "slot_use")


class Prog:
    ENGS = ("pe", "dve", "act", "pool", "sp")

    def __init__(self, nc):
        self.nc = nc
        self.ops = []
        self.trk = {}
        self.eng_ops = {e: [] for e in self.ENGS}
        self.dma_cnt = {e: 0 for e in self.ENGS}
        self.slot_last = {}
        self.slot_uses = {}
        self.last_compute = {}
        self.barrier_deps = []
        self.sb_off = SB_BASE
        self.sb_names = 0
        self.sb_hi = 0

    def sb_reset(self, off=None):
        self.sb_off = SB_BASE if off is None else off

    def sbuf(self, name, shape, dtype=F32):
        esz = mybir.dt.size(dtype) if hasattr(mybir.dt, "size") else {F32: 4, BF16: 2, I32: 4, mybir.dt.float32r: 4}[dtype]
        free = 1
        for s in shape[1:]:
            free *= s
        nbytes = free * esz
        nbytes = (nbytes + 63) // 64 * 64
        self.sb_names += 1
        nm = f"{name}_{self.sb_names}"
        h = self.nc.alloc_sbuf_tensor_at(nm, list(shape), dtype, offset=self.sb_off)
        self.sb_off += nbytes
        self.sb_hi = max(self.sb_hi, self.sb_off)
        assert self.sb_off <= SB_LIMIT, f"SBUF overflow {self.sb_off}"
        return Buf(nm, h, h[:] if len(shape) == 1 else h[tuple(slice(None) for _ in shape)])

    def dram(self, name, shape, dtype=F32, kind="Internal"):
        h = self.nc.dram_tensor(name, list(shape), dtype, kind=kind)
        return Buf(name, h, h.ap())

    def _deps(self, reads, writes):
        deps = {}

        def add(i, kind):
            if i is None:
                return
            if i in deps:
                if kind == "RAW":
                    deps[i] = "RAW"
            else:
                deps[i] = kind

        def norm(x):
            if isinstance(x, tuple):
                return x[0].name, x[1]
            return x.name, None

        for r in reads:
            nm, key = norm(r)
            t = self.trk.setdefault(nm, {"w": {}, "r": {}})
            if key is None:
                for i in t["w"].values():
                    add(i, "RAW")
            else:
                add(t["w"].get(key), "RAW")
                add(t["w"].get(None), "RAW")
        for w in writes:
            nm, key = norm(w)
            t = self.trk.setdefault(nm, {"w": {}, "r": {}})
            if key is None:
                for i in t["w"].values():
                    add(i, "WAW")
                for l in t["r"].values():
                    for i in l:
                        add(i, "WAR")
            else:
                add(t["w"].get(key), "WAW")
                add(t["w"].get(None), "WAW")
                for i in t["r"].get(key, ()):
                    add(i, "WAR")
                for i in t["r"].get(None, ()):
                    add(i, "WAR")
        return deps, norm

    def add(self, eng, fn, reads=(), writes=(), dma=False):
        op = Op()
        op.eng = eng
        op.fn = fn
        op.dma = dma
        op.idx = len(self.ops)
        op.needs_inc = False
        op.count = None
        deps, norm = self._deps(reads, writes)
        for r in reads:
            nm, key = norm(r)
            if nm.startswith("ps"):
                for lst in self.trk[nm]["r"].values():
                    for i in lst[-4:]:
                        if self.ops[i].eng != eng and i not in deps:
                            deps[i] = "RAW"
        for d in self.barrier_deps:
            if d not in deps:
                deps[d] = "RAW"
        op.deps = deps
        for r in reads:
            nm, key = norm(r)
            self.trk[nm]["r"].setdefault(key, []).append(op.idx)
        for w in writes:
            nm, key = norm(w)
            t = self.trk[nm]
            if key is None:
                t["w"] = {None: op.idx}
                t["r"] = {}
            else:
                t["w"][key] = op.idx
                t["r"][key] = []
        if dma:
            n = self.dma_cnt[eng]
            self.dma_cnt[eng] = n + 1
            slot = n % DMA_RING
            op.slot = slot
            prev = self.slot_last.get((eng, slot))
            if prev is not None and prev not in op.deps:
                op.deps[prev] = "RAW"
            self.slot_last[(eng, slot)] = op.idx
            u = self.slot_uses.get((eng, slot), 0) + 1
            self.slot_uses[(eng, slot)] = u
            op.slot_use = u
        else:
            self.last_compute[eng] = op.idx
        self.ops.append(op)
        self.eng_ops[eng].append(op)
        return op

    def barrier(self):
        deps = list(self.last_compute.values()) + list(self.slot_last.values())
        self.barrier_deps = deps

    def emit(self, stack):
        nc = self.nc
        ops = self.ops
        real = {}
        for op in ops:
            lst = []
            for d, kind in op.deps.items():
                src = ops[d]
                if not src.dma and not op.dma and src.eng == op.eng:
                    if op.eng == "pe":
                        continue
                    if kind != "RAW":
                        continue
                lst.append(d)
                if not src.dma:
                    src.needs_inc = True
            real[op.idx] = lst
        for e in self.ENGS:
            c = 0
            for op in self.eng_ops[e]:
                if not op.dma and op.needs_inc:
                    c += 1
                    op.count = c
        sem = {e: stack.enter_context(nc.semaphore(f"s_{e}")) for e in self.ENGS}
        dsem = {}
        for (e, slot) in self.slot_last:
            dsem[(e, slot)] = stack.enter_context(nc.semaphore(f"d_{e}_{slot}"))
        block = stack.enter_context(nc.Block())
        engobj = {"pe": block.tensor, "dve": block.vector, "act": block.scalar,
                  "pool": block.gpsimd, "sp": block.sync}
        nwaits = [0]

        def run_engine(e):
            def body(eng):
                known = {}
                for op in self.eng_ops[e]:
                    need = {}
                    for d in real[op.idx]:
                        src = ops[d]
                        if src.dma:
                            s = dsem[(src.eng, src.slot)]
                            v = 16 * src.slot_use
                        else:
                            s = sem[src.eng]
                            v = src.count
                        k = id(s)
                        if known.get(k, 0) >= v:
                            continue
                        if k not in need or need[k][1] < v:
                            need[k] = (s, v)
                    for k, (s, v) in need.items():
                        eng.wait_ge(s, v)
                        known[k] = v
                        nwaits[0] += 1
                    ins = op.fn(eng)
                    if op.dma:
                        ins.then_inc(dsem[(op.eng, op.slot)], 16)
                    elif op.needs_inc:
                        ins.then_inc(sem[e], 1)
                for (ee, slot), s in dsem.items():
                    if ee == e:
                        v = 16 * self.slot_uses[(ee, slot)]
                        if known.get(id(s), 0) < v:
                            eng.wait_ge(s, v)
            engobj[e](body)

        for e in self.ENGS:
            if self.eng_ops[e]:
                run_engine(e)
        self.nwaits = nwaits[0]

    def dma(self, out, in_, reads, writes, eng="sp", **kw):
        return self.add(eng, lambda e: e.dma_start(out=out, in_=in_, **kw), reads, writes, dma=True)

    def mm(self, out, lhsT, rhs, reads, writes, start=True, stop=True, **kw):
        return self.add("pe", lambda e: e.matmul(out, lhsT, rhs, start=start, stop=stop, **kw), reads, writes)

    def tr(self, out, in_, ident, reads, writes):
        return self.add("pe", lambda e: e.transpose(out, in_, ident), reads, writes)

    def act(self, out, in_, func, reads, writes, **kw):
        return self.add("act", lambda e: e.activation(out, in_, func, **kw), reads, writes)

    def ew(self, eng, name, reads, writes, *a, **kw):
        return self.add(eng, lambda e: getattr(e, name)(*a, **kw), reads, writes)


import numpy as np, math
from contextlib import ExitStack

T = 4352; NT = 34; D = 1024; KC = 8; CTX = 256; L = 4096
EPS = 1e-6
NFEAT = 2192
NL = 2

PER_LAYER = {
 "w_mod": [D, 6144], "b_mod": [6144], "norm1_g": [D], "norm2_g": [D], "w_in": [D, 6544],
 "rw_mu": [2, 896], "rw_w0": [2, 256], "rw_w_up": [2, 32, 256], "rw_a0": [2, 256], "rw_a_up": [2, 32, 256],
 "rw_k_k": [256], "rw_k_a": [256], "rw_r_k": [4, 64], "rw_g_up": [64, 256], "rw_ln_g": [256], "rw_ln_b": [256],
 "dn_conv": [5, 768], "dn_a_log": [2, 4], "dn_dt_bias": [2, 4], "dn_norm_g": [64],
 "s5_lam_re": [2, 16, 64], "s5_lam_im": [2, 16, 64], "s5_log_step": [2, 16],
 "s5_b_re": [2, 16, 64, 16], "s5_b_im": [2, 16, 64, 16], "s5_c_re": [2, 16, 16, 64], "s5_c_im": [2, 16, 16, 64],
 "s5_d": [256], "s5_w_glu": [256, 256], "s5_b_glu": [256], "w_branch": [4, 256, D], "w_out": [D, D],
 "moe_w1": [16, D, 256], "moe_w3": [16, D, 256], "moe_w2": [16, 256, D],
}
SHARED = {"c_ctx": [D], "router_w": [D, 16], "router_b": [16], "final_g": [D]}


class K:
    pass


def build(dbg=(), stop_after=None, nlayers=NL, yT_input=False, mixers=("fn", "s5", "dn", "rw")):
    nc = bass.Bass("TRN2", target_bir_lowering=False)
    P = Prog(nc)
    k = K(); k.P = P; k.nc = nc; k.dbg = set(dbg)
    I = {}
    I["x"] = P.dram("x", [L, D], F32, kind="ExternalInput")
    I["ctx"] = P.dram("ctx", [CTX, D], F32, kind="ExternalInput")
    I["c"] = P.dram("c", [D], F32, kind="ExternalInput")
    for n, s in SHARED.items():
        I[n] = P.dram(n, s, F32, kind="ExternalInput")
    for n, s in PER_LAYER.items():
        I[n] = P.dram(n, [NL] + s, F32, kind="ExternalInput")
    k.I = I
    k.out = P.dram("out", [L, D], F32, kind="ExternalOutput")

    def scratch(name, shape, dtype=F32):
        kind = "ExternalOutput" if name in k.dbg else "Internal"
        return P.dram(name, shape, dtype, kind=kind)
    k.scratch = scratch
    k.xres = scratch("xres", [T, D])
    k.mods = [scratch(f"mods{l}", [2, 6144]) for l in range(NL)]
    k.featT = scratch("featT", [NFEAT, T])
    k.projfn = scratch("projfn", [T, 256])
    k.yT = P.dram("yT", [1024, T], BF16, kind="ExternalInput") if yT_input else scratch("yT", [1024, T], BF16)
    k.yT_input = yT_input
    k.mixers = set(mixers)
    alloc_dplr_scratch(k)
    k.mTd = scratch("mTd", [1024, T], BF16)
    k.hTd = scratch("hTd", [1024, T], BF16)
    k.ps = []
    for i in range(8):
        h = nc.alloc_psum_tensor(f"ps{i}", [128, 512], F32)
        k.ps.append(Buf(f"ps{i}", h, h[:, :]))
    k.ones_b = P.sbuf("ones_b", [128, 128], BF16)
    k.ident_b = P.sbuf("ident_b", [128, 128], BF16)
    k.ones_r = P.sbuf("ones_r", [128, 128], mybir.dt.float32r)
    k.ones_f = P.sbuf("ones_f", [128, 128], F32)
    k.ident_f = P.sbuf("ident_f", [128, 128], F32)
    P.ew("pool", "memset", [], [k.ones_b], k.ones_b[:, :], 1.0)
    P.ew("pool", "memset", [], [k.ones_f], k.ones_f[:, :], 1.0)
    P.add("act", lambda e: e.activation(k.ones_r[:, :], k.ones_f[:, :], AF.Copy), [k.ones_f], [k.ones_r])
    P.add("pool", lambda e: e.affine_select(k.ident_b[:, :], k.ones_b[:, :], [[-1, 128]], ALU.is_equal, 0.0, base=0, channel_multiplier=1), [k.ones_b], [k.ident_b])
    P.add("pool", lambda e: e.affine_select(k.ident_f[:, :], k.ones_f[:, :], [[-1, 128]], ALU.is_equal, 0.0, base=0, channel_multiplier=1), [k.ones_f], [k.ident_f])
    gen_consts(k)
    k.const_mark = P.sb_off

    P.dma(k.xres[0:CTX, :], I["ctx"][:, :], [I["ctx"]], [k.xres])
    P.dma(k.xres[CTX:T, :], I["x"][:, :], [I["x"]], [k.xres], eng="pool")

    phase_mods(k)
    if stop_after == "mods":
        return finish(k)
    for l in range(nlayers):
        phase_norm(k, l, which=1)
        for kc in range(KC):
            P.dma(k.hTd[kc * 128:(kc + 1) * 128, :], k.hT[:, kc, :], [k.hT], [(k.hTd, kc)], eng=("sp" if kc % 2 else "pool"))
        phase_inproj(k, l)
        if stop_after == f"inproj{l}":
            return finish(k)
        if not k.yT_input:
            if "fn" in k.mixers:
                phase_fnet(k, l)
            if stop_after == f"fnet{l}":
                return finish(k)
            phase_mixers(k, l)
            if stop_after == f"mix{l}":
                return finish(k)
        phase_load_hT(k, l)
        phase_merge(k, l, final=(l == nlayers - 1))
        if stop_after == f"merge{l}":
            return finish(k)
        phase_norm(k, l, which=2)
        phase_moe(k, l, final=(l == nlayers - 1))
        if stop_after == f"moe{l}":
            return finish(k)
    return finish(k)


def finish(k):
    k.stack = ExitStack()
    k.P.emit(k.stack)
    return k


def phase_mods(k):
    P = k.P; I = k.I
    P.barrier(); P.sb_reset(k.const_mark)
    cT = P.sbuf("cT", [128, 8, 2], F32)
    P.dma(cT[:, :, 0], I["c"].ap.rearrange("(kc p) -> p kc", p=128), [I["c"]], [cT], allow_slow_non_contiguous=True)
    P.dma(cT[:, :, 1], I["c_ctx"].ap.rearrange("(kc p) -> p kc", p=128), [I["c_ctx"]], [cT], allow_slow_non_contiguous=True)
    cS = P.sbuf("cS", [128, 8, 2], F32)
    P.act(cS[:, :, :], cT[:, :, :], AF.Silu, [cT], [cS])
    wts = [P.sbuf(f"wm{i}", [128, 3072], F32) for i in range(2)]
    bm = P.sbuf("bm", [2, 6144], F32)
    msb = P.sbuf("msb", [2, 6144], F32)
    n = 0
    for l in range(NL):
        P.dma(bm[:, :], I["b_mod"].ap[l:l + 1, :].broadcast_to([2, 6144]) if False else I["b_mod"].ap[l, :].partition_broadcast(2), [I["b_mod"]], [bm])
        for half in range(2):
            for kc in range(8):
                wt = wts[n % 2]; n += 1
                P.dma(wt[:, :], I["w_mod"].ap[l, kc * 128:(kc + 1) * 128, half * 3072:(half + 1) * 3072], [I["w_mod"]], [wt], eng=("sp" if n % 2 else "pool"))
                for j in range(6):
                    P.mm(k.ps[j][0:2, :], cS[:, kc, :], wt[:, j * 512:(j + 1) * 512], [cS, wt], [k.ps[j]], start=(kc == 0), stop=(kc == 7))
            for j in range(6):
                o = half * 3072 + j * 512
                P.ew("dve", "tensor_tensor", [k.ps[j], bm], [(msb, o)], msb[:, o:o + 512], k.ps[j][0:2, :], bm[:, o:o + 512], ALU.add)
        P.dma(k.mods[l][:, :], msb[:, :], [msb], [k.mods[l]])


def load_mod_tiles(k, l, which):
    P = k.P; I = k.I
    gname = "norm1_g" if which == 1 else "norm2_g"
    so = 0 if which == 1 else 3072
    gb = P.sbuf("gb", [128, D], F32)
    P.dma(gb[:, :], I[gname].ap[l, :].partition_broadcast(128), [I[gname]], [gb])
    res = []
    for row in range(2):
        sc = P.sbuf(f"sc{row}", [128, D], F32)
        sh = P.sbuf(f"sh{row}", [128, D], F32)
        P.dma(sc[:, :], k.mods[l].ap[row, so + 1024:so + 2048].partition_broadcast(128), [k.mods[l]], [sc], eng="pool")
        P.dma(sh[:, :], k.mods[l].ap[row, so:so + 1024].partition_broadcast(128), [k.mods[l]], [sh])
        P.ew("dve", "scalar_tensor_tensor", [sc, gb], [sc], sc[:, :], sc[:, :], 1.0, gb[:, :], ALU.add, ALU.mult)
        res.append((sc, sh))
    return res


def phase_load_hT(k, l):
    P = k.P
    P.barrier(); P.sb_reset(k.const_mark)
    k.hT = P.sbuf("hT", [128, KC, T], BF16)
    k.norm_mark = P.sb_off
    for kc in range(KC):
        P.dma(k.hT[:, kc, :], k.hTd[kc * 128:(kc + 1) * 128, :], [k.hTd], [(k.hT, ("ld", kc))], eng=("sp" if kc % 2 else "pool"))


def phase_norm(k, l, which):
    P = k.P
    P.barrier(); P.sb_reset(k.const_mark)
    k.hT = P.sbuf("hT", [128, KC, T], BF16)
    if which == 2:
        k.comb = P.sbuf("comb", [128, NT, 16], F32)
    k.norm_mark = P.sb_off
    (A_l, sh_l), (A_c, sh_c) = load_mod_tiles(k, l, which)
    NB = 4
    xts = [P.sbuf(f"xt{i}", [128, D], F32) for i in range(NB)]
    hfs = [P.sbuf(f"hf{i}", [128, D], F32) for i in range(NB)]
    hbs = [P.sbuf(f"hb{i}", [128, D], BF16) for i in range(NB)]
    junk = P.sbuf("junk", [128, D], F32)
    ss = [P.sbuf(f"ss{i}", [128, 1], F32) for i in range(NB)]
    rs = [P.sbuf(f"rs{i}", [128, 1], F32) for i in range(NB)]
    epsb = P.sbuf("epsb", [128, 1], F32)
    P.ew("pool", "memset", [], [epsb], epsb[:, :], EPS)
    if which == 2:
        router_setup(k)
    for i in range(NT):
        xt = xts[i % NB]; hf = hfs[i % NB]; hb = hbs[i % NB]; s = ss[i % NB]; r = rs[i % NB]
        A, sh = (A_c, sh_c) if i < 2 else (A_l, sh_l)
        P.dma(xt[:, :], k.xres[i * 128:(i + 1) * 128, :], [(k.xres, i)], [xt], eng=("sp" if i % 2 else "pool"))
        P.add("act", lambda e, xt=xt, s=s: e.activation(junk[:, :], xt[:, :], AF.Square, accum_out=s[:, :]), [xt], [junk, s])
        P.add("act", lambda e, s=s, r=r: e.activation(r[:, :], s[:, :], AF.Sqrt, scale=1.0 / D, bias=epsb[:, :]), [s, epsb], [r])
        P.ew("dve", "reciprocal", [r], [r], r[:, :], r[:, :])
        P.ew("dve", "scalar_tensor_tensor", [xt, r, A], [hf], hf[:, :], xt[:, :], r[:, 0:1], A[:, :], ALU.mult, ALU.mult)
        P.ew("dve", "tensor_tensor", [hf, sh], [hf], hf[:, :], hf[:, :], sh[:, :], ALU.add)
        ACT(P, hb[:, :], hf[:, :], AF.Copy, [hf], [hb])
        if which == 2:
            router_tile(k, i, hf)
        pst = k.ps[i % 2]
        pv = pst.ap.bitcast(BF16)
        for kc in range(KC):
            P.tr(pv[:, kc * 128:(kc + 1) * 128], hb[:, kc * 128:(kc + 1) * 128], k.ident_b[:, :], [hb, k.ident_b], [pst])
        P.add("act", lambda e, pv=pv, i=i: e.activation(k.hT[:, :, i * 128:(i + 1) * 128], pv.rearrange("p (a b) -> p a b", a=KC), AF.Copy), [pst], [(k.hT, i)])
    if which == 2:
        router_batch(k)


FCH = [(256 + 128 * j, 128) for j in range(7)] + [(1152 + 128 * j, 128) for j in range(6)] + \
      [(1920, 128), (2048, 128), (2176, 16), (2192, 128), (2320, 128)]


def phase_inproj(k, l):
    P = k.P; I = k.I
    P.barrier(); P.sb_reset(k.norm_mark)
    wb = P.sbuf("wb", [128, KC, 2448], BF16)
    stg = [P.sbuf(f"wstg{i}", [128, 2448], F32) for i in range(2)]
    for kc in range(KC):
        st = stg[kc % 2]
        P.dma(st[:, :], I["w_in"].ap[l, kc * 128:(kc + 1) * 128, 0:2448], [I["w_in"]], [st], eng=("sp" if kc % 2 else "pool"))
        if kc % 2:
            P.ew("dve", "tensor_copy", [st], [(wb, kc)], wb[:, kc, :], st[:, :])
        else:
            P.add("act", lambda e, st=st, kc=kc: e.activation(wb[:, kc, :], st[:, :], AF.Copy), [st], [(wb, kc)])
    P.sb_reset(stg[0].sb_off if hasattr(stg[0], "sb_off") else P.sb_off)
    fo = [P.sbuf(f"fo{i}", [128, 256], F32) for i in range(2)]
    for i in range(NT):
        pst = k.ps[i % 2]
        for kc in range(KC):
            P.mm(pst[:, 0:256], k.hT[:, kc, i * 128:(i + 1) * 128], wb[:, kc, 0:256], [(k.hT, i), wb], [pst], start=(kc == 0), stop=(kc == KC - 1))
        f = fo[i % 2]
        P.add("act", lambda e, f=f, pst=pst: e.activation(f[:, :], pst[:, 0:256], AF.Copy), [pst], [f])
        P.dma(k.projfn[i * 128:(i + 1) * 128, :], f[:, :], [f], [(k.projfn, i)])
    so = [P.sbuf(f"so{i}", [128, T], F32) for i in range(2)]
    n = 0
    for ci, (c0, ncol) in enumerate(FCH):
        s = so[ci % 2]
        for b in range(9):
            t0 = b * 512; tn = min(512, T - t0)
            pst = k.ps[2 + (n % 4)]; n += 1
            for kc in range(KC):
                P.mm(pst[0:ncol, 0:tn], wb[:, kc, c0:c0 + ncol], k.hT[:, kc, t0:t0 + tn], [wb, k.hT], [pst], start=(kc == 0), stop=(kc == KC - 1))
            if n % 2:
                P.ew("dve", "tensor_copy", [pst], [(s, b)], s[0:ncol, t0:t0 + tn], pst[0:ncol, 0:tn])
            else:
                P.add("act", lambda e, s=s, pst=pst, ncol=ncol, t0=t0, tn=tn: e.activation(s[0:ncol, t0:t0 + tn], pst[0:ncol, 0:tn], AF.Copy), [pst], [(s, b)])
        P.dma(k.featT[c0 - 256:c0 - 256 + ncol, :], s[0:ncol, :], [s], [(k.featT, ci)], eng=("sp" if ci % 2 else "pool"))


def TT(P, eng, out, a, b, op, reads, writes):
    return P.add(eng, lambda e: e.tensor_tensor(out, a, b, op), reads, writes)


def TS(P, eng, out, a, s1, s2, op0, op1, reads, writes):
    if s2 is None:
        return P.add(eng, lambda e: e.tensor_scalar(out, a, s1, None, op0), reads, writes)
    return P.add(eng, lambda e: e.tensor_scalar(out, a, s1, s2, op0, op1), reads, writes)


def STT(P, eng, out, a, s, b, op0, op1, reads, writes):
    return P.add(eng, lambda e: e.scalar_tensor_tensor(out, a, s, b, op0, op1), reads, writes)


def ACT(P, out, in_, func, reads, writes, **kw):
    return P.add("act", lambda e: e.activation(out, in_, func, **kw), reads, writes)


def gen_consts(k):
    P = k.P
    k.jrow_i = P.sbuf("jrow_i", [128, 128], I32)
    P.add("pool", lambda e: e.iota(k.jrow_i[:, :], [[1, 128]], base=0, channel_multiplier=0), [], [k.jrow_i])
    ti = P.sbuf("ti", [128, 32], I32)
    k.lcols = P.sbuf("lcols", [128, 32], F32)
    P.add("pool", lambda e: e.iota(ti[:, 0:32], [[128, 32]], base=0, channel_multiplier=1), [], [ti])
    P.ew("dve", "tensor_copy", [ti], [k.lcols], k.lcols[:, :], ti[:, 0:32])
    k.negpi = P.sbuf("negpi", [128, 1], F32)
    P.ew("pool", "memset", [], [k.negpi], k.negpi[:, :], -math.pi)
    k.halfpi = P.sbuf("halfpi", [128, 1], F32)
    P.ew("pool", "memset", [], [k.halfpi], k.halfpi[:, :], math.pi / 2)
    pmi = P.sbuf("pmi", [128, 1], I32)
    P.add("pool", lambda e: e.iota(pmi[:, :], [[1, 1]], base=0, channel_multiplier=1), [], [pmi])
    TS(P, "dve", pmi[:, :], pmi[:, :], 63, None, ALU.bitwise_and, None, [pmi], [pmi])
    pm = P.sbuf("pm", [128, 1], F32)
    P.ew("dve", "tensor_copy", [pmi], [pm], pm[:, :], pmi[:, :])
    v = P.sbuf("v64", [128, 128], I32)
    TS(P, "dve", v[:, :], k.jrow_i[:, 0:128], 63, None, ALU.bitwise_and, None, [k.jrow_i], [v])
    TS(P, "dve", v[:, :], v[:, :], pm[:, 0:1], None, ALU.mult, None, [v, pm], [v])
    TS(P, "dve", v[:, :], v[:, :], 63, None, ALU.bitwise_and, None, [v], [v])
    w = P.sbuf("w64", [128, 128], I32)
    TS(P, "dve", w[:, :], v[:, :], 16, None, ALU.add, None, [v], [w])
    TS(P, "dve", w[:, :], w[:, :], 63, None, ALU.bitwise_and, None, [w], [w])
    bmk = P.sbuf("bmk", [128, 128], F32)
    P.ew("pool", "memset", [], [bmk], bmk[:, :], 0.0)
    P.ew("pool", "memset", [bmk], [bmk], bmk[0:64, 0:64], 1.0)
    P.ew("pool", "memset", [bmk], [bmk], bmk[64:128, 64:128], 1.0)
    k.blk64 = bmk
    sn = P.sbuf("sn64", [128, 128], F32)
    cn = P.sbuf("cn64", [128, 128], F32)
    ACT(P, sn[:, :], v[:, :], AF.Sin, [v, k.negpi], [sn], scale=2 * math.pi / 64, bias=k.negpi[:, :])
    ACT(P, cn[:, :], w[:, :], AF.Sin, [w, k.negpi], [cn], scale=2 * math.pi / 64, bias=k.negpi[:, :])
    k.Cn64 = P.sbuf("Cn64", [128, 128], BF16)
    k.S64 = P.sbuf("S64", [128, 128], BF16)
    TT(P, "dve", k.Cn64[:, :], cn[:, :], bmk[:, :], ALU.mult, [cn, bmk], [k.Cn64])
    STT(P, "dve", k.S64[:, :], sn[:, :], -1.0, bmk[:, :], ALU.mult, ALU.mult, [sn, bmk], [k.S64])
    k.blk64r = P.sbuf("blk64r", [128, 128], mybir.dt.float32r)
    P.add("act", lambda e: e.activation(k.blk64r[:, :], bmk[:, :], AF.Copy), [bmk], [k.blk64r])
    k.blk64b = P.sbuf("blk64b", [128, 128], BF16)
    P.ew("dve", "tensor_copy", [bmk], [k.blk64b], k.blk64b[:, :], bmk[:, :])


def phase_fnet(k, l):
    P = k.P
    P.barrier(); P.sb_reset(k.const_mark)
    jr = P.sbuf("jrow_big", [128, 4096], I32)
    P.add("pool", lambda e: e.iota(jr[:, :], [[1, 4096]], base=0, channel_multiplier=0), [], [jr])
    fmark = P.sb_off
    for (t0, Ls) in ((0, CTX), (CTX, L)):
        P.sb_reset(fmark)
        nt = Ls // 128
        KQ = min(1024, Ls)
        nb = KQ // 512 if KQ >= 512 else 1
        bw = min(512, KQ)
        uf = P.sbuf("uf", [128, nt, 256], F32)
        ub = P.sbuf("ub", [128, nt, 256], BF16)
        P.dma(uf[:, :, :], k.projfn.ap[t0:t0 + Ls, :].rearrange("(n p) c -> p n c", p=128), [k.projfn], [uf])
        P.ew("dve", "tensor_copy", [uf], [ub], ub[:, :, :], uf[:, :, :])
        Z = P.sbuf("Z", [128, 2, 2, Ls], BF16)
        vs = [P.sbuf(f"fv{i}", [128, KQ], I32) for i in range(2)]
        ws = [P.sbuf(f"fw{i}", [128, KQ], I32) for i in range(2)]
        sns = [P.sbuf(f"fsn{i}", [128, KQ], BF16) for i in range(2)]
        cns = [P.sbuf(f"fcn{i}", [128, KQ], BF16) for i in range(2)]
        n = 0
        for kq in range(Ls // KQ):
            for lc in range(nt):
                v = vs[n % 2]; w = ws[n % 2]; sn = sns[n % 2]; cn = cns[n % 2]; n += 1
                TS(P, "dve", v[:, :], jr[:, kq * KQ:(kq + 1) * KQ], k.lcols[:, lc:lc + 1], None, ALU.mult, None, [jr, k.lcols], [v])
                TS(P, "dve", v[:, :], v[:, :], Ls - 1, None, ALU.bitwise_and, None, [v], [v])
                TS(P, "dve", w[:, :], v[:, :], Ls // 4, None, ALU.add, None, [v], [w])
                TS(P, "dve", w[:, :], w[:, :], Ls - 1, None, ALU.bitwise_and, None, [w], [w])
                ACT(P, sn[:, :], v[:, :], AF.Sin, [v, k.negpi], [sn], scale=2 * math.pi / Ls, bias=k.negpi[:, :])
                ACT(P, cn[:, :], w[:, :], AF.Sin, [w, k.negpi], [cn], scale=2 * math.pi / Ls, bias=k.negpi[:, :])
                for h in range(2):
                    for ti_, trig in enumerate((cn, sn)):
                        for b in range(nb):
                            pst = k.ps[(h * 2 + ti_) * nb + b]
                            P.mm(pst[:, 0:bw], ub[:, lc, h * 128:(h + 1) * 128], trig[:, b * bw:(b + 1) * bw], [ub, trig], [pst], start=(lc == 0), stop=(lc == nt - 1))
            for h in range(2):
                for ti_ in range(2):
                    for b in range(nb):
                        pst = k.ps[(h * 2 + ti_) * nb + b]
                        o = kq * KQ + b * bw
                        if (h + ti_ + b) % 2:
                            P.ew("dve", "tensor_copy", [pst], [(Z, (h, ti_, o))], Z[:, h, ti_, o:o + bw], pst[:, 0:bw])
                        else:
                            ACT(P, Z[:, h, ti_, o:o + bw], pst[:, 0:bw], AF.Copy, [pst], [(Z, (h, ti_, o))])
        yst = P.sbuf("yst", [128, 2, Ls], BF16)
        scale = 1.0 / math.sqrt(Ls * 64.0)
        n = 0
        for h in range(2):
            for b in range(max(1, Ls // 512)):
                o = b * 512; bw2 = min(512, Ls)
                pst = k.ps[n % 8]; n += 1
                P.mm(pst[:, 0:bw2], k.Cn64[:, :], Z[:, h, 0, o:o + bw2], [k.Cn64, Z], [pst], start=True, stop=False)
                P.mm(pst[:, 0:bw2], k.S64[:, :], Z[:, h, 1, o:o + bw2], [k.S64, Z], [pst], start=False, stop=True)
                ACT(P, yst[:, h, o:o + bw2], pst[:, 0:bw2], AF.Copy, [pst], [(yst, (h, b))], scale=scale)
        for h in range(2):
            P.dma(k.yT[h * 128:(h + 1) * 128, t0:t0 + Ls], yst[:, h, :], [yst], [(k.yT, ("fn", h, t0))])


def router_setup(k):
    P = k.P; I = k.I
    k.rw = P.sbuf("rw", [128, KC, 16], F32)
    P.dma(k.rw[:, :, :], I["router_w"].ap.rearrange("(kc p) e -> p kc e", p=128), [I["router_w"]], [k.rw])
    k.rb = P.sbuf("rb", [128, 16], F32)
    P.dma(k.rb[:, :], I["router_b"].ap.partition_broadcast(128), [I["router_b"]], [k.rb])
    k.hTf = [P.sbuf(f"hTf{i}", [128, KC, 128], F32) for i in range(2)]
    k.sc_all = P.sbuf("sc_all", [128, NT, 16], F32)


def router_tile(k, i, hf):
    P = k.P
    hTf = k.hTf[i % 2]
    for half in range(2):
        pst = k.ps[2 + half]
        for q in range(4):
            kc = half * 4 + q
            P.tr(pst[:, q * 128:(q + 1) * 128], hf[:, kc * 128:(kc + 1) * 128], k.ident_f[:, :], [hf, k.ident_f], [pst])
        ACT(P, hTf[:, half * 4:(half + 1) * 4, :], pst.ap.rearrange("p (a b) -> p a b", a=4), AF.Copy, [pst], [(hTf, half)])
    pl = k.ps[4 + (i % 2)]
    for kc in range(KC):
        P.mm(pl[:, 0:16], hTf[:, kc, :], k.rw[:, kc, :], [hTf, k.rw], [pl], start=(kc == 0), stop=(kc == KC - 1))
    ACT(P, k.sc_all[:, i, :], pl[:, 0:16], AF.Sigmoid, [pl], [(k.sc_all, i)])


def router_batch(k):
    P = k.P
    BIG = 1.0e4
    N = NT
    def tl(name, w):
        return P.sbuf("rb_" + name, [128, N * w], F32)
    sc = k.sc_all
    sel = tl("sel", 16); eq = tl("eq", 16); s2 = tl("s2", 16); msk = tl("msk", 16); m2t = tl("m2t", 16); e1 = tl("e1", 16); e2 = tl("e2", 16); ws = tl("ws", 16)
    m1 = tl("m1", 4); m2 = tl("m2", 4); gs = tl("gs", 4); ing = tl("ing", 4)
    gm = tl("gm", 1); t1 = tl("t1", 1); t2 = tl("t2", 1); nrm = tl("nrm", 1)
    scf = sc.ap.rearrange("p n e -> p (n e)")
    v_ne = lambda t: t.ap.rearrange("p (n e) -> p n e", e=16)
    v_ge = lambda t: t.ap.rearrange("p (g e) -> p g e", e=4)
    v_ng = lambda t: t.ap.rearrange("p (n g) -> p n g", g=4)
    TT(P, "dve", v_ne(sel), sc.ap, k.rb.ap.unsqueeze(1).broadcast_to([128, N, 16]), ALU.add, [sc, k.rb], [sel])
    P.add("dve", lambda e: e.tensor_reduce(m1.ap, v_ge(sel), AX.X, ALU.max), [sel], [m1])
    TT(P, "dve", v_ge(eq), v_ge(sel), m1.ap.unsqueeze(2).broadcast_to([128, N * 4, 4]), ALU.is_equal, [sel, m1], [eq])
    STT(P, "dve", s2.ap, eq.ap, -BIG, sel.ap, ALU.mult, ALU.add, [eq, sel], [s2])
    P.add("dve", lambda e: e.tensor_reduce(m2.ap, v_ge(s2), AX.X, ALU.max), [s2], [m2])
    TT(P, "dve", gs.ap, m1.ap, m2.ap, ALU.add, [m1, m2], [gs])
    P.add("dve", lambda e: e.tensor_reduce(gm.ap, v_ng(gs), AX.X, ALU.max), [gs], [gm])
    TT(P, "dve", v_ng(ing), v_ng(gs), gm.ap.unsqueeze(2).broadcast_to([128, N, 4]), ALU.is_equal, [gs, gm], [ing])
    TS(P, "dve", ing.ap, ing.ap, -1.0, BIG, ALU.add, ALU.mult, [ing], [ing])
    TT(P, "dve", v_ge(msk), v_ge(sel), ing.ap.unsqueeze(2).broadcast_to([128, N * 4, 4]), ALU.add, [sel, ing], [msk])
    P.add("dve", lambda e: e.tensor_reduce(t1.ap, v_ne(msk), AX.X, ALU.max), [msk], [t1])
    TT(P, "dve", v_ne(e1), v_ne(msk), t1.ap.unsqueeze(2).broadcast_to([128, N, 16]), ALU.is_equal, [msk, t1], [e1])
    STT(P, "dve", m2t.ap, e1.ap, -BIG, msk.ap, ALU.mult, ALU.add, [e1, msk], [m2t])
    P.add("dve", lambda e: e.tensor_reduce(t2.ap, v_ne(m2t), AX.X, ALU.max), [m2t], [t2])
    TT(P, "dve", v_ne(e2), v_ne(m2t), t2.ap.unsqueeze(2).broadcast_to([128, N, 16]), ALU.is_equal, [m2t, t2], [e2])
    TT(P, "dve", e1.ap, e1.ap, e2.ap, ALU.add, [e1, e2], [e1])
    TT(P, "dve", ws.ap, e1.ap, scf, ALU.mult, [e1, sc], [ws])
    P.add("dve", lambda e: e.tensor_reduce(nrm.ap, v_ne(ws), AX.X, ALU.add), [ws], [nrm])
    P.ew("dve", "reciprocal", [nrm], [nrm], nrm.ap, nrm.ap)
    TT(P, "dve", k.comb.ap, v_ne(ws), nrm.ap.unsqueeze(2).broadcast_to([128, N, 16]), ALU.mult, [ws, nrm], [k.comb])


def phase_merge(k, l, final=False):
    P = k.P; I = k.I
    P.barrier(); P.sb_reset(k.norm_mark)
    mTd = k.mTd
    wbr = P.sbuf("wbr", [128, 4, 2, D], BF16)
    wbfs = [P.sbuf(f"wbf{i}", [128, 2, D], F32) for i in range(2)]
    for i in range(4):
        P.dma(wbfs[i % 2][:, :, :], I["w_branch"].ap[l, i].rearrange("(c p) n -> p c n", p=128), [I["w_branch"]], [wbfs[i % 2]])
        P.ew("pool", "tensor_copy", [wbfs[i % 2]], [(wbr, i)], wbr[:, i, :, :], wbfs[i % 2][:, :, :])
    mark = k.norm_mark
    mst = [P.sbuf(f"mst{i}", [128, 512], BF16) for i in range(2)]
    wgf = [P.sbuf(f"wgf{i}", [128, KC, 4, 128], F32) for i in range(1)]
    wgb = [P.sbuf(f"wgb{i}", [128, KC, 4, 128], BF16) for i in range(2)]
    yb = [P.sbuf(f"yb{i}", [128, 8, 512], BF16) for i in range(2)]
    sg = [P.sbuf(f"sg{i}", [128, 512], F32) for i in range(2)]
    tmp = [P.sbuf(f"mtmp{i}", [128, 512], F32) for i in range(2)]
    acc = [P.sbuf(f"macc{i}", [128, 512], F32) for i in range(2)]
    n = 0; nb = 0
    for oc in range(8):
        wf = wgf[0]; wg = wgb[oc % 2]
        src = I["w_in"].ap[l, :, 2448:6544].rearrange("(kc p) (i n) -> p kc i n", p=128, i=4)[:, :, :, oc * 128:(oc + 1) * 128]
        for kc in range(KC):
            P.dma(wf[:, kc, :, :], src[:, kc, :, :], [I["w_in"]], [(wf, kc)], eng=("sp" if kc % 2 else "pool"))
        ACT(P, wg[:, :, :, :], wf[:, :, :, :], AF.Copy, [wf], [wg])
        for tb in range(9):
            t0 = tb * 512; tn = min(512, T - t0)
            y = yb[nb % 2]; a = acc[nb % 2]; nb += 1
            P.dma(y[:, :, 0:tn], k.yT.ap[:, t0:t0 + tn].rearrange("(j p) t -> p j t", p=128), [k.yT], [y])
            for i in range(4):
                pm = k.ps[(n % 2) * 2]; pg = k.ps[(n % 2) * 2 + 1]; s_ = sg[n % 2]; tm = tmp[n % 2]; n += 1
                for c2 in range(2):
                    P.mm(pm[:, 0:tn], wbr[:, i, c2, oc * 128:(oc + 1) * 128], y[:, i * 2 + c2, 0:tn], [wbr, y], [pm], start=(c2 == 0), stop=(c2 == 1))
                for kc in range(KC):
                    P.mm(pg[:, 0:tn], wg[:, kc, i, :], k.hT[:, kc, t0:t0 + tn], [wg, k.hT], [pg], start=(kc == 0), stop=(kc == KC - 1))
                ACT(P, s_[:, 0:tn], pg[:, 0:tn], AF.Sigmoid, [pg], [s_])
                if i == 0:
                    TT(P, "dve", a[:, 0:tn], s_[:, 0:tn], pm[:, 0:tn], ALU.mult, [s_, pm], [a])
                else:
                    TT(P, "dve", tm[:, 0:tn], s_[:, 0:tn], pm[:, 0:tn], ALU.mult, [s_, pm], [tm])
                    TT(P, "dve", a[:, 0:tn], a[:, 0:tn], tm[:, 0:tn], ALU.add, [a, tm], [a])
            ms = mst[nb % 2]
            ACT(P, ms[:, 0:tn], a[:, 0:tn], AF.Copy, [a], [ms])
            P.dma(mTd[oc * 128:(oc + 1) * 128, t0:t0 + tn], ms[:, 0:tn], [ms], [(mTd, (oc, tb))])
    P.barrier(); P.sb_reset(mark)
    wof = P.sbuf("wof", [128, KC, D], F32)
    wo = P.sbuf("wo", [128, KC, D], BF16)
    P.dma(wof[:, :, :], I["w_out"].ap[l].rearrange("(kc p) n -> p kc n", p=128), [I["w_out"]], [wof])
    P.ew("pool", "tensor_copy", [wof], [wo], wo[:, :, :], wof[:, :, :])
    P.sb_reset(wof_off(P, wof))
    g1 = []
    for row in range(2):
        g = P.sbuf(f"g1_{row}", [128, D], F32)
        P.dma(g[:, :], k.mods[l].ap[row, 2048:3072].partition_broadcast(128), [k.mods[l]], [g])
        g1.append(g)
    xts = [P.sbuf(f"mx{i}", [128, D], F32) for i in range(2)]
    tms = [P.sbuf(f"mt{i}", [128, D], F32) for i in range(2)]
    mTs = [P.sbuf(f"mTs{i}", [128, KC, 128], BF16) for i in range(2)]
    for i in range(2 if final else 0, NT):
        xt = xts[i % 2]; tm = tms[i % 2]; g = g1[1] if i < 2 else g1[0]
        mT = mTs[i % 2]
        P.dma(mT[:, :, :], mTd.ap[:, i * 128:(i + 1) * 128].rearrange("(kc p) t -> p kc t", p=128), [mTd], [mT])
        P.dma(xt[:, :], k.xres[i * 128:(i + 1) * 128, :], [(k.xres, i)], [xt])
        for nh in range(2):
            pst = k.ps[(i % 2) * 2 + nh]
            for kc in range(KC):
                P.mm(pst[:, :], mT[:, kc, :], wo[:, kc, nh * 512:(nh + 1) * 512], [mT, wo], [pst], start=(kc == 0), stop=(kc == KC - 1))
            TT(P, "dve", tm[:, nh * 512:(nh + 1) * 512], pst[:, :], g[:, nh * 512:(nh + 1) * 512], ALU.mult, [pst, g], [(tm, nh)])
        TT(P, "dve", xt[:, :], xt[:, :], tm[:, :], ALU.add, [xt, tm], [xt])
        P.dma(k.xres[i * 128:(i + 1) * 128, :], xt[:, :], [xt], [(k.xres, i)], eng="pool")


def wof_off(P, buf):
    return P.sb_off


def phase_moe(k, l, final):
    P = k.P; I = k.I
    P.barrier(); P.sb_reset(k.norm_mark)
    g2 = []
    for row in range(2):
        g = P.sbuf(f"g2_{row}", [128, D], F32)
        P.dma(g[:, :], k.mods[l].ap[row, 5120:6144].partition_broadcast(128), [k.mods[l]], [g])
        g2.append(g)
    if final:
        fg = P.sbuf("fg", [128, D], F32)
        P.dma(fg[:, :], I["final_g"].ap.partition_broadcast(128), [I["final_g"]], [fg])
        epsb = P.sbuf("epsb2", [128, 1], F32)
        P.ew("pool", "memset", [], [epsb], epsb[:, :], EPS)
    yacc = P.sbuf("yacc", [128, 7, D], F32)
    w13f = [P.sbuf(f"w13f{i}", [128, 2, KC, 256], F32) for i in range(1)]
    w2f = [P.sbuf(f"w2f{i}", [128, 2, D], F32) for i in range(1)]
    w13 = [P.sbuf(f"w13b{i}", [128, 2, KC, 256], BF16) for i in range(2)]
    w2 = [P.sbuf(f"w2b{i}", [128, 2, D], BF16) for i in range(2)]
    xts = [P.sbuf(f"ox{i}", [128, D], F32) for i in range(2)]
    tms = [P.sbuf(f"ot{i}", [128, D], F32) for i in range(2)]
    junk = P.sbuf("ojunk", [128, D], F32)
    ss = [P.sbuf(f"oss{i}", [128, 1], F32) for i in range(2)]
    sbs = [(2, 7), (9, 7), (16, 6), (22, 6), (28, 6)] if final else [(0, 7), (7, 7), (14, 7), (21, 7), (28, 6)]
    a1 = [P.sbuf(f"a1w_{i}", [128, 2, 512], F32) for i in range(2)]
    ab = [P.sbuf(f"abw_{i}", [128, 2, 512], BF16) for i in range(2)]
    ne = 0; nw = 0; orr = 0
    for (ts0, nts) in sbs:
        pending = None

        def emit_out(item):
            nonlocal orr
            e_, w2b_, ab_, j_, nt_ = item
            for tt_ in range(nt_):
                til = j_ + tt_
                gi = ts0 + til
                for nh in range(2):
                    po = k.ps[4 + (orr % 4)]; orr += 1
                    for dh in range(2):
                        P.mm(po[:, :], ab_[:, dh, tt_ * 128:(tt_ + 1) * 128], w2b_[:, dh, nh * 512:(nh + 1) * 512], [ab_, w2b_], [po], start=(dh == 0), stop=(dh == 1))
                    dst = yacc[:, til, nh * 512:(nh + 1) * 512]
                    if e_ == 0:
                        TS(P, "dve", dst, po[:, :], k.comb[:, gi, e_:e_ + 1], None, ALU.mult, None, [po, (k.comb, gi)], [(yacc, (til, nh))])
                    else:
                        STT(P, "dve", dst, po[:, :], k.comb[:, gi, e_:e_ + 1], dst, ALU.mult, ALU.add, [po, (k.comb, gi), (yacc, (til, nh))], [(yacc, (til, nh))])
        for e in range(16):
            wf = w13f[0]; w2f_ = w2f[0]; wb = w13[ne % 2]; w2b = w2[ne % 2]; ne += 1
            P.dma(wf[:, 0, :, :], I["moe_w1"].ap[l, e].rearrange("(kc p) n -> p kc n", p=128), [I["moe_w1"]], [(wf, 0)])
            P.dma(wf[:, 1, :, :], I["moe_w3"].ap[l, e].rearrange("(kc p) n -> p kc n", p=128), [I["moe_w3"]], [(wf, 1)], eng="pool")
            P.dma(w2f_[:, :, :], I["moe_w2"].ap[l, e].rearrange("(c p) n -> p c n", p=128), [I["moe_w2"]], [w2f_])
            ACT(P, wb[:, 0, :, :], wf[:, 0, :, :], AF.Copy, [(wf, 0)], [(wb, 0)])
            P.ew("pool", "tensor_copy", [(wf, 1)], [(wb, 1)], wb[:, 1, :, :], wf[:, 1, :, :])
            P.ew("dve", "tensor_copy", [w2f_], [w2b], w2b[:, :, :], w2f_[:, :, :])
            j = 0
            while j < nts:
                nt4 = min(4, nts - j)
                tok0 = (ts0 + j) * 128; ntok = nt4 * 128
                a1_ = a1[nw % 2]; ab_ = ab[nw % 2]; nw += 1
                pbanks = [[k.ps[0], k.ps[1]], [k.ps[2], k.ps[3]]]
                for which in range(2):
                    for dh in range(2):
                        pp = pbanks[which][dh]
                        for kc in range(KC):
                            P.mm(pp[:, 0:ntok], wb[:, which, kc, dh * 128:(dh + 1) * 128], k.hT[:, kc, tok0:tok0 + ntok], [wb, k.hT], [pp], start=(kc == 0), stop=(kc == KC - 1))
                for dh in range(2):
                    ACT(P, a1_[:, dh, 0:ntok], pbanks[0][dh][:, 0:ntok], AF.Silu, [pbanks[0][dh]], [(a1_, dh)])
                    TT(P, "dve", ab_[:, dh, 0:ntok], a1_[:, dh, 0:ntok], pbanks[1][dh][:, 0:ntok], ALU.mult, [(a1_, dh), pbanks[1][dh]], [(ab_, dh)])
                if pending is not None:
                    emit_out(pending)
                pending = (e, w2b, ab_, j, nt4)
                j += nt4
        if pending is not None:
            emit_out(pending)
            pending = None
        for til in range(nts):
            gi = ts0 + til
            xt = xts[gi % 2]; tm = tms[gi % 2]; g = g2[1] if gi < 2 else g2[0]
            if final and gi < 2:
                continue
            P.dma(xt[:, :], k.xres[gi * 128:(gi + 1) * 128, :], [(k.xres, gi)], [xt])
            TT(P, "pool", tm[:, :], yacc[:, til, :], g[:, :], ALU.mult, [yacc, g], [tm])
            TT(P, "pool", xt[:, :], xt[:, :], tm[:, :], ALU.add, [xt, tm], [xt])
            if not final:
                P.dma(k.xres[gi * 128:(gi + 1) * 128, :], xt[:, :], [xt], [(k.xres, gi)], eng="pool")
            else:
                s = ss[gi % 2]
                P.add("act", lambda e_, xt=xt, s=s: e_.activation(junk[:, :], xt[:, :], AF.Square, accum_out=s[:, :]), [xt], [junk, s])
                P.add("act", lambda e_, s=s: e_.activation(s[:, :], s[:, :], AF.Sqrt, scale=1.0 / D, bias=epsb[:, :]), [s, epsb], [s])
                P.ew("dve", "reciprocal", [s], [s], s[:, :], s[:, :])
                STT(P, "dve", tm[:, :], xt[:, :], s[:, 0:1], fg[:, :], ALU.mult, ALU.mult, [xt, s, fg], [tm])
                P.dma(k.out[(gi - 2) * 128:(gi - 1) * 128, :], tm[:, :], [tm], [(k.out, gi)], eng="pool")


def phase_mixers(k, l):
    phase_zero_missing(k, l)
    if "s5" in k.mixers:
        phase_s5(k, l)
    if "dn" in k.mixers and "rw" in k.mixers:
        phase_rw_dn(k, l)
    elif "dn" in k.mixers:
        phase_dn(k, l)
    elif "rw" in k.mixers:
        phase_rw(k, l)


def phase_zero_missing(k, l):
    P = k.P
    P.barrier(); P.sb_reset(k.const_mark)
    z = P.sbuf("zfill", [128, T], BF16)
    P.ew("pool", "memset", [], [z], z[:, :], 0.0)
    for j in range(2, 6):
        P.dma(k.yT[j * 128:(j + 1) * 128, :], z[:, :], [z], [(k.yT, ("z", j))])


def phase_s5(k, l):
    P = k.P; I = k.I
    P.barrier(); P.sb_reset(k.const_mark)
    TC = 256
    NCH = T // TC
    S5ROW = 1936
    E2 = P.sbuf("E2", [128, 16, 2, TC], F32); E3 = P.sbuf("E3", [128, 16, 2, TC], F32)
    Rt = P.sbuf("Rt", [128, 16, TC], F32)
    Bb = P.sbuf("Bb", [128, 2, 2, 8, 128], BF16)
    Cb = P.sbuf("Cb", [128, 2, 2, 8, 128], BF16)
    s5mark = P.sb_off
    def ptile(name, shape=(128, 2, 8)):
        return P.sbuf(name, list(shape), F32)
    lre = ptile("lre"); lim = ptile("lim"); dt = ptile("dt")
    for d in range(2):
        P.dma(lre[:, d, :], I["s5_lam_re"].ap[l, d].rearrange("(b g) n -> (g n) b", g=2), [I["s5_lam_re"]], [lre], allow_slow_non_contiguous=True)
        P.dma(lim[:, d, :], I["s5_lam_im"].ap[l, d].rearrange("(b g) n -> (g n) b", g=2), [I["s5_lam_im"]], [lim], allow_slow_non_contiguous=True)
        for gl in range(2):
            P.dma(dt[gl * 64:(gl + 1) * 64, d, :], I["s5_log_step"].ap[l, d].rearrange("(b g) -> g b", g=2)[gl].partition_broadcast(64), [I["s5_log_step"]], [dt], allow_slow_non_contiguous=True)
    ACT(P, dt[:, :, :], dt[:, :, :], AF.Exp, [dt], [dt])
    are = ptile("are"); th = ptile("th"); rr = ptile("rr")
    TT(P, "dve", are[:, :, :], lre[:, :, :], dt[:, :, :], ALU.mult, [lre, dt], [are])
    TT(P, "dve", th[:, :, :], lim[:, :, :], dt[:, :, :], ALU.mult, [lim, dt], [th])
    ACT(P, rr[:, :, :], are[:, :, :], AF.Exp, [are], [rr])
    q = ptile("q"); qi = P.sbuf("qi", [128, 2, 8], I32); thm = ptile("thm"); thc = ptile("thc")
    TS(P, "dve", q[:, :, :], th[:, :, :], 1.0 / (2 * math.pi), None, ALU.mult, None, [th], [q])
    P.ew("dve", "tensor_copy", [q], [qi], qi[:, :, :], q[:, :, :])
    P.ew("dve", "tensor_copy", [qi], [q], q[:, :, :], qi[:, :, :])
    STT(P, "dve", thm[:, :, :], q[:, :, :], -2 * math.pi, th[:, :, :], ALU.mult, ALU.add, [q, th], [thm])
    gt = ptile("gt")
    TS(P, "dve", gt[:, :, :], thm[:, :, :], math.pi, -2 * math.pi, ALU.is_gt, ALU.mult, [thm], [gt])
    TT(P, "dve", thm[:, :, :], thm[:, :, :], gt[:, :, :], ALU.add, [thm, gt], [thm])
    TS(P, "dve", gt[:, :, :], thm[:, :, :], -math.pi, 2 * math.pi, ALU.is_lt, ALU.mult, [thm], [gt])
    TT(P, "dve", thm[:, :, :], thm[:, :, :], gt[:, :, :], ALU.add, [thm, gt], [thm])
    TS(P, "dve", thc[:, :, :], thm[:, :, :], math.pi / 2, None, ALU.add, None, [thm], [thc])
    TS(P, "dve", gt[:, :, :], thc[:, :, :], math.pi, -2 * math.pi, ALU.is_gt, ALU.mult, [thc], [gt])
    TT(P, "dve", thc[:, :, :], thc[:, :, :], gt[:, :, :], ALU.add, [thc, gt], [thc])
    sn1 = ptile("sn1"); cs1 = ptile("cs1")
    ACT(P, sn1[:, :, :], thm[:, :, :], AF.Sin, [thm], [sn1])
    ACT(P, cs1[:, :, :], thc[:, :, :], AF.Sin, [thc], [cs1])
    nre = ptile("nre"); nim = ptile("nim"); den = ptile("den"); kre = ptile("kre"); kim = ptile("kim"); t1 = ptile("t1"); t2 = ptile("t2")
    TT(P, "dve", nre[:, :, :], rr[:, :, :], cs1[:, :, :], ALU.mult, [rr, cs1], [nre])
    TS(P, "dve", nre[:, :, :], nre[:, :, :], -1.0, None, ALU.add, None, [nre], [nre])
    TT(P, "dve", nim[:, :, :], rr[:, :, :], sn1[:, :, :], ALU.mult, [rr, sn1], [nim])
    TT(P, "dve", t1[:, :, :], lre[:, :, :], lre[:, :, :], ALU.mult, [lre], [t1])
    TT(P, "dve", t2[:, :, :], lim[:, :, :], lim[:, :, :], ALU.mult, [lim], [t2])
    TT(P, "dve", den[:, :, :], t1[:, :, :], t2[:, :, :], ALU.add, [t1, t2], [den])
    P.ew("dve", "reciprocal", [den], [den], den[:, :, :], den[:, :, :])
    TT(P, "dve", t1[:, :, :], nre[:, :, :], lre[:, :, :], ALU.mult, [nre, lre], [t1])
    TT(P, "dve", t2[:, :, :], nim[:, :, :], lim[:, :, :], ALU.mult, [nim, lim], [t2])
    TT(P, "dve", kre[:, :, :], t1[:, :, :], t2[:, :, :], ALU.add, [t1, t2], [kre])
    TT(P, "dve", kre[:, :, :], kre[:, :, :], den[:, :, :], ALU.mult, [kre, den], [kre])
    TT(P, "dve", t1[:, :, :], nim[:, :, :], lre[:, :, :], ALU.mult, [nim, lre], [t1])
    TT(P, "dve", t2[:, :, :], nre[:, :, :], lim[:, :, :], ALU.mult, [nre, lim], [t2])
    TT(P, "dve", kim[:, :, :], t1[:, :, :], t2[:, :, :], ALU.subtract, [t1, t2], [kim])
    TT(P, "dve", kim[:, :, :], kim[:, :, :], den[:, :, :], ALU.mult, [kim, den], [kim])
    Ere = P.sbuf("Ere", [128, 16, TC], F32); Eim = P.sbuf("Eim", [128, 16, TC], F32)
    cs1f = cs1.ap.rearrange("p d b -> p (d b)"); sn1f = sn1.ap.rearrange("p d b -> p (d b)")
    P.ew("dve", "tensor_copy", [cs1], [Ere], Ere[:, :, 0:1], cs1f.unsqueeze(2))
    P.ew("dve", "tensor_copy", [sn1], [Eim], Eim[:, :, 0:1], sn1f.unsqueeze(2))
    ta = P.sbuf("Eta", [128, 16, TC // 2], F32); tb_ = P.sbuf("Etb", [128, 16, TC // 2], F32)
    m = 1
    while m < TC:
        cr = Ere[:, :, m - 1:m].broadcast_to([128, 16, m]); ci = Eim[:, :, m - 1:m].broadcast_to([128, 16, m])
        TT(P, "dve", ta[:, :, 0:m], Ere[:, :, 0:m], cr, ALU.mult, [Ere], [ta])
        TT(P, "pool", tb_[:, :, 0:m], Eim[:, :, 0:m], ci, ALU.mult, [Eim], [tb_])
        TT(P, "dve", Ere[:, :, m:2 * m], ta[:, :, 0:m], tb_[:, :, 0:m], ALU.subtract, [ta, tb_, Eim], [Ere])
        TT(P, "dve", ta[:, :, 0:m], Ere[:, :, 0:m], ci, ALU.mult, [Ere, Eim], [ta])
        TT(P, "pool", tb_[:, :, 0:m], Eim[:, :, 0:m], cr, ALU.mult, [Eim, Ere], [tb_])
        TT(P, "dve", Eim[:, :, m:2 * m], ta[:, :, 0:m], tb_[:, :, 0:m], ALU.add, [ta, tb_], [Eim])
        m *= 2
    P.ew("dve", "tensor_copy", [Ere], [E2], E2[:, :, 0, :], Ere[:, :, :])
    P.ew("pool", "tensor_copy", [Ere], [E2], E2[:, :, 1, :], Ere[:, :, :])
    P.ew("dve", "tensor_copy", [Eim], [E3], E3[:, :, 0, :], Eim[:, :, :])
    TS(P, "dve", E3[:, :, 1, :], Eim[:, :, :], -1.0, None, ALU.mult, None, [Eim], [E3])
    P.ew("dve", "tensor_copy", [rr], [Rt], Rt[:, :, :], rr.ap.rearrange("p d b -> p (d b)").unsqueeze(2).broadcast_to([128, 16, TC]))
    Bf = P.sbuf("Bf", [128, 2, 2, 8, 128], F32)
    Cf = P.sbuf("Cf", [128, 2, 2, 8, 128], F32)
    P.ew("pool", "memset", [], [Bf], Bf[:, :, :, :, :], 0.0)
    P.ew("pool", "memset", [], [Cf], Cf[:, :, :, :, :], 0.0)
    nq = 0
    for d in range(2):
        for g in range(16):
            blk = g // 2; gl = g % 2; c0 = (g % 8) * 16
            for ri, (bn, cn_) in enumerate((("s5_b_re", "s5_c_re"), ("s5_b_im", "s5_c_im"))):
                eng = "sp" if nq % 2 else "pool"; nq += 1
                P.dma(Bf[c0:c0 + 16, d, ri, blk, gl * 64:(gl + 1) * 64], I[bn].ap[l, d, g].rearrange("n h -> h n"), [I[bn]], [Bf], eng=eng, allow_slow_non_contiguous=True)
                P.dma(Cf[gl * 64:(gl + 1) * 64, d, ri, blk, c0:c0 + 16], I[cn_].ap[l, d, g].rearrange("h n -> n h"), [I[cn_]], [Cf], eng=eng, allow_slow_non_contiguous=True)
    P.ew("dve", "tensor_copy", [Bf], [Bb], Bb[:, :, :, :, :], Bf[:, :, :, :, :])
    c1 = P.sbuf("c1", [128, 8, 128], F32); c2 = P.sbuf("c2", [128, 8, 128], F32)
    for d in range(2):
        kr = kre[:, d, :].unsqueeze(2).broadcast_to([128, 8, 128]); ki = kim[:, d, :].unsqueeze(2).broadcast_to([128, 8, 128])
        TT(P, "dve", c1[:, :, :], Cf[:, d, 0, :, :], kr, ALU.mult, [Cf, kre], [c1])
        TT(P, "pool", c2[:, :, :], Cf[:, d, 1, :, :], ki, ALU.mult, [Cf, kim], [c2])
        TT(P, "dve", Cb[:, d, 0, :, :], c1[:, :, :], c2[:, :, :], ALU.subtract, [c1, c2], [(Cb, (d, 0))])
        TT(P, "dve", c1[:, :, :], Cf[:, d, 0, :, :], ki, ALU.mult, [Cf, kim], [c1])
        TT(P, "pool", c2[:, :, :], Cf[:, d, 1, :, :], kr, ALU.mult, [Cf, kre], [c2])
        STT(P, "dve", Cb[:, d, 1, :, :], c1[:, :, :], -1.0, c2[:, :, :], ALU.mult, ALU.subtract, [c1, c2], [(Cb, (d, 1))])
    P.barrier()
    P.sb_reset(s5mark)
    uS0 = P.sbuf("uS0", [128, 2, T], BF16)
    uS = [uS0, uS0]
    yacc_off = P.sb_off
    yacc = P.sbuf("s5yacc", [128, 2, T], F32)
    ur_off = P.sb_off
    ur = P.sbuf("ur", [128, 2, T], F32)
    for h in range(2):
        P.dma(ur[:, h, :], k.featT[S5ROW + h * 128:S5ROW + (h + 1) * 128, :], [k.featT], [ur], eng=("sp" if h else "pool"))
    for h in range(2):
        P.ew("dve", "tensor_copy", [ur], [(uS[0], h)], uS[0][:, h, 0:CTX], ur[:, h, 0:CTX])
        P.ew("dve", "tensor_copy", [ur], [(uS[0], h)], uS[0][:, h, CTX:T].rearrange("p (c r) -> p c r", c=64), ur[:, h, CTX:T].rearrange("p (r c) -> p c r", c=64))
    P.barrier(); P.sb_reset(ur_off)
    def wt(name, dt=F32):
        return [P.sbuf(f"{name}{i}", [128, 2, TC], dt) for i in range(4)]
    tA = wt("s5A"); tB = wt("s5B"); tC = wt("s5C"); tD = wt("s5D"); sbb = wt("s5sbb", BF16)
    carry = P.sbuf("carry", [128, 8, 2], F32)
    zero1 = P.sbuf("zero1", [128, 1], F32)
    P.ew("pool", "memset", [], [zero1], zero1[:, :], 0.0)
    n = 0
    for d in range(2):
        for ci in range(NCH):
            t0 = ci * TC
            if d == 0:
                sl0, sl1 = t0, t0 + TC; rev = False
            elif t0 < CTX:
                sl0, sl1 = 0, CTX; rev = True
            else:
                sl0 = CTX + (L - (t0 - CTX) - TC); sl1 = sl0 + TC; rev = True
            for h in range(2):
                usl = uS0[:, h, sl0:sl1]
                if rev:
                    usl = usl[:, ::-1]
                blks = [h * 4 + q for q in range(4)]
                pzs_ = [k.ps[q] for q in range(4)]
                py = k.ps[4 + (n % 2)]; n += 1
                for q, blk in enumerate(blks):
                    pz = pzs_[q]
                    P.mm(pz[:, 0:TC], Bb[:, d, 0, blk, :], usl, [Bb, (uS0, h)], [pz], start=True, stop=True)
                    P.mm(pz[:, TC:2 * TC], Bb[:, d, 1, blk, :], usl, [Bb, (uS0, h)], [pz], start=True, stop=True)
                for q, blk in enumerate(blks):
                    e16 = d * 8 + blk
                    pz3 = pzs_[q].ap.rearrange("p (a b) -> p a b", a=2)
                    TT(P, "dve", tA[q][:, :, :], pz3, E2[:, e16, :, :], ALU.mult, [pzs_[q], E2], [tA[q]])
                    TT(P, "dve", tB[q][:, :, :], pz3[:, ::-1, :], E3[:, e16, :, :], ALU.mult, [pzs_[q], E3], [tB[q]])
                for q, blk in enumerate(blks):
                    TT(P, "pool", tC[q][:, :, :], tA[q][:, :, :], tB[q][:, :, :], ALU.add, [tA[q], tB[q]], [tC[q]])
                for q, blk in enumerate(blks):
                    e16 = d * 8 + blk
                    for c_ in range(2):
                        init = zero1[:, 0:1] if ci == 0 else carry[:, blk, c_:c_ + 1]
                        P.add("dve", lambda e, q=q, c_=c_, init=init, e16=e16: e.tensor_tensor_scan(tA[q][:, c_, :], Rt[:, e16, :], tC[q][:, c_, :], init, ALU.mult, ALU.add), [Rt, tC[q], (carry, blk), zero1], [(tA[q], c_)])
                for q, blk in enumerate(blks):
                    ACT(P, tB[q][:, :, :], tA[q][:, ::-1, :], AF.Copy, [tA[q]], [tB[q]])
                for q, blk in enumerate(blks):
                    e16 = d * 8 + blk
                    TT(P, "pool", tC[q][:, :, :], tA[q][:, :, :], E2[:, e16, :, :], ALU.mult, [tA[q], E2], [tC[q]])
                    TT(P, "dve" if q % 2 else "pool", tD[q][:, :, :], tB[q][:, :, :], E3[:, e16, :, :], ALU.mult, [tB[q], E3], [tD[q]])
                for q, blk in enumerate(blks):
                    TT(P, "dve", tA[q][:, :, :], tC[q][:, :, :], tD[q][:, :, :], ALU.subtract, [tC[q], tD[q]], [tA[q]])
                for q, blk in enumerate(blks):
                    ACT(P, sbb[q][:, :, :], tA[q][:, :, :], AF.Copy, [tA[q]], [sbb[q]])
                    ACT(P, carry[:, blk, :], tA[q][:, :, TC - 1], AF.Copy, [tA[q]], [(carry, blk)])
                for q, blk in enumerate(blks):
                    P.mm(py[:, 0:TC], Cb[:, d, 0, blk, :], sbb[q][:, 0, :], [Cb, sbb[q]], [py], start=(q == 0), stop=False)
                    P.mm(py[:, 0:TC], Cb[:, d, 1, blk, :], sbb[q][:, 1, :], [Cb, sbb[q]], [py], start=False, stop=(q == 3))
                if d == 0:
                    ACT(P, yacc[:, h, t0:t0 + TC], py[:, 0:TC], AF.Copy, [py], [(yacc, (h, sl0 // TC))])
                else:
                    dst = yacc[:, h, sl0:sl1][:, ::-1]
                    TT(P, "dve", dst, dst, py[:, 0:TC], ALU.add, [py, (yacc, (h, sl0 // TC))], [(yacc, (h, sl0 // TC))])
    P.barrier(); P.sb_reset(ur_off)
    ur = P.sbuf("ur2", [128, 2, T], F32)
    for h in range(2):
        P.dma(ur[:, h, :], k.featT[S5ROW + h * 128:S5ROW + (h + 1) * 128, :], [k.featT], [ur], eng=("sp" if h else "pool"))
    ur_end = P.sb_off
    dsk = P.sbuf("dsk", [128, 2], F32); bgl = P.sbuf("bgl", [128, 2], F32)
    P.dma(dsk[:, :], I["s5_d"].ap[l].rearrange("(h p) -> p h", p=128), [I["s5_d"]], [dsk], allow_slow_non_contiguous=True)
    P.dma(bgl[:, :], I["s5_b_glu"].ap[l].rearrange("(h p) -> p h", p=128), [I["s5_b_glu"]], [bgl], allow_slow_non_contiguous=True)
    wgf = P.sbuf("s5wgf", [128, 2, 256], F32); wgb = P.sbuf("s5wgb", [128, 2, 256], BF16)
    P.dma(wgf[:, :, :], I["s5_w_glu"].ap[l].rearrange("(c p) n -> p c n", p=128), [I["s5_w_glu"]], [wgf])
    P.ew("dve", "tensor_copy", [wgf], [wgb], wgb[:, :, :], wgf[:, :, :])
    zb = uS0
    P.barrier()
    for h in range(2):
        STT(P, "dve", yacc[:, h, 0:CTX], ur[:, h, 0:CTX], dsk[:, h:h + 1], yacc[:, h, 0:CTX], ALU.mult, ALU.add, [ur, dsk, yacc], [yacc])
        STT(P, "dve", yacc[:, h, CTX:T].rearrange("p (c r) -> p c r", c=64), ur[:, h, CTX:T].rearrange("p (r c) -> p c r", c=64), dsk[:, h:h + 1], yacc[:, h, CTX:T].rearrange("p (c r) -> p c r", c=64), ALU.mult, ALU.add, [ur, dsk, yacc], [yacc])
    P.barrier()
    save_off = P.sb_off
    P.sb_reset(ur_off)
    zf = P.sbuf("s5zf", [128, 2, T], F32)
    P.sb_reset(max(save_off, P.sb_off))
    for h in range(2):
        y = yacc[:, h, :]
        TT(P, "dve", zf[:, h, :], y, y, ALU.mult, [yacc], [zf])
        TS(P, "dve", zf[:, h, :], zf[:, h, :], 0.044715, 1.0, ALU.mult, ALU.add, [zf], [zf])
        TT(P, "dve", zf[:, h, :], zf[:, h, :], y, ALU.mult, [zf, yacc], [zf])
        ACT(P, zf[:, h, :], zf[:, h, :], AF.Sigmoid, [zf], [zf], scale=1.5957691216057308)
        TT(P, "dve", zf[:, h, :], zf[:, h, :], y, ALU.mult, [zf, yacc], [zf])
        ACT(P, zb[:, h, :], zf[:, h, :], AF.Copy, [zf], [zb])
    P.barrier()
    save2 = P.sb_off
    P.sb_reset(yacc_off)
    yd = P.sbuf("s5yd", [128, 2, T], BF16)
    P.sb_reset(save2)
    sgl = [P.sbuf(f"sgl{i}", [128, 512], F32) for i in range(2)]
    blocks = [(0, CTX)] + [(CTX + b * 512, 512) for b in range(8)]
    n = 0
    for (t0, tn) in blocks:
        for ho in range(2):
            pg = k.ps[n % 2]; sg_ = sgl[n % 2]; n += 1
            for hi in range(2):
                P.mm(pg[:, 0:tn], wgb[:, hi, ho * 128:(ho + 1) * 128], zb[:, hi, t0:t0 + tn], [wgb, zb], [pg], start=(hi == 0), stop=(hi == 1))
            ACT(P, sg_[:, 0:tn], pg[:, 0:tn], AF.Sigmoid, [pg, bgl], [sg_], bias=bgl[:, ho:ho + 1])
            if t0 < CTX:
                TT(P, "dve", yd[:, ho, 0:CTX], zf[:, ho, 0:CTX], sg_[:, 0:CTX], ALU.mult, [zf, sg_], [(yd, (ho, t0))])
            else:
                c0 = (t0 - CTX) // 64
                dst = yd[:, ho, CTX:T].rearrange("p (r c) -> p c r", c=64)[:, c0:c0 + 8, :]
                TT(P, "dve", dst, zf[:, ho, t0:t0 + tn].rearrange("p (c r) -> p c r", c=8), sg_[:, 0:tn].rearrange("p (c r) -> p c r", c=8), ALU.mult, [zf, sg_], [(yd, (ho, t0))])
    for ho in range(2):
        P.dma(k.yT[768 + ho * 128:768 + (ho + 1) * 128, :], yd[:, ho, :], [yd], [(k.yT, ("s5", ho))])


def Bf_mark(P, Bf, k):
    return P.sb_off


USE_F32R = False


def R32(ap):
    return ap.bitcast(mybir.dt.float32r) if USE_F32R else ap


NCHK = T // 64
INV_DT = mybir.dt.float32r
BLK = 128


def build_masks(k):
    P = k.P
    ones = P.sbuf("m_ones", [64, 8, 64], F32)
    P.ew("pool", "memset", [], [ones], ones[:, :, :], 1.0)
    k.m_ts_s = P.sbuf("m_ts_s", [64, 8, 64], F32)
    k.m_st_s = P.sbuf("m_st_s", [64, 8, 64], F32)
    k.m_st_i = P.sbuf("m_st_i", [64, 8, 64], F32)
    P.add("pool", lambda e: e.affine_select(k.m_ts_s[:, :, :], ones[:, :, :], [[0, 8], [-1, 64]], ALU.is_gt, 0.0, base=0, channel_multiplier=1), [ones], [k.m_ts_s])
    P.add("pool", lambda e: e.affine_select(k.m_st_s[:, :, :], ones[:, :, :], [[0, 8], [1, 64]], ALU.is_gt, 0.0, base=0, channel_multiplier=-1), [ones], [k.m_st_s])
    P.add("pool", lambda e: e.affine_select(k.m_st_i[:, :, :], ones[:, :, :], [[0, 8], [1, 64]], ALU.is_ge, 0.0, base=0, channel_multiplier=-1), [ones], [k.m_st_i])
    k.identf8 = P.sbuf("identf8", [64, 8, 64], F32)
    P.add("pool", lambda e: e.affine_select(k.identf8[:, :, :], ones[:, :, :], [[0, 8], [1, 64]], ALU.is_equal, 0.0, base=0, channel_multiplier=-1), [ones], [k.identf8])


class DplrChain:
    def __init__(self, k, tag, opsD, pcD, youtD, opmap, dn_rows, banks):
        P = k.P
        self.k = k; self.opsD = opsD; self.youtD = youtD; self.opmap = opmap; self.dn_rows = dn_rows
        self.b = [k.ps[i] for i in banks]
        self.NO = max(opmap.values()) + 1
        self.pc = P.sbuf("pc", [64, 8, NCHK], F32)
        P.dma(self.pc[:, :, :], pcD.ap.rearrange("i c n -> c i n"), [pcD], [self.pc])
        self.ST = P.sbuf("ST", [64, 8, 64], F32); self.STb = P.sbuf("STb", [64, 8, 64], BF16)
        P.ew("pool", "memset", [], [self.ST], self.ST[:, :, :], 0.0)
        P.ew("pool", "memset", [], [self.STb], self.STb[:, :, :], 0.0)
        self.opt = [[P.sbuf(f"opt{b}_{o}", [64, 8, BLK], BF16) for o in range(self.NO)] for b in range(2)]
        self.yblk = [P.sbuf(f"yblk{b}", [64, 8, BLK], F32) for b in range(2)]

        def t3(name, dt=F32, n=2):
            return [P.sbuf(f"{name}{i}", [64, 8, 64], dt) for i in range(n)]
        self.S_ab = t3("Sab", INV_DT); self.S_abT = t3("SabT", INV_DT)
        self.Xs = t3("Xs", INV_DT, 1)[0]; self.Ub = t3("Ub", BF16, 1)[0]
        self.S_akT2 = t3("SakT2", BF16); self.S_rbT2 = t3("SrbT2", BF16); self.S_rkT2 = t3("SrkT2", BF16)
        self.BhT2 = t3("BhT2", BF16); self.KhT2 = t3("KhT2", BF16); self.vT2 = t3("vT2", BF16)
        self.Pm2 = [t3("PmA", INV_DT), t3("PmB", INV_DT)]
        if dn_rows is not None:
            self.Lg = [P.sbuf(f"Lg{b}", [2, 8, BLK], F32) for b in range(2)]
            self.Rg = [P.sbuf(f"Rg{b}", [2, 8, BLK], F32) for b in range(2)]
            for b in range(2):
                P.ew("pool", "memset", [], [self.Lg[b]], self.Lg[b][:, :, :], 1.0)
                P.ew("pool", "memset", [], [self.Rg[b]], self.Rg[b][:, :, :], 1.0)
            self.bias = []
            for nm, mk in (("b_ts_s", k.m_ts_s), ("b_st_s", k.m_st_s), ("b_st_i", k.m_st_i)):
                bt = P.sbuf(nm, [64, 8, 64], F32)
                TS(P, "dve", bt[:, :, :], mk[:, :, :], -1.0, 30000.0, ALU.add, ALU.mult, [mk], [bt])
                self.bias.append(bt)
            self.D = [P.sbuf(nm, [64, 8, 64], F32) for nm in ("D_ts", "D_st_s", "D_st_i")]

    def load(self, blk):
        P = self.k.P
        t0 = blk * BLK
        ob = self.opt[blk % 2]
        for o in range(self.NO):
            P.dma(ob[o][:, :, :], self.opsD.ap[o, :, :, t0:t0 + BLK].rearrange("i c t -> c i t"), [self.opsD], [ob[o]], eng=("sp" if o % 2 else "pool"))
        if self.dn_rows is not None:
            GrowD, nGrowD = self.dn_rows
            lg = self.Lg[blk % 2]; rg = self.Rg[blk % 2]
            P.dma(lg[0:1, :, :], GrowD.ap[:, t0:t0 + BLK].unsqueeze(0), [GrowD], [(lg, 0)])
            P.dma(rg[1:2, :, :], nGrowD.ap[:, t0:t0 + BLK].unsqueeze(0), [nGrowD], [(rg, 0)])

    def store(self, blk):
        P = self.k.P
        t0 = blk * BLK
        yb = self.yblk[blk % 2]
        P.dma(self.youtD.ap[:, :, t0:t0 + BLK].rearrange("i c t -> c i t"), yb[:, :, :], [yb], [(self.youtD, blk)], eng="pool")

    def par(self, g):
        k = self.k; P = k.P
        blk, jl = divmod(g, BLK // 64)
        slot = g % 2
        b0, b1, b2 = self.b[0], self.b[1], self.b[2]
        ob = self.opt[blk % 2]
        ib = k.ident_b[0:64, 0:64]
        S_ab, S_abT = self.S_ab, self.S_abT
        S_akT, S_rbT, S_rkT = self.S_akT2[slot], self.S_rbT2[slot], self.S_rkT2[slot]
        Pm = self.Pm2[slot]; BhT, KhT, vT = self.BhT2[slot], self.KhT2[slot], self.vT2[slot]

        def sub(pst, i):
            return pst[0:64, i * 64:(i + 1) * 64]

        def v3(pst):
            return pst.ap[0:64, :].rearrange("p (i c) -> p i c", i=8)
        sl = slice(jl * 64, (jl + 1) * 64)
        O = {n: ob[ix] for n, ix in self.opmap.items()}
        if self.dn_rows is None:
            mult = {"ab": (k.m_ts_s, 1.0), "abT": (k.m_st_s, 1.0), "akT": (k.m_st_s, 1.0), "rbT": (k.m_st_i, 1.0), "rkT": (k.m_st_i, 1.0)}
        else:
            lg = self.Lg[blk % 2]; rg = self.Rg[blk % 2]
            D_ts, D_st_s, D_st_i = self.D
            bias_ts_s, bias_st_s, bias_st_i = self.bias
            pd = b1; pdt = b2
            for i in range(8):
                P.mm(sub(pd, i), lg[:, i, sl], rg[:, i, sl], [lg, rg], [pd])
                P.mm(sub(pdt, i), rg[:, i, sl], lg[:, i, sl], [lg, rg], [pdt])
            TT(P, "dve", D_ts[:, :, :], v3(pd), bias_ts_s[:, :, :], ALU.add, [pd, bias_ts_s], [D_ts])
            ACT(P, D_ts[:, :, :], D_ts[:, :, :], AF.Exp, [D_ts], [D_ts])
            yield
            TT(P, "dve", D_st_s[:, :, :], v3(pdt), bias_st_s[:, :, :], ALU.add, [pdt, bias_st_s], [D_st_s])
            ACT(P, D_st_s[:, :, :], D_st_s[:, :, :], AF.Exp, [D_st_s], [D_st_s])
            TT(P, "dve", D_st_i[:, :, :], v3(pdt), bias_st_i[:, :, :], ALU.add, [pdt, bias_st_i], [D_st_i])
            ACT(P, D_st_i[:, :, :], D_st_i[:, :, :], AF.Exp, [D_st_i], [D_st_i])
            yield
            mult = {"ab": (D_ts, -1.0), "abT": (D_st_s, -1.0), "akT": (D_st_s, -1.0), "rbT": (D_st_i, 1.0), "rkT": (D_st_i, 1.0)}
        A0 = S_ab[0]; B0 = S_abT[0]
        kinds = [("ab", "sc_a", "sc_b", A0, b0), ("abT", "sc_b", "sc_a", B0, b1), ("akT", "sc_k", "sc_a", S_akT, b2),
                 ("rbT", "sc_b", "sc_r", S_rbT, b0), ("rkT", "sc_k", "sc_r", S_rkT, b1)]
        for kind, ln, rn, dst, pst in kinds:
            for i in range(8):
                P.mm(sub(pst, i), O[ln][:, i, sl], O[rn][:, i, sl], [O[ln], O[rn]], [pst])
            mt, sgn = mult[kind]
            STT(P, "dve", dst[:, :, :], v3(pst), sgn, mt[:, :, :], ALU.mult, ALU.mult, [pst, mt], [dst])
            yield
        for name, dst, pst, half in (("Bh", BhT, b2, 0), ("Kh", KhT, b2, 1), ("v", vT, b0, 0)):
            pv = pst.ap.bitcast(BF16)
            for i in range(8):
                P.tr(pv[0:64, half * 512 + i * 64: half * 512 + (i + 1) * 64], O[name][:, i, sl], ib, [O[name], k.ident_b], [pst])
            ACT(P, dst[:, :, :], pv[0:64, half * 512:(half + 1) * 512].rearrange("p (i c) -> p i c", i=8), AF.Copy, [pst], [dst])
            yield
        Pc_ = Pm[0]
        TT(P, "pool", Pc_[:, :, :], B0[:, :, :], k.identf8[:, :, :], ALU.add, [B0, k.identf8], [Pc_])
        Ac, Bc = A0, B0
        for jj in range(5):
            An = S_ab[(jj + 1) % 2]; Bn = S_abT[(jj + 1) % 2]; Pn = Pm[(jj + 1) % 2]
            pa = b0; pb = b1; pp = b2
            for i in range(8):
                P.mm(sub(pa, i), R32(Bc[:, i, :]), R32(Ac[:, i, :]), [Ac, Bc], [pa])
            ACT(P, An[:, :, :], v3(pa), AF.Copy, [pa], [An])
            if jj < 4:
                for i in range(8):
                    P.mm(sub(pb, i), R32(Ac[:, i, :]), R32(Bc[:, i, :]), [Ac, Bc], [pb])
                P.ew("dve", "tensor_copy", [pb], [Bn], Bn[:, :, :], v3(pb))
            yield
            for i in range(8):
                P.mm(sub(pp, i), R32(An[:, i, :]), R32(Pc_[:, i, :]), [An, Pc_], [pp])
            TT(P, "dve", Pn[:, :, :], v3(pp), Pc_[:, :, :], ALU.add, [pp, Pc_], [Pn])
            yield
            Ac, Bc, Pc_ = An, Bn, Pn

    def seq(self, g):
        k = self.k; P = k.P
        blk, jl = divmod(g, BLK // 64)
        slot = g % 2
        bq = self.b[3]
        ob = self.opt[blk % 2]; yb = self.yblk[blk % 2]
        S_akT, S_rbT, S_rkT = self.S_akT2[slot], self.S_rbT2[slot], self.S_rkT2[slot]
        NT = self.Pm2[slot][1]; BhT, KhT, vT = self.BhT2[slot], self.KhT2[slot], self.vT2[slot]
        Xs, Ub, ST, STb, pc = self.Xs, self.Ub, self.ST, self.STb, self.pc

        def sub(pst, i):
            return pst[0:64, i * 64:(i + 1) * 64]

        def v3(pst):
            return pst.ap[0:64, :].rearrange("p (i c) -> p i c", i=8)
        cj = g
        sl = slice(jl * 64, (jl + 1) * 64)
        O = {n: ob[ix] for n, ix in self.opmap.items()}
        for i in range(8):
            P.mm(sub(bq, i), O["st_a"][:, i, sl], STb[:, i, :], [O["st_a"], STb], [bq], start=True, stop=False)
            P.mm(sub(bq, i), S_akT[:, i, :], vT[:, i, :], [S_akT, vT], [bq], start=False, stop=True)
        ACT(P, Xs[:, :, :], v3(bq), AF.Copy, [bq], [Xs])
        yield
        for i in range(8):
            P.mm(sub(bq, i), R32(NT[:, i, :]), R32(Xs[:, i, :]), [NT, Xs], [bq])
        P.ew("dve", "tensor_copy", [bq], [Ub], Ub[:, :, :], v3(bq))
        yield
        for i in range(8):
            P.mm(sub(bq, i), BhT[:, i, :], Ub[:, i, :], [BhT, Ub], [bq], start=True, stop=False)
            P.mm(sub(bq, i), KhT[:, i, :], vT[:, i, :], [KhT, vT], [bq], start=False, stop=True)
        TT(P, "pool", ST[:, :, :], ST[:, :, :], pc[:, :, cj:cj + 1].broadcast_to([64, 8, 64]), ALU.mult, [ST, pc], [ST])
        TT(P, "dve", ST[:, :, :], ST[:, :, :], v3(bq), ALU.add, [ST, bq], [ST])
        yield
        for i in range(8):
            P.mm(sub(bq, i), STb[:, i, :], O["st_r"][:, i, sl], [STb, O["st_r"]], [bq], start=True, stop=False)
            P.mm(sub(bq, i), Ub[:, i, :], S_rbT[:, i, :], [Ub, S_rbT], [bq], start=False, stop=False)
            P.mm(sub(bq, i), vT[:, i, :], S_rkT[:, i, :], [vT, S_rkT], [bq], start=False, stop=True)
        ACT(P, yb[:, :, sl], v3(bq), AF.Copy, [bq], [(yb, jl)])
        ACT(P, STb[:, :, :], ST[:, :, :], AF.Copy, [ST], [STb])
        yield


def dplr_run(k, chains_spec):
    P = k.P
    P.barrier(); P.sb_reset(k.const_mark)
    build_masks(k)
    chains = []
    for ci, (tag, opsD, pcD, youtD, opmap, dn_rows) in enumerate(chains_spec):
        banks = (0, 1, 2, 3) if ci == 0 else (4, 5, 6, 7)
        chains.append(DplrChain(k, tag, opsD, pcD, youtD, opmap, dn_rows, banks))
    spb = BLK // 64
    nstep = NCHK

    def drive(gens):
        live = list(gens)
        while live:
            nxt = []
            for g_ in live:
                try:
                    next(g_)
                    nxt.append(g_)
                except StopIteration:
                    pass
            live = nxt
    for c in chains:
        c.load(0)
        c.load(1)
    drive([c.par(0) for c in chains])
    for g in range(nstep):
        gens = []
        for c in chains:
            if g + 1 < nstep:
                gens.append(c.par(g + 1))
            gens.append(c.seq(g))
        drive(gens)
        if (g + 1) % spb == 0:
            blk = g // spb
            for c in chains:
                c.store(blk)
                if blk + 2 < T // BLK:
                    c.load(blk + 2)


def seg_pairs(d):
    if d == 0:
        return [((0, T), (0, T), False)]
    return [((0, CTX), (0, CTX), True), ((CTX, T), (CTX, T), True)]


def nat2scan(src, d, lo, hi):
    a = src[:, lo:hi]
    return a[:, ::-1] if d == 1 else a


DN_MAP = {"sc_a": 0, "sc_b": 1, "sc_k": 0, "sc_r": 2, "st_a": 3, "st_r": 4, "Bh": 5, "Kh": 6, "v": 7}
RW_MAP = {"sc_a": 0, "sc_b": 1, "sc_k": 2, "sc_r": 3, "st_a": 0, "st_r": 3, "Bh": 4, "Kh": 5, "v": 6}


def alloc_dplr_scratch(k):
    s = k.scratch
    k.dn_ops = s("dn_ops", [8, 8, 64, T], BF16); k.dn_pc = s("dn_pc", [8, 64, NCHK]); k.dn_y = s("dn_y", [8, 64, T])
    k.dn_G = s("dn_G", [8, T]); k.dn_nG = s("dn_nG", [8, T]); k.dn_rows = s("dn_rows", [3, 8, T])
    k.rw_ops = s("rw_ops", [7, 8, 64, T], BF16); k.rw_pc = s("rw_pc", [8, 64, NCHK]); k.rw_y = s("rw_y", [8, 64, T])


def phase_dn_prep(k, l):
    P = k.P; I = k.I
    P.barrier(); P.sb_reset(k.const_mark)
    mark0 = P.sb_off
    def rt(name):
        return P.sbuf(name, [8, T], F32)
    t_al = rt("t_al"); t_be = rt("t_be"); t_lar = rt("t_lar"); t_ber = rt("t_ber"); rm = rt("rm"); Gn = rt("Gn"); Gr = rt("Gr"); tmp1 = rt("tmp1"); tmp2 = rt("tmp2")
    P.dma(t_al[:, :], k.featT[1920:1928, :], [k.featT], [t_al])
    P.dma(t_be[:, :], k.featT[1928:1936, :], [k.featT], [t_be], eng="pool")
    alog = P.sbuf("alog", [8, 1], F32); dtb = P.sbuf("dtb", [8, 1], F32); one8 = P.sbuf("one8", [8, 1], F32)
    P.dma(alog[:, :], I["dn_a_log"].ap[l].rearrange("d (h o) -> (d h) o", o=1), [I["dn_a_log"]], [alog])
    P.dma(dtb[:, :], I["dn_dt_bias"].ap[l].rearrange("d (h o) -> (d h) o", o=1), [I["dn_dt_bias"]], [dtb])
    P.ew("pool", "memset", [], [one8], one8[:, :], 1.0)
    ACT(P, alog[:, :], alog[:, :], AF.Exp, [alog], [alog])
    TS(P, "dve", alog[:, :], alog[:, :], -1.0, None, ALU.mult, None, [alog], [alog])
    ACT(P, t_al[:, :], t_al[:, :], AF.Exp, [t_al, dtb], [t_al], bias=dtb[:, :])
    ACT(P, t_al[:, :], t_al[:, :], AF.Ln, [t_al, one8], [t_al], bias=one8[:, :])
    TS(P, "dve", t_al[:, :], t_al[:, :], alog[:, 0:1], None, ALU.mult, None, [t_al, alog], [t_al])
    ACT(P, t_be[:, :], t_be[:, :], AF.Sigmoid, [t_be], [t_be])
    for (dlo, dhi), (slo, shi), _ in seg_pairs(1):
        P.ew("dve", "tensor_copy", [t_al], [t_lar], t_lar[:, dlo:dhi], t_al[:, slo:shi][:, ::-1])
        P.ew("pool", "tensor_copy", [t_be], [t_ber], t_ber[:, dlo:dhi], t_be[:, slo:shi][:, ::-1])
    P.ew("pool", "memset", [], [rm], rm[:, :], 1.0)
    P.ew("pool", "memset", [rm], [rm], rm.ap.rearrange("p (n c) -> p n c", c=64)[:, :, 0:1], 0.0)
    P.add("dve", lambda e: e.tensor_tensor_scan(Gn[:, :], rm[:, :], t_al[:, :], 0.0, ALU.mult, ALU.add), [rm, t_al], [Gn])
    P.add("dve", lambda e: e.tensor_tensor_scan(Gr[:, :], rm[:, :], t_lar[:, :], 0.0, ALU.mult, ALU.add), [rm, t_lar], [Gr])

    def put_rows(dst_fn, nat, rev):
        P.dma(dst_fn(0, 4), nat[0:4, :], [nat], [k.dn_rows, k.dn_G, k.dn_nG])
        P.dma(dst_fn(4, 8), rev[4:8, :], [rev], [k.dn_rows, k.dn_G, k.dn_nG], eng="pool")
    put_rows(lambda a, b: k.dn_rows.ap[0, a:b, :], t_be, t_ber)
    put_rows(lambda a, b: k.dn_G.ap[a:b, :], Gn, Gr)
    ACT(P, tmp1[:, :], Gn[:, :], AF.Exp, [Gn], [tmp1]); ACT(P, tmp2[:, :], Gr[:, :], AF.Exp, [Gr], [tmp2])
    put_rows(lambda a, b: k.dn_rows.ap[1, a:b, :], tmp1, tmp2)
    pcr = P.sbuf("pcr", [8, 2, NCHK], F32)
    for j, (G_, tp) in enumerate(((Gn, tmp1), (Gr, tmp2))):
        G3 = G_.ap.rearrange("p (n c) -> p n c", c=64)
        ACT(P, pcr[:, j, :], G3[:, :, 63], AF.Exp, [G_], [(pcr, j)])
    TS(P, "dve", t_al[:, :], Gn[:, :], -1.0, None, ALU.mult, None, [Gn], [t_al])
    TS(P, "dve", t_lar[:, :], Gr[:, :], -1.0, None, ALU.mult, None, [Gr], [t_lar])
    put_rows(lambda a, b: k.dn_nG.ap[a:b, :], t_al, t_lar)
    for G_, tp in ((Gn, tmp1), (Gr, tmp2)):
        G3 = G_.ap.rearrange("p (n c) -> p n c", c=64)
        TT(P, "dve", tp.ap.rearrange("p (n c) -> p n c", c=64), G3[:, :, 63:64].broadcast_to([8, NCHK, 64]), G3, ALU.subtract, [G_], [tp])
        ACT(P, tp[:, :], tp[:, :], AF.Exp, [tp], [tp])
    put_rows(lambda a, b: k.dn_rows.ap[2, a:b, :], tmp1, tmp2)
    pcs = k.scratch(f"dn_pcrow{l}", [8, NCHK])
    P.dma(pcs.ap[0:4, :], pcr[0:4, 0, :], [pcr], [pcs])
    P.dma(pcs.ap[4:8, :], pcr[4:8, 1, :], [pcr], [pcs])
    pcb = P.sbuf("pcb", [64, 8, NCHK], F32)
    for i in range(8):
        P.dma(pcb[:, i, :], pcs.ap[i, :].partition_broadcast(64), [pcs], [(pcb, i)])
    P.dma(k.dn_pc.ap.rearrange("i c n -> c i n"), pcb[:, :, :], [pcb], [k.dn_pc])
    P.barrier(); P.sb_reset(mark0)
    def ft(name, dt=F32):
        return P.sbuf(name, [128, T], dt)
    xin = ft("dxin"); u = [ft(f"du{j}") for j in range(3)]
    bbc = [ft(f"dbc{j}") for j in range(3)]; kb = ft("dkb")
    outs = [ft(f"dout{j}", BF16) for j in range(3)]
    sq = [P.sbuf(f"dsq{j}", [128, 512], mybir.dt.float32r) for j in range(2)]
    rs = [P.sbuf(f"drs{j}", [128, 512], F32) for j in range(2)]
    cw = P.sbuf("dcw", [128, 3, 5], F32)
    epsb = P.sbuf("depsb", [128, 1], F32)
    P.ew("pool", "memset", [], [epsb], epsb[:, :], EPS)
    no = 0
    for h in range(2):
        for part in range(3):
            P.dma(cw[:, part, :], I["dn_conv"].ap[l, :, part * 256 + h * 128: part * 256 + (h + 1) * 128].rearrange("j c -> c j"), [I["dn_conv"]], [(cw, part)], allow_slow_non_contiguous=True)
        for part in range(3):
            r0 = 896 + part * 256 + h * 128
            P.dma(xin[:, :], k.featT[r0:r0 + 128, :], [k.featT], [xin])
            acc = u[part]
            TS(P, "dve", acc[:, :], xin[:, :], cw[:, part, 2:3], None, ALU.mult, None, [xin, cw], [acc])
            n_ = 0
            for (lo, hi) in ((0, CTX), (CTX, T)):
                for j in (0, 1, 3, 4):
                    sh = j - 2
                    a = max(lo, lo - sh); b = min(hi, hi - sh)
                    STT(P, "dve", acc[:, a:b], xin[:, a + sh:b + sh], cw[:, part, j:j + 1], acc[:, a:b], ALU.mult, ALU.add, [xin, cw, acc], [acc]); n_ += 1
            ACT(P, acc[:, :], acc[:, :], AF.Silu, [acc], [acc])
        n_ = 0
        for part in range(2):
            for b in range(9):
                t0 = b * 512; tn = min(512, T - t0)
                s_ = sq[n_ % 2]; r_ = rs[n_ % 2]; pst = k.ps[n_ % 2]; n_ += 1
                ACT(P, s_[:, 0:tn], u[part][:, t0:t0 + tn], AF.Square, [u[part]], [s_])
                P.mm(pst[:, 0:tn], k.blk64r[:, :], s_[:, 0:tn], [k.blk64r, s_], [pst])
                ACT(P, r_[:, 0:tn], pst[:, 0:tn], AF.Ln, [pst, epsb], [r_], bias=epsb[:, :])
                ACT(P, r_[:, 0:tn], r_[:, 0:tn], AF.Exp, [r_], [r_], scale=-0.5)
                if part == 0:
                    STT(P, "dve", u[0][:, t0:t0 + tn], u[0][:, t0:t0 + tn], 0.125, r_[:, 0:tn], ALU.mult, ALU.mult, [u[0], r_], [u[0]])
                else:
                    TT(P, "dve", u[1][:, t0:t0 + tn], u[1][:, t0:t0 + tn], r_[:, 0:tn], ALU.mult, [u[1], r_], [u[1]])
        for d in range(2):
            i = d * 4 + 2 * h
            for j in range(3):
                for hh in range(2):
                    P.dma(bbc[j][hh * 64:(hh + 1) * 64, :], k.dn_rows.ap[j, i + hh, :].partition_broadcast(64), [k.dn_rows], [(bbc[j], hh)], eng=("sp" if (j + hh) % 2 else "pool"))
            beta_bc, EG_bc, E2_bc = bbc

            def emit(o_idx, fn):
                nonlocal no
                ot = outs[no % 3]; no += 1
                for (dlo, dhi), (slo, shi), _ in seg_pairs(d):
                    fn(ot, dlo, dhi)
                P.dma(k.dn_ops.ap[o_idx, i:i + 2, :, :].rearrange("i c t -> (i c) t"), ot[:, :], [ot], [(k.dn_ops, (o_idx, i))], eng=("sp" if no % 2 else "pool"))
            qs = lambda lo, hi: nat2scan(u[0], d, lo, hi)
            ks = lambda lo, hi: nat2scan(u[1], d, lo, hi)
            vs = lambda lo, hi: nat2scan(u[2], d, lo, hi)
            for (dlo, dhi), _, _ in seg_pairs(d):
                TT(P, "dve", kb[:, dlo:dhi], ks(dlo, dhi), beta_bc[:, dlo:dhi], ALU.mult, [u[1], beta_bc], [kb])
            emit(0, lambda ot, lo, hi: ACT(P, ot[:, lo:hi], kb[:, lo:hi], AF.Copy, [kb], [ot]))
            emit(1, lambda ot, lo, hi: ACT(P, ot[:, lo:hi], ks(lo, hi), AF.Copy, [u[1]], [ot]))
            emit(2, lambda ot, lo, hi: ACT(P, ot[:, lo:hi], qs(lo, hi), AF.Copy, [u[0]], [ot]))
            emit(3, lambda ot, lo, hi: STT(P, "dve", ot[:, lo:hi], kb[:, lo:hi], -1.0, EG_bc[:, lo:hi], ALU.mult, ALU.mult, [kb, EG_bc], [ot]))
            emit(4, lambda ot, lo, hi: TT(P, "dve", ot[:, lo:hi], qs(lo, hi), EG_bc[:, lo:hi], ALU.mult, [u[0], EG_bc], [ot]))
            emit(5, lambda ot, lo, hi: TT(P, "dve", ot[:, lo:hi], ks(lo, hi), E2_bc[:, lo:hi], ALU.mult, [u[1], E2_bc], [ot]))
            emit(6, lambda ot, lo, hi: TT(P, "dve", ot[:, lo:hi], kb[:, lo:hi], E2_bc[:, lo:hi], ALU.mult, [kb, E2_bc], [ot]))
            emit(7, lambda ot, lo, hi: ACT(P, ot[:, lo:hi], vs(lo, hi), AF.Copy, [u[2]], [ot]))


def phase_dn_post(k, l):
    P = k.P; I = k.I
    P.barrier(); P.sb_reset(k.const_mark)
    yf = P.sbuf("pyf", [128, T], F32); yr = P.sbuf("pyr", [128, T], F32); gt = P.sbuf("pgt", [128, T], F32)
    ob = P.sbuf("pob", [128, T], BF16)
    ng = P.sbuf("png", [128, 1], F32); epsb = P.sbuf("pepsb", [128, 1], F32)
    P.ew("pool", "memset", [], [epsb], epsb[:, :], EPS)
    P.dma(ng[0:64, :], I["dn_norm_g"].ap[l].rearrange("(c o) -> c o", o=1), [I["dn_norm_g"]], [ng])
    P.dma(ng[64:128, :], I["dn_norm_g"].ap[l].rearrange("(c o) -> c o", o=1), [I["dn_norm_g"]], [ng])
    sq = [P.sbuf(f"psq{j}", [128, 512], mybir.dt.float32r) for j in range(2)]
    rs = [P.sbuf(f"prs{j}", [128, 512], F32) for j in range(2)]
    n_ = 0
    for h in range(2):
        P.dma(yf[:, :], k.dn_y.ap[2 * h:2 * h + 2].rearrange("i c t -> (i c) t"), [k.dn_y], [yf])
        P.dma(yr[:, :], k.dn_y.ap[4 + 2 * h:4 + 2 * h + 2].rearrange("i c t -> (i c) t"), [k.dn_y], [yr], eng="pool")
        P.dma(gt[:, :], k.featT[1664 + h * 128:1664 + (h + 1) * 128, :], [k.featT], [gt])
        for (dlo, dhi), (slo, shi), _ in seg_pairs(1):
            TT(P, "dve", yf[:, slo:shi], yf[:, slo:shi], yr[:, dlo:dhi][:, ::-1], ALU.add, [yf, yr], [yf])
        ACT(P, gt[:, :], gt[:, :], AF.Silu, [gt], [gt])
        for b in range(9):
            t0 = b * 512; tn = min(512, T - t0)
            s_ = sq[n_ % 2]; r_ = rs[n_ % 2]; pst = k.ps[n_ % 2]; n_ += 1
            ACT(P, s_[:, 0:tn], yf[:, t0:t0 + tn], AF.Square, [yf], [s_])
            P.mm(pst[:, 0:tn], k.blk64r[:, :], s_[:, 0:tn], [k.blk64r, s_], [pst])
            ACT(P, r_[:, 0:tn], pst[:, 0:tn], AF.Ln, [pst, epsb], [r_], scale=1.0 / 64, bias=epsb[:, :])
            ACT(P, r_[:, 0:tn], r_[:, 0:tn], AF.Exp, [r_], [r_], scale=-0.5)
            STT(P, "dve", r_[:, 0:tn], yf[:, t0:t0 + tn], ng[:, 0:1], r_[:, 0:tn], ALU.mult, ALU.mult, [yf, ng, r_], [r_])
            TT(P, "dve", ob[:, t0:t0 + tn], r_[:, 0:tn], gt[:, t0:t0 + tn], ALU.mult, [r_, gt], [ob])
        P.dma(k.yT[512 + h * 128:512 + (h + 1) * 128, :], ob[:, :], [ob], [(k.yT, ("dn", h))])


def phase_dn(k, l):
    phase_dn_prep(k, l)
    dplr_run(k, [("dn", k.dn_ops, k.dn_pc, k.dn_y, DN_MAP, (k.dn_G, k.dn_nG))])
    phase_dn_post(k, l)


def rw_shift(k, l, dst, src, r0, n, mu, c0):
    P = k.P; I = k.I
    P.dma(mu[0:n, :], I["rw_mu"].ap[l, :, r0:r0 + n].rearrange("m c -> c m"), [I["rw_mu"]], [mu], allow_slow_non_contiguous=True)
    TT(P, "dve", c0[0:n, :], mu[0:n, 0:1], mu[0:n, 1:2], ALU.add, [mu], [c0])
    TS(P, "dve", c0[0:n, :], c0[0:n, :], -1.0, 1.0, ALU.mult, ALU.add, [c0], [c0])
    if dst is src:
        raise ValueError
    TS(P, "dve", dst[0:n, :], src[0:n, :], c0[0:n, 0:1], None, ALU.mult, None, [src, c0], [dst])
    for (lo, hi) in ((0, CTX), (CTX, T)):
        STT(P, "dve", dst[0:n, lo + 1:hi], src[0:n, lo:hi - 1], mu[0:n, 0:1], dst[0:n, lo + 1:hi], ALU.mult, ALU.add, [src, mu, dst], [dst])
        STT(P, "dve", dst[0:n, lo:hi - 1], src[0:n, lo + 1:hi], mu[0:n, 1:2], dst[0:n, lo:hi - 1], ALU.mult, ALU.add, [src, mu, dst], [dst])


def phase_rw_prep(k, l):
    P = k.P; I = k.I
    P.barrier(); P.sb_reset(k.const_mark)
    def ft(name, dt=F32, n=128):
        return P.sbuf(name, [n, T], dt)
    kS = ft("rkS"); kk = ft("rkk"); lwS = ft("rlwS"); aK = ft("raK"); bb = ft("rbb"); G = ft("rG"); xin = G; E = ft("rE"); rm = ft("rrm", BF16)
    rS = ft("rrS", BF16); vS = ft("rvS", BF16); wlS = ft("rwlS", BF16); alS = ft("ralS", BF16)
    outs = [ft(f"rout{j}", BF16) for j in range(2)]
    mu = P.sbuf("rmu", [128, 2], F32); c0 = P.sbuf("rc0", [128, 1], F32)
    sq = [P.sbuf(f"rsq{j}", [128, 512], mybir.dt.float32r) for j in range(2)]
    rs = [P.sbuf(f"rrs{j}", [128, 512], F32) for j in range(2)]
    epsb = P.sbuf("repsb", [128, 1], F32)
    P.ew("pool", "memset", [], [epsb], epsb[:, :], EPS)
    P.ew("pool", "memset", [], [rm], rm[:, :], 1.0)
    P.ew("pool", "memset", [rm], [rm], rm.ap.rearrange("p (n c) -> p n c", c=64)[:, :, 0:1], 0.0)
    P.dma(xin[0:32, :], k.featT[768:800, :], [k.featT], [xin])
    rw_shift(k, l, E, xin, 768, 32, mu, c0)
    ACT(P, wlS[0:32, :], E[0:32, :], AF.Tanh, [E], [wlS])
    P.dma(xin[0:32, :], k.featT[800:832, :], [k.featT], [xin])
    rw_shift(k, l, E, xin, 800, 32, mu, c0)
    ACT(P, alS[0:32, :], E[0:32, :], AF.Copy, [E], [alS])
    wuf = P.sbuf("rwuf", [32, 2, 2, 256], F32); wub = P.sbuf("rwub", [32, 2, 2, 256], BF16)
    for d in range(2):
        P.dma(wuf[:, d, 0, :], I["rw_w_up"].ap[l, d], [I["rw_w_up"]], [wuf])
        P.dma(wuf[:, d, 1, :], I["rw_a_up"].ap[l, d], [I["rw_a_up"]], [wuf])
    P.ew("dve", "tensor_copy", [wuf], [wub], wub[:, :, :, :], wuf[:, :, :, :])
    cols = P.sbuf("rcols", [128, 8], F32)
    pcb = P.sbuf("rpcb", [128, NCHK], F32)
    no = 0; n_ = 0
    for h in range(2):
        hs = slice(h * 128, (h + 1) * 128)
        for d in range(2):
            P.dma(cols[:, d:d + 1], I["rw_w0"].ap[l, d, hs].rearrange("(c o) -> c o", o=1), [I["rw_w0"]], [cols])
            P.dma(cols[:, 2 + d:3 + d], I["rw_a0"].ap[l, d, hs].rearrange("(c o) -> c o", o=1), [I["rw_a0"]], [cols])
        P.dma(cols[:, 4:5], I["rw_k_k"].ap[l, hs].rearrange("(c o) -> c o", o=1), [I["rw_k_k"]], [cols])
        P.dma(cols[:, 5:6], I["rw_k_a"].ap[l, hs].rearrange("(c o) -> c o", o=1), [I["rw_k_a"]], [cols])
        P.dma(xin[:, :], k.featT[h * 128:(h + 1) * 128, :], [k.featT], [xin])
        rw_shift(k, l, rS, xin, h * 128, 128, mu, c0)
        P.dma(xin[:, :], k.featT[512 + h * 128:512 + (h + 1) * 128, :], [k.featT], [xin])
        rw_shift(k, l, vS, xin, 512 + h * 128, 128, mu, c0)
        P.dma(xin[:, :], k.featT[256 + h * 128:256 + (h + 1) * 128, :], [k.featT], [xin])
        rw_shift(k, l, kS, xin, 256 + h * 128, 128, mu, c0)
        TS(P, "dve", kk[:, :], kS[:, :], cols[:, 4:5], None, ALU.mult, None, [kS, cols], [kk])
        for b in range(9):
            t0 = b * 512; tn = min(512, T - t0)
            s_ = sq[n_ % 2]; r_ = rs[n_ % 2]; pst = k.ps[n_ % 2]; n_ += 1
            ACT(P, s_[:, 0:tn], kk[:, t0:t0 + tn], AF.Square, [kk], [s_])
            P.mm(pst[:, 0:tn], k.blk64r[:, :], s_[:, 0:tn], [k.blk64r, s_], [pst])
            ACT(P, r_[:, 0:tn], pst[:, 0:tn], AF.Ln, [pst, epsb], [r_], bias=epsb[:, :])
            ACT(P, r_[:, 0:tn], r_[:, 0:tn], AF.Exp, [r_], [r_], scale=-0.5)
            TT(P, "dve", kk[:, t0:t0 + tn], kk[:, t0:t0 + tn], r_[:, 0:tn], ALU.mult, [kk, r_], [kk])
        for d in range(2):
            i = d * 4 + 2 * h
            for b in range(9):
                t0 = b * 512; tn = min(512, T - t0)
                pw = k.ps[2 + (n_ % 2)]; pa = k.ps[4 + (n_ % 2)]; n_ += 1
                P.mm(pw[:, 0:tn], wub[:, d, 0, hs], wlS[0:32, t0:t0 + tn], [wub, wlS], [pw])
                P.mm(pa[:, 0:tn], wub[:, d, 1, hs], alS[0:32, t0:t0 + tn], [wub, alS], [pa])
                ACT(P, E[:, t0:t0 + tn], pw[:, 0:tn], AF.Sigmoid, [pw, cols], [E], bias=cols[:, d:d + 1])
                ACT(P, aK[:, t0:t0 + tn], pa[:, 0:tn], AF.Sigmoid, [pa, cols], [aK], bias=cols[:, 2 + d:3 + d])
            for (dlo, dhi), _, _ in seg_pairs(d):
                TS(P, "dve", lwS[:, dlo:dhi], nat2scan(E, d, dlo, dhi), -R_DECAY, None, ALU.mult, None, [E], [lwS])
            TT(P, "dve", bb[:, :], kk[:, :], aK[:, :], ALU.mult, [kk, aK], [bb])
            TS(P, "dve", aK[:, :], aK[:, :], -1.0, cols[:, 5:6], ALU.add, ALU.mult, [aK, cols], [aK])
            STT(P, "dve", aK[:, :], aK[:, :], 1.0, kS[:, :], ALU.add, ALU.mult, [aK, kS], [aK])
            P.add("dve", lambda e: e.tensor_tensor_scan(G[:, :], rm[:, :], lwS[:, :], 0.0, ALU.mult, ALU.add), [rm, lwS], [G])
            G3 = G.ap.rearrange("p (n c) -> p n c", c=64)
            ACT(P, pcb[:, :], G3[:, :, 63], AF.Exp, [G], [pcb])
            P.dma(k.rw_pc.ap[i:i + 2].rearrange("i c n -> (i c) n"), pcb[:, :], [pcb], [(k.rw_pc, i)])

            def emit(o_idx, fn):
                nonlocal no
                ot = outs[no % 2]; no += 1
                for (dlo, dhi), _, _ in seg_pairs(d):
                    fn(ot, dlo, dhi)
                P.dma(k.rw_ops.ap[o_idx, i:i + 2, :, :].rearrange("i c t -> (i c) t"), ot[:, :], [ot], [(k.rw_ops, (o_idx, i))], eng=("sp" if no % 2 else "pool"))
            sc = lambda tl: (lambda lo, hi: nat2scan(tl, d, lo, hi))
            kks, bs, kms, rs_, vs_ = sc(kk), sc(bb), sc(aK), sc(rS), sc(vS)
            TT(P, "dve", E[:, :], G[:, :], lwS[:, :], ALU.subtract, [G, lwS], [E])
            ACT(P, E[:, :], E[:, :], AF.Exp, [E], [E])
            emit(0, lambda ot, lo, hi: STT(P, "dve", ot[:, lo:hi], kks(lo, hi), -1.0, E[:, lo:hi], ALU.mult, ALU.mult, [kk, E], [ot]))
            ACT(P, E[:, :], G[:, :], AF.Exp, [G], [E], scale=-1.0)
            emit(1, lambda ot, lo, hi: TT(P, "dve", ot[:, lo:hi], bs(lo, hi), E[:, lo:hi], ALU.mult, [bb, E], [ot]))
            emit(2, lambda ot, lo, hi: TT(P, "dve", ot[:, lo:hi], kms(lo, hi), E[:, lo:hi], ALU.mult, [aK, E], [ot]))
            ACT(P, E[:, :], G[:, :], AF.Exp, [G], [E])
            emit(3, lambda ot, lo, hi: TT(P, "dve", ot[:, lo:hi], rs_(lo, hi), E[:, lo:hi], ALU.mult, [rS, E], [ot]))
            TT(P, "dve", E.ap.rearrange("p (n c) -> p n c", c=64), G3[:, :, 63:64].broadcast_to([128, NCHK, 64]), G3, ALU.subtract, [G], [E])
            ACT(P, E[:, :], E[:, :], AF.Exp, [E], [E])
            emit(4, lambda ot, lo, hi: TT(P, "dve", ot[:, lo:hi], bs(lo, hi), E[:, lo:hi], ALU.mult, [bb, E], [ot]))
            emit(5, lambda ot, lo, hi: TT(P, "dve", ot[:, lo:hi], kms(lo, hi), E[:, lo:hi], ALU.mult, [aK, E], [ot]))
            emit(6, lambda ot, lo, hi: ACT(P, ot[:, lo:hi], vs_(lo, hi), AF.Copy, [vS], [ot]))


R_DECAY = math.exp(-0.5)
RW_GN_EPS = 64e-5


def phase_rw_post(k, l):
    P = k.P; I = k.I
    P.barrier(); P.sb_reset(k.const_mark)
    def ft(name, dt=F32, n=128):
        return P.sbuf(name, [n, T], dt)
    xin = ft("qxin"); yf = ft("qyf"); yr = ft("qyr"); rS = ft("qrS"); kS = ft("qkS"); vS = ft("qvS"); tmp = ft("qtmp")
    glS = ft("qglS", BF16); ob = ft("qob", BF16)
    mu = P.sbuf("qmu", [128, 2], F32); c0 = P.sbuf("qc0", [128, 1], F32)
    cols = P.sbuf("qcols", [128, 4], F32)
    gne = P.sbuf("qgne", [128, 1], F32)
    P.ew("pool", "memset", [], [gne], gne[:, :], RW_GN_EPS)
    guf = P.sbuf("qguf", [64, 256], F32); gub = P.sbuf("qgub", [64, 256], BF16)
    P.dma(guf[:, :], I["rw_g_up"].ap[l], [I["rw_g_up"]], [guf])
    P.ew("dve", "tensor_copy", [guf], [gub], gub[:, :], guf[:, :])
    P.dma(xin[0:64, :], k.featT[832:896, :], [k.featT], [xin])
    rw_shift(k, l, tmp, xin, 832, 64, mu, c0)
    ACT(P, glS[0:64, :], tmp[0:64, :], AF.Sigmoid, [tmp], [glS])
    w = [[P.sbuf(f"qw{j}_{b}", [128, 512], F32) for b in range(2)] for j in range(3)]
    n_ = 0
    for h in range(2):
        hs = slice(h * 128, (h + 1) * 128)
        P.dma(cols[:, 0:1], I["rw_r_k"].ap[l, 2 * h:2 * h + 2].rearrange("h (c o) -> (h c) o", o=1), [I["rw_r_k"]], [cols])
        P.dma(cols[:, 1:2], I["rw_ln_g"].ap[l, hs].rearrange("(c o) -> c o", o=1), [I["rw_ln_g"]], [cols])
        P.dma(cols[:, 2:3], I["rw_ln_b"].ap[l, hs].rearrange("(c o) -> c o", o=1), [I["rw_ln_b"]], [cols])
        for dst, r0 in ((rS, h * 128), (kS, 256 + h * 128), (vS, 512 + h * 128)):
            P.dma(xin[:, :], k.featT[r0:r0 + 128, :], [k.featT], [xin])
            rw_shift(k, l, dst, xin, r0, 128, mu, c0)
        P.dma(yf[:, :], k.rw_y.ap[2 * h:2 * h + 2].rearrange("i c t -> (i c) t"), [k.rw_y], [yf])
        P.dma(yr[:, :], k.rw_y.ap[4 + 2 * h:4 + 2 * h + 2].rearrange("i c t -> (i c) t"), [k.rw_y], [yr], eng="pool")
        for (dlo, dhi), (slo, shi), _ in seg_pairs(1):
            TT(P, "dve", yf[:, slo:shi], yf[:, slo:shi], yr[:, dlo:dhi][:, ::-1], ALU.add, [yf, yr], [yf])
        STT(P, "dve", tmp[:, :], rS[:, :], cols[:, 0:1], kS[:, :], ALU.mult, ALU.mult, [rS, cols, kS], [tmp])
        for b in range(9):
            t0 = b * 512; tn = min(512, T - t0)
            ym, s_, o_ = w[0][n_ % 2], w[1][n_ % 2], w[2][n_ % 2]
            pm = k.ps[(n_ % 2) * 4]; pv = k.ps[(n_ % 2) * 4 + 1]; pb = k.ps[(n_ % 2) * 4 + 2]; pg = k.ps[(n_ % 2) * 4 + 3]; n_ += 1
            P.mm(pm[:, 0:tn], k.blk64[:, :], yf[:, t0:t0 + tn], [k.blk64, yf], [pm])
            STT(P, "dve", ym[:, 0:tn], pm[:, 0:tn], -1.0 / 64, yf[:, t0:t0 + tn], ALU.mult, ALU.add, [pm, yf], [ym])
            ACT(P, s_[:, 0:tn], ym[:, 0:tn], AF.Square, [ym], [s_])
            P.mm(pv[:, 0:tn], k.blk64[:, :], s_[:, 0:tn], [k.blk64, s_], [pv])
            ACT(P, s_[:, 0:tn], pv[:, 0:tn], AF.Ln, [pv, gne], [s_], scale=1.0 / 64, bias=gne[:, :])
            ACT(P, s_[:, 0:tn], s_[:, 0:tn], AF.Exp, [s_], [s_], scale=-0.5)
            TT(P, "dve", ym[:, 0:tn], ym[:, 0:tn], s_[:, 0:tn], ALU.mult, [ym, s_], [ym])
            TS(P, "dve", ym[:, 0:tn], ym[:, 0:tn], cols[:, 1:2], cols[:, 2:3], ALU.mult, ALU.add, [ym, cols], [ym])
            P.mm(pb[:, 0:tn], k.blk64[:, :], tmp[:, t0:t0 + tn], [k.blk64, tmp], [pb])
            TT(P, "dve", o_[:, 0:tn], pb[:, 0:tn], vS[:, t0:t0 + tn], ALU.mult, [pb, vS], [o_])
            TT(P, "dve", o_[:, 0:tn], o_[:, 0:tn], ym[:, 0:tn], ALU.add, [o_, ym], [o_])
            P.mm(pg[:, 0:tn], gub[:, hs], glS[0:64, t0:t0 + tn], [gub, glS], [pg])
            TT(P, "dve", ob[:, t0:t0 + tn], o_[:, 0:tn], pg[:, 0:tn], ALU.mult, [o_, pg], [ob])
        P.dma(k.yT[256 + h * 128:256 + (h + 1) * 128, :], ob[:, :], [ob], [(k.yT, ("rw", h))])


def phase_rw(k, l):
    phase_rw_prep(k, l)
    dplr_run(k, [("rw", k.rw_ops, k.rw_pc, k.rw_y, RW_MAP, None)])
    phase_rw_post(k, l)


def phase_rw_dn(k, l):
    phase_rw_prep(k, l)
    phase_dn_prep(k, l)
    dplr_run(k, [("rw", k.rw_ops, k.rw_pc, k.rw_y, RW_MAP, None), ("dn", k.dn_ops, k.dn_pc, k.dn_y, DN_MAP, (k.dn_G, k.dn_nG))])
    phase_rw_post(k, l)
    phase_dn_post(k, l)


from concourse.bass_utils import run_bass_kernel_spmd

_CACHE = {}


def kernel(**inputs):
    if "k" not in _CACHE:
        _CACHE["k"] = build()
    k = _CACHE["k"]
    f32 = np.float32
    shared = {n: np.ascontiguousarray(inputs[n], dtype=f32) for n in list(SHARED) + list(PER_LAYER)}
    in_maps = []
    for b in range(8):
        m = dict(shared)
        m["x"] = np.ascontiguousarray(inputs["x"][b], dtype=f32)
        m["ctx"] = np.ascontiguousarray(inputs["ctx"][b], dtype=f32)
        m["c"] = np.ascontiguousarray(inputs["c"][b], dtype=f32)
        in_maps.append(m)
    res = run_bass_kernel_spmd(k.nc, in_maps, core_ids=list(range(8)))
    return np.stack([np.asarray(r["out"], dtype=f32) for r in res.results], axis=0)
```
